# Optimizing a Trainium2 kernel written in Bass

```python
import jax, jax.numpy as jnp
from jax import lax
import numpy as np

D_MODEL = 1024
BATCH = 8
SEQ = 4096
DEPTH = 2

MEM_LEN = 256
HEAD_DIM = 64
NSA_HEADS = 8
NSA_KV_HEADS = 2
NSA_GROUP = NSA_HEADS // NSA_KV_HEADS
CMP_BLOCK = 32
CMP_STRIDE = 16
CMP_HIDDEN = 256
SLC_BLOCK = 64
SLC_TOP = 16
WINDOW = 512
NSA_QBLOCK = 64
SLC_FORCED_SCORE = 1e4
SGU_GROUPS = 4
SGU_CH = 128
SGU_CHUNK = 128
HGRN_HEADS = 4
HGRN_DK = 128
HGRN_DV = 128
HGRN_CHUNK = 64
CONV_CH = 512
CONV_WIDTH = 3
XATTN_HEADS = 4
XATTN_DIM = D_MODEL // XATTN_HEADS
N_EXPERTS = 32
TOP_K = 4
D_EXPERT = D_MODEL
SWIGLU_LIMIT = 7.0
SWIGLU_ALPHA = 1.702
MOE_BLOCK = 128

ROPE_THETA = 10000.0
LN_EPS = 1e-5
RMS_EPS = 1e-6
NEG_INF = -1e30
DEEPNORM_ALPHA = (2 * DEPTH) ** 0.25
DEEPNORM_BETA = (8 * DEPTH) ** -0.25

N_EVEN = (DEPTH + 1) // 2
N_ODD = DEPTH // 2
NSA_Q = NSA_HEADS * HEAD_DIM
NSA_KV = 3 * 2 * NSA_KV_HEADS * HEAD_DIM
NSA_GATES = 3 * NSA_HEADS
SGU_W = SGU_GROUPS * SGU_CH
EVEN_IN = NSA_Q + NSA_KV + NSA_GATES + 2 * SGU_W
EVEN_OUT = NSA_Q + SGU_W
HGRN_W = HGRN_HEADS * HGRN_DK
ODD_IN = 4 * HGRN_W + 3 * CONV_CH
ODD_OUT = HGRN_W + CONV_CH

kernel_name = "hybrid_nsa_gmlp_hgrn2_conv_moe_deepnorm"


def layer_norm(x, g, b):
    xf = x.astype(jnp.float32)
    mu = jnp.mean(xf, axis=-1, keepdims=True)
    var = jnp.mean(jnp.square(xf - mu), axis=-1, keepdims=True)
    return ((xf - mu) * lax.rsqrt(var + LN_EPS) * g + b).astype(x.dtype)


def rope_tables(positions):
    inv = 1.0 / (ROPE_THETA ** (jnp.arange(0, HEAD_DIM, 2, dtype=jnp.float32) / HEAD_DIM))
    ang = positions.astype(jnp.float32)[..., None] * inv
    return jnp.cos(ang)[:, :, None, :], jnp.sin(ang)[:, :, None, :]


def apply_rope(x, cos, sin):
    x1, x2 = jnp.split(x.astype(jnp.float32), 2, axis=-1)
    return jnp.concatenate([x1 * cos - x2 * sin, x2 * cos + x1 * sin], axis=-1).astype(x.dtype)


def masked_softmax(s, mask):
    s = jnp.where(mask, s.astype(jnp.float32), NEG_INF)
    return jax.nn.softmax(s, axis=-1) * mask


def cmp_to_slc_matrix(seq):
    nc = (seq - CMP_BLOCK) // CMP_STRIDE + 1
    ns = seq // SLC_BLOCK
    cs = np.arange(nc)[:, None] * CMP_STRIDE
    ss = np.arange(ns)[None, :] * SLC_BLOCK
    ov = np.clip(np.minimum(cs + CMP_BLOCK, ss + SLC_BLOCK) - np.maximum(cs, ss), 0, None)
    return jnp.asarray(ov / CMP_BLOCK, dtype=jnp.float32)


def nsa_mixer(q, kv, gates, cos, sin, cmp_pos, cmp_w1, cmp_w2):
    Bsz, S, _ = q.shape
    G, Hg, hd, QB = NSA_KV_HEADS, NSA_GROUP, HEAD_DIM, NSA_QBLOCK
    dt = q.dtype
    q = q.reshape(Bsz, S, NSA_HEADS, hd)
    q_rot = apply_rope(q, cos, sin)
    kv = kv.reshape(Bsz, S, 6, G, hd)
    k_cmp, v_cmp, k_slc, v_slc, k_win, v_win = [kv[:, :, i] for i in range(6)]
    k_slc = apply_rope(k_slc, cos, sin)
    k_win = apply_rope(k_win, cos, sin)

    nc = (S - CMP_BLOCK) // CMP_STRIDE + 1
    idx = np.arange(nc)[:, None] * CMP_STRIDE + np.arange(CMP_BLOCK)[None, :]

    def compress(t, pos, w1, w2):
        blocks = t[:, idx] + pos[None, None, :, None, :]
        blocks = blocks.transpose(0, 3, 1, 2, 4).reshape(Bsz, G, nc, CMP_BLOCK * hd)
        return jax.nn.gelu(blocks @ w1) @ w2

    kc = compress(k_cmp, cmp_pos[0], cmp_w1[0], cmp_w2[0])
    vc = compress(v_cmp, cmp_pos[1], cmp_w1[1], cmp_w2[1])
    cmp_end = np.arange(nc) * CMP_STRIDE + CMP_BLOCK - 1

    n_slc = S // SLC_BLOCK
    n_top = min(SLC_TOP, n_slc)
    ks_blk = k_slc.reshape(Bsz, n_slc, SLC_BLOCK, G, hd).transpose(0, 3, 1, 2, 4)
    vs_blk = v_slc.reshape(Bsz, n_slc, SLC_BLOCK, G, hd).transpose(0, 3, 1, 2, 4)
    m_cmp = cmp_to_slc_matrix(S)
    slc_ids = jnp.arange(n_slc)
    b_ix = jnp.arange(Bsz)[:, None, None, None]
    g_ix = jnp.arange(G)[None, None, :, None]

    kw_pad = jnp.pad(k_win, ((0, 0), (WINDOW, 0), (0, 0), (0, 0)))
    vw_pad = jnp.pad(v_win, ((0, 0), (WINDOW, 0), (0, 0), (0, 0)))

    gates = jax.nn.sigmoid(gates.astype(jnp.float32)).reshape(Bsz, S, G, Hg, 3)
    scale = hd ** -0.5

    def block(qi):
        s0 = qi * QB
        t = s0 + jnp.arange(QB)
        qb = lax.dynamic_slice_in_dim(q, s0, QB, axis=1).reshape(Bsz, QB, G, Hg, hd)
        qr = lax.dynamic_slice_in_dim(q_rot, s0, QB, axis=1).reshape(Bsz, QB, G, Hg, hd)
        gb = lax.dynamic_slice_in_dim(gates, s0, QB, axis=1)

        s = jnp.einsum('bqghd,bgnd->bqghn', qb, kc) * scale
        mask = (cmp_end[None, :] <= t[:, None])[None, :, None, None, :]
        p_cmp = masked_softmax(s, mask)
        o_cmp = jnp.einsum('bqghn,bgnd->bqghd', p_cmp.astype(dt), vc)

        imp = jnp.einsum('bqgn,nj->bqgj', p_cmp.sum(axis=3), m_cmp)
        cur = (t // SLC_BLOCK)[:, None]
        forced = ((slc_ids == 0) | (slc_ids == cur) | (slc_ids == cur - 1))[None, :, None, :]
        valid = (slc_ids <= cur)[None, :, None, :]
        score = jnp.where(forced, SLC_FORCED_SCORE, jnp.where(valid, imp, -1.0))
        _, sel = lax.top_k(score, n_top)
        k_sel = ks_blk[b_ix, g_ix, sel]
        v_sel = vs_blk[b_ix, g_ix, sel].reshape(Bsz, QB, G, n_top * SLC_BLOCK, hd)
        tok = sel[..., None] * SLC_BLOCK + jnp.arange(SLC_BLOCK)
        m_slc = (tok <= t[None, :, None, None, None]).reshape(Bsz, QB, G, 1, n_top * SLC_BLOCK)
        s = jnp.einsum('bqghd,bqgnkd->bqghnk', qr, k_sel).reshape(Bsz, QB, G, Hg, n_top * SLC_BLOCK) * scale
        p = masked_softmax(s, m_slc)
        o_slc = jnp.einsum('bqghm,bqgmd->bqghd', p.astype(dt), v_sel)

        kw = lax.dynamic_slice_in_dim(kw_pad, s0, QB + WINDOW, axis=1)
        vw = lax.dynamic_slice_in_dim(vw_pad, s0, QB + WINDOW, axis=1)
        kpos = s0 - WINDOW + jnp.arange(QB + WINDOW)
        dist = t[:, None] - kpos[None, :]
        m_win = (kpos[None, :] >= 0) & (dist >= 0) & (dist < WINDOW)
        s = jnp.einsum('bqghd,bkgd->bqghk', qr, kw) * scale
        p = masked_softmax(s, m_win[None, :, None, None, :])
        o_win = jnp.einsum('bqghk,bkgd->bqghd', p.astype(dt), vw)

        o = gb[..., 0:1] * o_cmp + gb[..., 1:2] * o_slc + gb[..., 2:3] * o_win
        return o.astype(dt).reshape(Bsz, QB, NSA_Q)

    out = lax.map(block, jnp.arange(S // QB))
    return out.transpose(1, 0, 2, 3).reshape(Bsz, S, NSA_Q)


def sgu_mixer(u, v, ln_g, ln_b, w_s, b_s):
    Bsz, S, _ = u.shape
    u = jax.nn.gelu(u)
    v = jax.nn.gelu(v).reshape(Bsz, S // SGU_CHUNK, SGU_CHUNK, SGU_GROUPS, SGU_CH)
    v = layer_norm(v, ln_g, ln_b)
    causal = jnp.tril(jnp.ones((SGU_CHUNK, SGU_CHUNK), dtype=w_s.dtype))
    mix = jnp.einsum('gts,bnsgc->bntgc', w_s * causal, v) + b_s.T[:, :, None]
    return u * mix.reshape(Bsz, S, SGU_W)


def hgrn2_mixer(q, f_raw, i, g, lb, norm_g):
    Bsz, S, _ = q.shape
    H, dk, dv, C = HGRN_HEADS, HGRN_DK, HGRN_DV, HGRN_CHUNK
    z = f_raw.astype(jnp.float32)
    log_f = jnp.logaddexp(jnp.log(lb), jnp.log1p(-lb) + jax.nn.log_sigmoid(z))
    k = (1.0 - lb) * jax.nn.sigmoid(-z)
    n = S // C

    def chunks(t, d):
        return t.astype(jnp.float32).reshape(Bsz, n, C, H, d).transpose(1, 0, 3, 2, 4)

    qc, kc, vc, lc = chunks(q, dk), chunks(k, dk), chunks(i, dv), chunks(log_f, dk)
    causal = jnp.tril(jnp.ones((C, C), dtype=bool))[None, None, :, :, None]

    def step(state, inp):
        qt, kt, vt, lt = inp
        b = jnp.cumsum(lt, axis=2)
        o_inter = jnp.einsum('bhtk,bhkv->bhtv', qt * jnp.exp(b), state)
        decay = jnp.exp(jnp.where(causal, b[:, :, :, None, :] - b[:, :, None, :, :], NEG_INF))
        attn = jnp.einsum('bhtk,bhsk,bhtsk->bhts', qt, kt, decay)
        o = o_inter + jnp.einsum('bhts,bhsv->bhtv', attn, vt)
        b_last = b[:, :, -1:, :]
        new_state = (jnp.exp(b_last[:, :, 0, :])[..., None] * state
                     + jnp.einsum('bhsk,bhsv->bhkv', kt * jnp.exp(b_last - b), vt))
        return new_state, o

    state0 = jnp.zeros((Bsz, H, dk, dv), jnp.float32)
    _, o = lax.scan(step, state0, (qc, kc, vc, lc))
    o = o.transpose(1, 0, 3, 2, 4).reshape(Bsz, S, H, dv)
    o = o * lax.rsqrt(jnp.mean(jnp.square(o), axis=-1, keepdims=True) + RMS_EPS) * norm_g
    return (o.reshape(Bsz, S, H * dv) * jax.nn.silu(g.astype(jnp.float32))).astype(q.dtype)


def short_conv_mixer(h, b_gate, c_gate, conv_w, conv_b):
    z = c_gate * h
    y = lax.conv_general_dilated(z, conv_w[:, None, :].astype(z.dtype), window_strides=(1,),
                                 padding=((CONV_WIDTH - 1, 0),),
                                 dimension_numbers=('NWC', 'WIO', 'NWC'),
                                 feature_group_count=CONV_CH) + conv_b
    return b_gate * y


def memory_cross_attention(x, mem, w_q, w_kv, w_o):
    Bsz, S, _ = x.shape
    q = (x @ w_q).reshape(Bsz, S, XATTN_HEADS, XATTN_DIM)
    kv = (mem @ w_kv).reshape(Bsz, mem.shape[1], 2, XATTN_HEADS, XATTN_DIM)
    s = jnp.einsum('bqhd,bkhd->bhqk', q, kv[:, :, 0]).astype(jnp.float32) * XATTN_DIM ** -0.5
    p = jax.nn.softmax(s, axis=-1).astype(x.dtype)
    o = jnp.einsum('bhqk,bkhd->bqhd', p, kv[:, :, 1]).reshape(Bsz, S, XATTN_HEADS * XATTN_DIM)
    return o @ w_o


def moe_ffn(x, w_router, b_router, w_gu, b_gu, w_dn, b_dn):
    Bsz, S, D = x.shape
    xt = x.reshape(-1, D)
    T = xt.shape[0]
    logits = (xt @ w_router).astype(jnp.float32) + b_router
    top_v, top_e = lax.top_k(logits, TOP_K)
    gate = jax.nn.softmax(top_v, axis=-1)
    TK = T * TOP_K
    e_flat = top_e.reshape(-1)
    tok_flat = jnp.arange(TK, dtype=jnp.int32) // TOP_K
    g_flat = gate.reshape(-1)
    order = jnp.argsort(e_flat)
    e_s, tok_s, g_s = e_flat[order], tok_flat[order], g_flat[order]
    counts = jnp.bincount(e_flat, length=N_EXPERTS)
    start = jnp.cumsum(counts) - counts
    padded = (counts + MOE_BLOCK - 1) // MOE_BLOCK * MOE_BLOCK
    pend = jnp.cumsum(padded)
    pstart = pend - padded
    dest = pstart[e_s] + (jnp.arange(TK) - start[e_s])
    R = (-(-TK // MOE_BLOCK) + N_EXPERTS) * MOE_BLOCK
    nb = R // MOE_BLOCK
    row_tok = jnp.zeros((R,), jnp.int32).at[dest].set(tok_s)
    row_gate = jnp.zeros((R,), jnp.float32).at[dest].set(g_s)
    blk_e = jnp.minimum(jnp.searchsorted(pend, jnp.arange(nb) * MOE_BLOCK, side='right'), N_EXPERTS - 1)

    def expert_block(args):
        e, toks = args
        h = xt[toks] @ w_gu[e] + b_gu[e]
        h_gate = jnp.minimum(h[:, :D_EXPERT], SWIGLU_LIMIT)
        h_up = jnp.clip(h[:, D_EXPERT:], -SWIGLU_LIMIT, SWIGLU_LIMIT)
        act = (h_up + 1.0) * (h_gate * jax.nn.sigmoid(h_gate * SWIGLU_ALPHA))
        return act @ w_dn[e] + b_dn[e]

    y = lax.map(expert_block, (blk_e, row_tok.reshape(nb, MOE_BLOCK))).reshape(R, D)
    out = jnp.zeros((T, D), jnp.float32).at[row_tok].add(y.astype(jnp.float32) * row_gate[:, None])
    return out.astype(x.dtype).reshape(Bsz, S, D)


def setup_inputs(seed: int = 0) -> dict:
    key = jax.random.key(seed)
    ks = jax.random.split(key, 32)
    f32 = jnp.float32

    def nrm(k, shape, fan_in, scale=1.0):
        return jax.random.normal(k, shape, f32) * (fan_in ** -0.5 * scale)

    def small(k, shape):
        return 0.01 * jax.random.normal(k, shape, f32)

    offset = jax.random.randint(ks[2], (BATCH, 1), 0, 2048, dtype=jnp.int32)
    positions = offset + jnp.arange(SEQ, dtype=jnp.int32)[None, :]
    return {
        "x": jax.random.normal(ks[0], (BATCH, SEQ, D_MODEL), f32),
        "mem": jax.random.normal(ks[1], (BATCH, MEM_LEN, D_MODEL), f32),
        "positions": positions,
        "w_in_even": nrm(ks[3], (N_EVEN, D_MODEL, EVEN_IN), D_MODEL),
        "nsa_cmp_pos": 0.1 * jax.random.normal(ks[4], (N_EVEN, 2, CMP_BLOCK, HEAD_DIM), f32),
        "nsa_cmp_w1": nrm(ks[5], (N_EVEN, 2, CMP_BLOCK * HEAD_DIM, CMP_HIDDEN), CMP_BLOCK * HEAD_DIM),
        "nsa_cmp_w2": nrm(ks[6], (N_EVEN, 2, CMP_HIDDEN, HEAD_DIM), CMP_HIDDEN),
        "sgu_ln_g": 1.0 + small(ks[7], (N_EVEN, SGU_GROUPS, SGU_CH)),
        "sgu_ln_b": small(ks[8], (N_EVEN, SGU_GROUPS, SGU_CH)),
        "sgu_w": nrm(ks[9], (N_EVEN, SGU_GROUPS, SGU_CHUNK, SGU_CHUNK), SGU_CHUNK),
        "sgu_b": 1.0 + small(ks[10], (N_EVEN, SGU_GROUPS, SGU_CHUNK)),
        "w_out_even": nrm(ks[11], (N_EVEN, EVEN_OUT, D_MODEL), EVEN_OUT, DEEPNORM_BETA),
        "w_in_odd": nrm(ks[12], (N_ODD, D_MODEL, ODD_IN), D_MODEL),
        "hgrn_lb_logits": 0.1 * jax.random.normal(ks[13], (DEPTH, HGRN_W), f32),
        "hgrn_norm_g": 1.0 + small(ks[14], (N_ODD, HGRN_HEADS, HGRN_DV)),
        "conv_w": nrm(ks[15], (N_ODD, CONV_WIDTH, CONV_CH), CONV_WIDTH),
        "conv_b": small(ks[16], (N_ODD, CONV_CH)),
        "w_out_odd": nrm(ks[17], (N_ODD, ODD_OUT, D_MODEL), ODD_OUT, DEEPNORM_BETA),
        "xattn_w_q": nrm(ks[18], (DEPTH, D_MODEL, XATTN_HEADS * XATTN_DIM), D_MODEL),
        "xattn_w_kv": nrm(ks[19], (DEPTH, D_MODEL, 2 * XATTN_HEADS * XATTN_DIM), D_MODEL),
        "xattn_w_o": nrm(ks[20], (DEPTH, XATTN_HEADS * XATTN_DIM, D_MODEL), XATTN_HEADS * XATTN_DIM, DEEPNORM_BETA),
        "ln_g": 1.0 + small(ks[21], (DEPTH, 3, D_MODEL)),
        "ln_b": small(ks[22], (DEPTH, 3, D_MODEL)),
        "router_w": nrm(ks[23], (DEPTH, D_MODEL, N_EXPERTS), D_MODEL),
        "router_b": small(ks[24], (DEPTH, N_EXPERTS)),
        "expert_w_gu": nrm(ks[25], (DEPTH, N_EXPERTS, D_MODEL, 2 * D_EXPERT), D_MODEL),
        "expert_b_gu": small(ks[26], (DEPTH, N_EXPERTS, 2 * D_EXPERT)),
        "expert_w_dn": nrm(ks[27], (DEPTH, N_EXPERTS, D_EXPERT, D_MODEL), D_EXPERT, DEEPNORM_BETA),
        "expert_b_dn": small(ks[28], (DEPTH, N_EXPERTS, D_MODEL)),
    }


def reference(x, mem, positions, w_in_even, nsa_cmp_pos, nsa_cmp_w1, nsa_cmp_w2, sgu_ln_g, sgu_ln_b,
              sgu_w, sgu_b, w_out_even, w_in_odd, hgrn_lb_logits, hgrn_norm_g, conv_w, conv_b,
              w_out_odd, xattn_w_q, xattn_w_kv, xattn_w_o, ln_g, ln_b, router_w, router_b,
              expert_w_gu, expert_b_gu, expert_w_dn, expert_b_dn):
    cos, sin = rope_tables(positions)
    lb_weights = jax.nn.softmax(hgrn_lb_logits.astype(jnp.float32), axis=0)
    even_split = [NSA_Q, NSA_Q + NSA_KV, NSA_Q + NSA_KV + NSA_GATES, NSA_Q + NSA_KV + NSA_GATES + SGU_W]
    for layer in range(DEPTH):
        j = layer // 2
        if layer % 2 == 0:
            proj = x @ w_in_even[j]
            q, kv, gts, u, v = jnp.split(proj, even_split, axis=-1)
            o_a = nsa_mixer(q, kv, gts, cos, sin, nsa_cmp_pos[j], nsa_cmp_w1[j], nsa_cmp_w2[j])
            o_b = sgu_mixer(u, v, sgu_ln_g[j], sgu_ln_b[j], sgu_w[j], sgu_b[j])
            mix = jnp.concatenate([o_a, o_b], axis=-1) @ w_out_even[j]
        else:
            proj = x @ w_in_odd[j]
            q, f_raw, i_in, g_out, h, b_gate, c_gate = jnp.split(proj, 7, axis=-1)
            lb = jnp.sum(lb_weights[1:layer + 1], axis=0)
            o_c = hgrn2_mixer(q, f_raw, i_in, g_out, lb, hgrn_norm_g[j])
            o_d = short_conv_mixer(h, b_gate, c_gate, conv_w[j], conv_b[j])
            mix = jnp.concatenate([o_c, o_d], axis=-1) @ w_out_odd[j]
        x = layer_norm(DEEPNORM_ALPHA * x + mix, ln_g[layer, 0], ln_b[layer, 0])
        xa = memory_cross_attention(x, mem, xattn_w_q[layer], xattn_w_kv[layer], xattn_w_o[layer])
        x = layer_norm(DEEPNORM_ALPHA * x + xa, ln_g[layer, 1], ln_b[layer, 1])
        ff = moe_ffn(x, router_w[layer], router_b[layer], expert_w_gu[layer], expert_b_gu[layer],
                     expert_w_dn[layer], expert_b_dn[layer])
        x = layer_norm(DEEPNORM_ALPHA * x + ff, ln_g[layer, 2], ln_b[layer, 2])
    return x
```

```python
import numpy as np
import ml_dtypes
from contextlib import ExitStack
import concourse.bass as bass
import concourse.mybir as mybir
from concourse.bass_utils import run_bass_kernel_spmd

F32 = mybir.dt.float32
BF16 = mybir.dt.bfloat16
I32 = mybir.dt.int32
U32 = mybir.dt.uint32
AF = mybir.ActivationFunctionType
ALU = mybir.AluOpType

S = 4096
D = 1024
NT = S // 128
NG = S // 512
ALPHA = 4 ** 0.25
LN_EPS = 1e-5
RMS_EPS = 1e-6
NEGB = -32768.0
CAP = 768


class Res:
    __slots__ = ("name", "w", "r", "dsem", "excl")

    def __init__(self, name):
        self.name = name
        self.excl = False
        self.w = {}
        self.r = {}
        self.dsem = None


class T:
    __slots__ = ("t", "r")

    def __init__(self, t, name):
        self.t = t
        self.r = Res(name)

    def __getitem__(self, k):
        return self.t[k]


def _r(x):
    return x.r if isinstance(x, T) else x


class Sem:
    __slots__ = ("h", "count", "uid")
    _n = 0

    def __init__(self, h):
        self.h = h
        self.count = 0
        Sem._n += 1
        self.uid = Sem._n


class Ctx:
    def __init__(self, nc, same_engine_sync=True):
        self.nc = nc
        self.eng = {"pe": nc.tensor, "dve": nc.vector, "act": nc.scalar, "pool": nc.gpsimd, "sp": nc.sync}
        self.esem = {k: Sem(nc.alloc_semaphore("es_" + k)) for k in self.eng}
        self.waited = {}
        self.same = same_engine_sync
        self.free_dsems = []
        self.all_dsems = []
        self.barrier = {}
        self.n_dsem = 0
        self.n_inst = 0

    def _dsem(self, r):
        if r.dsem is None:
            if self.free_dsems:
                r.dsem = self.free_dsems.pop()
            else:
                r.dsem = Sem(self.nc.alloc_semaphore("ds%d" % self.n_dsem))
                self.all_dsems.append(r.dsem)
                self.n_dsem += 1
        return r.dsem

    def set_barrier(self):
        bar = {s.uid: (s, s.count) for s in self.esem.values() if s.count > 0}
        for ds in self.all_dsems:
            if ds.count > 0:
                bar[ds.uid] = (ds, 16 * ds.count)
        self.barrier = bar

    def release(self, r):
        if r.dsem is not None:
            self.free_dsems.append(r.dsem)
            r.dsem = None

    def _collect(self, reads, writes, skip=None):
        deps = {}

        def add(d):
            for k, (s, v) in d.items():
                if skip is not None and s is skip:
                    continue
                if k not in deps or deps[k][1] < v:
                    deps[k] = (s, v)
        for r in reads:
            add(r.w)
        for w in writes:
            add(w.w)
            add(w.r)
        return deps

    def _waits(self, ename, deps):
        e = self.eng[ename]
        own = self.esem[ename].uid
        for k, (s, v) in deps.items():
            if k == own and (ename == "pe" or not self.same):
                continue
            key = (ename, k)
            if self.waited.get(key, 0) >= v:
                continue
            e.wait_ge(s.h, v)
            self.waited[key] = v
            self.n_inst += 1

    def op(self, ename, fn, reads=(), writes=()):
        reads = [_r(x) for x in reads]
        writes = [_r(x) for x in writes]
        writes = writes + [x for x in reads if x.excl and x not in writes]
        reads = [x for x in reads if not x.excl]
        self._waits(ename, self._collect(reads, writes))
        ins = fn(self.eng[ename])
        s = self.esem[ename]
        ins.then_inc(s.h, 1)
        s.count += 1
        v = s.count
        self.n_inst += 1
        for w in writes:
            w.w = {s.uid: (s, v)}
            w.r = {}
        for r in reads:
            if r not in writes:
                r.r[s.uid] = (s, v)
        return ins

    def dma(self, q, out, in_, reads=(), writes=(), disjoint=False, **kw):
        reads = [_r(x) for x in reads]
        writes = [_r(x) for x in writes]
        dst = writes[0]
        ds = self._dsem(dst)
        self._waits(q, self._collect(reads, writes, skip=ds if disjoint else None))
        ins = self.eng[q].dma_start(out=out, in_=in_, **kw)
        ins.then_inc(ds.h, 16)
        ds.count += 1
        v = 16 * ds.count
        self.n_inst += 1
        for w in writes:
            if disjoint:
                w.w[ds.uid] = (ds, v)
            else:
                w.w = {ds.uid: (ds, v)}
                w.r = {}
        for r in reads:
            r.r[ds.uid] = (ds, v)
        return ins

    def idma(self, out, out_off, in_, in_off, reads=(), writes=(), disjoint=False, **kw):
        reads = [_r(x) for x in reads]
        writes = [_r(x) for x in writes]
        dst = writes[0]
        ds = self._dsem(dst)
        self._waits("pool", self._collect(reads, writes, skip=ds if disjoint else None))
        ins = self.eng["pool"].indirect_dma_start(out=out, out_offset=out_off, in_=in_, in_offset=in_off, **kw)
        ins.then_inc(ds.h, 16)
        ds.count += 1
        v = 16 * ds.count
        self.n_inst += 1
        for w in writes:
            if disjoint:
                w.w[ds.uid] = (ds, v)
            else:
                w.w = {ds.uid: (ds, v)}
                w.r = {}
        for r in reads:
            r.r[ds.uid] = (ds, v)
        return ins

    def wait_all(self, ename, rs):
        self._waits(ename, self._collect([_r(x) for x in rs], []))


class Builder:
    def __init__(self, nc, dbg=None):
        self.nc = nc
        self.c = Ctx(nc)
        self.dbg = dbg or {}
        self.inp = LazyInputs(self)
        self.psb = []
        self.rot_i = 0

    def din(self, name, shape, dt):
        ap = self.nc.dram_tensor(name, list(shape), dt, kind="ExternalInput").ap()
        self.inp[name] = ap
        return ap

    def dscratch(self, name, shape, dt):
        kind = "ExternalOutput" if name in self.dbg else "Internal"
        t = self.nc.dram_tensor(name, list(shape), dt, kind=kind)
        return T(t.ap(), name)

    def sb(self, es, name, shape, dt):
        self.n_sb = getattr(self, "n_sb", 0) + 1
        name = "%s_%d" % (name, self.n_sb)
        t = es.enter_context(self.nc.sbuf_tensor(name, list(shape), dt))
        tt = T(t, name)
        tt.r.w = dict(self.c.barrier)
        es.callback(self.c.release, tt.r)
        return tt

    def phase(self):
        b = self

        class _Ph(ExitStack):
            def __exit__(self, *a):
                r = super().__exit__(*a)
                b.c.set_barrier()
                return r
        return _Ph()

    def init_psum(self, es):
        for i in range(8):
            t = es.enter_context(self.nc.psum_tensor("psb%d" % i, [128, 512], F32))
            self.psb.append(T(t, "psb%d" % i))
            self.psb[-1].r.excl = True

    def rot(self, subset=None):
        subset = subset or list(range(8))
        b = self.psb[subset[self.rot_i % len(subset)]]
        self.rot_i += 1
        return b

    def V(self, fn, r, w):
        return self.c.op("dve", fn, r, w)

    def A(self, fn, r, w):
        return self.c.op("act", fn, r, w)

    def G(self, fn, r, w):
        return self.c.op("pool", fn, r, w)

    def P(self, fn, r, w):
        return self.c.op("pe", fn, r, w)

    def ld(self, out, in_, r, w, **kw):
        return self.c.dma("sp", out, in_, r, w, **kw)

    def st(self, out, in_, r, w, **kw):
        return self.c.dma("sp", out, in_, r, w, **kw)

    def sta(self, out, in_, r, w, **kw):
        return self.c.dma("act", out, in_, r, w, **kw)

    def ldw(self, out, in_, w, **kw):
        return self.c.dma("pool", out, in_, [], w, **kw)

    def mm_acc(self, ps_ap, ps_T, pairs, reads):
        n = len(pairs)
        for i, (l, rh) in enumerate(pairs):
            self.P(lambda e, l=l, rh=rh, i=i: e.matmul(ps_ap, lhsT=l, rhs=rh, start=(i == 0), stop=(i == n - 1)),
                   reads, [ps_T])

    def load_consts(self, es):
        nc = self.nc
        self.ident_f = self.sb(es, "ident_f", [128, 128], F32)
        self.ld(self.ident_f[:], self.inp["c_ident_f"], [], [self.ident_f])
        self.ident_b = self.sb(es, "ident_b", [128, 128], BF16)
        self.ld(self.ident_b[:], self.inp["c_ident_b"], [], [self.ident_b])

    def ln_setup(self, es, li, which, tag):
        g = self.sb(es, "lng" + tag, [128, D], F32)
        b = self.sb(es, "lnb" + tag, [128, D], F32)
        self.ld(g[:], self.inp["ln_g"][li, which:which + 1, :].broadcast_to([128, D]), [], [g])
        self.ld(b[:], self.inp["ln_b"][li, which:which + 1, :].broadcast_to([128, D]), [], [b])
        tmp = dict(
            g=g, b=b,
            st=self.sb(es, "lnst" + tag, [128, 2, 6], F32),
            mv=self.sb(es, "lnmv" + tag, [128, 2], F32),
            rs=self.sb(es, "lnrs" + tag, [128, 1], F32),
            xn=[self.sb(es, "lnxn%d" % i + tag, [128, D], F32) for i in range(2)],
            xTt=[self.sb(es, "lnxT%d" % i + tag, [128, 8, 128], BF16) for i in range(2)],
            xr=[self.sb(es, "lnxr%d" % i + tag, [128, D], F32) for i in range(2)],
            y=self.sb(es, "lny" + tag, [128, D], F32),
            i=0,
        )
        return tmp

    def ln_preload(self, L, ti, xres_src):
        k = L.setdefault("ip", 0) % 2
        L["ip"] += 1
        xr = L["xr"][k]
        self.ld(xr[:], xres_src.t[ti * 128:(ti + 1) * 128, :], [xres_src], [xr])
        return xr

    def ln_tile(self, L, ti, f_aps, f_res, xres_src, xres_dst, xT_dst, final_out=None, hook=None, psum_sub=None, xr_pre=None, defer=False):
        k = L["i"] % 2
        L["i"] += 1
        xr, xn, xTt, y = L["xr"][k], L["xn"][k], L["xTt"][k], L["y"]
        if xr_pre is not None:
            xr = xr_pre
        else:
            self.ld(xr[:], xres_src.t[ti * 128:(ti + 1) * 128, :], [xres_src], [xr])
        for h in range(2):
            self.V(lambda e, h=h: e.scalar_tensor_tensor(out=y[:, h * 512:(h + 1) * 512], in0=xr[:, h * 512:(h + 1) * 512],
                                                         scalar=float(ALPHA), in1=f_aps[h], op0=ALU.mult, op1=ALU.add),
                   [xr] + list(f_res), [y])
        for h in range(2):
            self.V(lambda e, h=h: e.bn_stats(out=L["st"][:, h, :], in_=y[:, h * 512:(h + 1) * 512]), [y], [L["st"]])
        self.V(lambda e: e.bn_aggr(out=L["mv"][:], in_=L["st"][:].rearrange("p a b -> p (a b)")), [L["st"]], [L["mv"]])
        self.A(lambda e: e.activation(out=L["rs"][:], in_=L["mv"][:, 1:2], func=AF.Ln, bias=LN_EPS, scale=1.0), [L["mv"]], [L["rs"]])
        self.A(lambda e: e.activation(out=L["rs"][:], in_=L["rs"][:], func=AF.Exp, scale=-0.5), [L["rs"]], [L["rs"]])
        self.V(lambda e: e.tensor_scalar(out=xn[:], in0=y[:], scalar1=L["mv"][:, 0:1], scalar2=L["rs"][:, 0:1],
                                         op0=ALU.subtract, op1=ALU.mult), [y, L["mv"], L["rs"]], [xn])
        self.V(lambda e: e.tensor_tensor(out=xn[:], in0=xn[:], in1=L["g"][:], op=ALU.mult), [xn, L["g"]], [xn])
        self.V(lambda e: e.tensor_tensor(out=xn[:], in0=xn[:], in1=L["b"][:], op=ALU.add), [xn, L["b"]], [xn])
        if final_out is not None:
            self.sta(final_out.t[ti * 128:(ti + 1) * 128, :], xn[:], [xn], [final_out], disjoint=True)
            return
        self.c.dma(L.get("stq", "act"), xres_dst.t[ti * 128:(ti + 1) * 128, :], xn[:], [xn], [xres_dst], disjoint=True)

        def part_b():
            self.transpose_store(xn, xTt, ti, xT_dst, hook=hook, psum_sub=psum_sub)
        if defer:
            return part_b
        part_b()

    def transpose_store(self, xn, xTt, ti, xT_dst, hook=None, psum_sub=None):
        pts = []
        for half in range(2):
            pt = self.rot(psum_sub)
            for j in range(4):
                ch = half * 4 + j
                self.P(lambda e, j=j, ch=ch: e.transpose(out=pt[:, j * 128:(j + 1) * 128], in_=xn[:, ch * 128:(ch + 1) * 128],
                                                         identity=self.ident_f[:]), [xn, self.ident_f], [pt])
            self.A(lambda e, half=half: e.activation(out=xTt[:, half * 4:(half + 1) * 4, :],
                                                      in_=pt[:].rearrange("p (a b) -> p a b", a=4), func=AF.Copy), [pt], [xTt])
            pts.append(pt)
        if hook is not None:
            hook(ti, pts, xn)
        self.sta(xT_dst.t[:, :, ti * 128:(ti + 1) * 128].rearrange("c p t -> p c t"), xTt[:], [xTt], [xT_dst], disjoint=True)

    def phase_t0(self, x_in, xT_dst):
        with self.phase() as es:
            xs = [self.sb(es, "t0x%d" % i, [128, D], F32) for i in range(2)]
            xTt = [self.sb(es, "t0xT%d" % i, [128, 8, 128], BF16) for i in range(2)]
            self.ld(xs[0][:], x_in.t[0:128, :], [x_in], [xs[0]])
            for ti in range(NT):
                k = ti % 2
                if ti + 1 < NT:
                    self.ld(xs[1 - k][:], x_in.t[(ti + 1) * 128:(ti + 2) * 128, :], [x_in], [xs[1 - k]])
                self.transpose_store(xs[k], xTt[k], ti, xT_dst)

    def phase_outproj_ln(self, mixT, w_ap, li, which, xres_src, xres_dst, xT_dst):
        with self.phase() as es:
            w = self.sb(es, "opw", [128, 8, D], BF16)
            for kk in range(8):
                self.ldw(w[:, kk, :], w_ap[kk * 128:(kk + 1) * 128, :], [w], disjoint=True)
            L = self.ln_setup(es, li, which, "op")
            mt = [self.sb(es, "opm%d" % i, [128, 8, 128], BF16) for i in range(2)]

            def pre(ti):
                k = ti % 2
                self.ld(mt[k][:], mixT.t[:, :, ti * 128:(ti + 1) * 128].rearrange("c p t -> p c t"), [mixT], [mt[k]])
                xr = self.ln_preload(L, ti, xres_src)
                ps = [self.rot(), self.rot()]
                for h in range(2):
                    self.mm_acc(ps[h][:, :], ps[h], [(mt[k][:, kk, :], w[:, kk, h * 512:(h + 1) * 512]) for kk in range(8)], [mt[k], w])
                return ps, xr
            nxt = pre(0)
            for ti in range(NT):
                cur = nxt
                if ti + 1 < NT:
                    nxt = pre(ti + 1)
                ps, xr = cur
                self.ln_tile(L, ti, [ps[0][:, :], ps[1][:, :]], ps, xres_src, xres_dst, xT_dst, xr_pre=xr)

    def rope_tables(self, es, pos_ap):
        cos = self.sb(es, "ropecos", [128, S], F32)
        sin = self.sb(es, "ropesin", [128, S], F32)
        invf = self.sb(es, "invf", [128, 1], F32)
        self.ld(invf[:], self.inp["c_invf"], [], [invf])
        twopi = 2 * np.pi
        c1 = float(np.float32(6.28125))
        c2 = float(twopi - 6.28125)
        with self.phase() as es2:
            pi_ = self.sb(es2, "rp_pi", [128, 1024], I32)
            ang = self.sb(es2, "rp_ang", [128, 1024], F32)
            ki = self.sb(es2, "rp_ki", [128, 1024], I32)
            kf = self.sb(es2, "rp_kf", [128, 1024], F32)
            rr = self.sb(es2, "rp_rr", [128, 1024], F32)
            w0 = self.sb(es2, "rp_w0", [128, 1024], F32)
            for q in range(4):
                sl = slice(q * 1024, (q + 1) * 1024)
                self.ld(pi_[:], pos_ap[0:1, sl].broadcast_to([128, 1024]), [], [pi_])
                self.V(lambda e: e.tensor_copy(out=ang[:], in_=pi_[:]), [pi_], [ang])
                self.V(lambda e: e.tensor_scalar(out=ang[:], in0=ang[:], scalar1=invf[:, 0:1], scalar2=None, op0=ALU.mult), [ang, invf], [ang])
                self.V(lambda e: e.tensor_scalar(out=ki[:], in0=ang[:], scalar1=float(1 / twopi), scalar2=None, op0=ALU.mult), [ang], [ki])
                self.V(lambda e: e.tensor_copy(out=kf[:], in_=ki[:]), [ki], [kf])
                self.V(lambda e: e.scalar_tensor_tensor(out=rr[:], in0=kf[:], scalar=-c1, in1=ang[:], op0=ALU.mult, op1=ALU.add), [kf, ang], [rr])
                self.V(lambda e: e.scalar_tensor_tensor(out=rr[:], in0=kf[:], scalar=-c2, in1=rr[:], op0=ALU.mult, op1=ALU.add), [kf, rr], [rr])
                for (dst, shift) in ((sin, 0.0), (cos, np.pi / 2)):
                    self.V(lambda e: e.tensor_scalar(out=w0[:], in0=rr[:], scalar1=float(shift), scalar2=float(np.pi), op0=ALU.add, op1=ALU.is_gt), [rr], [w0])
                    self.V(lambda e: e.tensor_scalar(out=w0[:], in0=w0[:], scalar1=float(-twopi), scalar2=float(shift), op0=ALU.mult, op1=ALU.add), [w0], [w0])
                    self.V(lambda e: e.tensor_tensor(out=w0[:], in0=w0[:], in1=rr[:], op=ALU.add), [w0, rr], [w0])
                    self.V(lambda e: e.tensor_scalar(out=w0[:], in0=w0[:], scalar1=float(-np.pi), scalar2=float(np.pi), op0=ALU.max, op1=ALU.min), [w0], [w0])
                    self.A(lambda e, dst=dst: e.activation(out=dst[:, sl], in_=w0[:], func=AF.Sin), [w0], [dst])
        return cos, sin

    def phase_even_proj(self, j, xT, pos_ap, d):
        inp = self.inp
        W = inp["w_in_even"][j]
        with self.phase() as es:
            cos, sin = self.rope_tables(es, pos_ap)
            rotm = self.sb(es, "rotm", [128, 128], BF16)
            self.ld(rotm[:], inp["c_rotm"], [], [rotm])

            def wload(name, cols, a):
                t = self.sb(es, name, [128, 8, cols], BF16)
                for kk in range(8):
                    self.ldw(t[:, kk, :], W[kk * 128:(kk + 1) * 128, a:a + cols], [t], disjoint=True)
                return t
            wq = wload("wq", 512, 0)
            wk4 = self.sb(es, "wk4", [128, 8, 4, 128], BF16)
            for i, a in enumerate((512, 640, 768, 1024)):
                for kk in range(8):
                    self.ldw(wk4[:, kk, i, :], W[kk * 128:(kk + 1) * 128, a:a + 128], [wk4], disjoint=True)
            wvt = self.sb(es, "wvt", [128, 8, 280], BF16)
            for (o, a, n) in ((0, 896, 128), (128, 1152, 128), (256, 1280, 24)):
                for kk in range(8):
                    self.ldw(wvt[:, kk, o:o + n], W[kk * 128:(kk + 1) * 128, a:a + n], [wvt], disjoint=True)
            wu = wload("wu", 512, 1304)
            wv = wload("wv", 512, 1816)
            lng = self.sb(es, "sgu_g", [128, 512], F32)
            lnb = self.sb(es, "sgu_bt", [128, 512], F32)
            self.ld(lng[:], inp["sgu_ln_g"][j:j + 1].rearrange("o g c -> o (g c)").broadcast_to([128, 512]), [], [lng])
            self.ld(lnb[:], inp["sgu_ln_b"][j:j + 1].rearrange("o g c -> o (g c)").broadcast_to([128, 512]), [], [lnb])
            bb = self.sb(es, "sgu_bb", [128, 4, 128], F32)
            self.ld(bb[:].rearrange("p g t -> p (g t)"), inp["sgu_b"][j:j + 1].rearrange("o g c -> o (g c)").broadcast_to([128, 512]), [], [bb])
            wmf = self.sb(es, "sgu_wf", [128, 4, 128], F32)
            tri = self.sb(es, "sgu_tri", [128, 128], F32)
            self.ld(wmf[:], inp["sgu_wT"][j].rearrange("g s t -> s g t"), [], [wmf])
            self.ld(tri[:], inp["c_tri"], [], [tri])
            wm = self.sb(es, "sgu_wm", [128, 4, 128], BF16)
            for g in range(4):
                self.V(lambda e, g=g: e.tensor_tensor(out=wm[:, g, :], in0=wmf[:, g, :], in1=tri[:], op=ALU.mult), [wmf, tri], [wm])
            xs = [self.sb(es, "ep_x%d" % i, [128, 8, 512], BF16) for i in range(2)]
            q_sb = self.sb(es, "ep_q", [128, 4, 512], BF16)
            qr_sb = self.sb(es, "ep_qr", [128, 4, 512], BF16)
            k_sb = self.sb(es, "ep_k", [128, 4, 512], BF16)
            kt_sb = self.sb(es, "ep_kt", [128, 512], BF16)
            ug = self.sb(es, "ep_ug", [128, 4, 512], BF16)
            ob = self.sb(es, "ep_ob", [128, 4, 512], BF16)
            t1 = self.sb(es, "ep_t1", [128, 512], F32)
            t2 = self.sb(es, "ep_t2", [128, 512], F32)
            vg = self.sb(es, "ep_vg", [128, 512], F32)
            vn = self.sb(es, "ep_vn", [128, 512], BF16)
            st = self.sb(es, "ep_st", [128, 4, 6], F32)
            mv = self.sb(es, "ep_mv", [128, 4, 2], F32)
            rs = self.sb(es, "ep_rs", [128, 4], F32)
            sg1 = self.sb(es, "ep_sg1", [128, 4, 128], F32)
            vs_st = self.sb(es, "ep_vs", [128, 4, 2, 65], BF16)
            vw_st = self.sb(es, "ep_vw", [128, 4, 2, 65], BF16)
            gt_st = self.sb(es, "ep_gt", [128, 4, 24], F32)
            self.G(lambda e: e.memset(vs_st[:], 1.0), [], [vs_st])
            self.G(lambda e: e.memset(vw_st[:], 1.0), [], [vw_st])

            def rope(ps, src_bf, dst_ap, dstT, Q):
                tsl = slice(Q * 512, (Q + 1) * 512)
                p2 = self.rot()
                self.P(lambda e: e.matmul(p2[:, :], lhsT=rotm[:], rhs=src_bf[0], start=True, stop=True), [rotm, src_bf[1]], [p2])
                self.V(lambda e: e.tensor_tensor(out=t1[:], in0=ps[:, :], in1=cos[:, tsl], op=ALU.mult), [ps, cos], [t1])
                self.V(lambda e: e.tensor_tensor(out=t2[:], in0=p2[:, :], in1=sin[:, tsl], op=ALU.mult), [p2, sin], [t2])
                self.V(lambda e: e.tensor_tensor(out=dst_ap, in0=t1[:], in1=t2[:], op=ALU.add), [t1, t2], [dstT])

            for Q in range(NG):
                x = xs[Q % 2]
                tsl = slice(Q * 512, (Q + 1) * 512)
                if Q == 0:
                    self.ld(x[:], xT.t[:, :, tsl].rearrange("c p t -> p c t"), [xT], [x])
                if Q + 1 < NG:
                    self.ld(xs[(Q + 1) % 2][:], xT.t[:, :, (Q + 1) * 512:(Q + 2) * 512].rearrange("c p t -> p c t"), [xT], [xs[(Q + 1) % 2]])
                for cq in range(4):
                    ps = self.rot()
                    self.mm_acc(ps[:, :], ps, [(wq[:, kk, cq * 128:(cq + 1) * 128], x[:, kk, :]) for kk in range(8)], [wq, x])
                    self.A(lambda e: e.activation(out=q_sb[:, cq, :], in_=ps[:, :], func=AF.Copy), [ps], [q_sb])
                    rope(ps, (q_sb[:, cq, :], q_sb), qr_sb[:, cq, :], qr_sb, Q)
                self.st(d["q"].t[:, :, tsl].rearrange("c p t -> p c t"), q_sb[:], [q_sb], [d["q"]], disjoint=True)
                self.st(d["qr"].t[:, :, tsl].rearrange("c p t -> p c t"), qr_sb[:], [qr_sb], [d["qr"]], disjoint=True)
                for i in range(4):
                    ps = self.rot()
                    self.mm_acc(ps[:, :], ps, [(wk4[:, kk, i, :], x[:, kk, :]) for kk in range(8)], [wk4, x])
                    if i < 2:
                        self.A(lambda e: e.activation(out=k_sb[:, i, :], in_=ps[:, :], func=AF.Copy), [ps], [k_sb])
                    else:
                        self.A(lambda e: e.activation(out=kt_sb[:], in_=ps[:, :], func=AF.Copy), [ps], [kt_sb])
                        rope(ps, (kt_sb[:], kt_sb), k_sb[:, i, :], k_sb, Q)
                for i, nm in enumerate(("kc", "vc", "ks", "kw")):
                    self.st(d[nm].t[:, tsl], k_sb[:, i, :], [k_sb], [d[nm]], disjoint=True)
                for cu in range(4):
                    ps = self.rot()
                    self.mm_acc(ps[:, :], ps, [(wu[:, kk, cu * 128:(cu + 1) * 128], x[:, kk, :]) for kk in range(8)], [wu, x])
                    self.A(lambda e: e.activation(out=ug[:, cu, :], in_=ps[:, :], func=AF.Gelu_apprx_tanh), [ps], [ug])
                for jt in range(4):
                    xl = lambda kk: x[:, kk, jt * 128:(jt + 1) * 128]
                    ps = self.rot()
                    self.mm_acc(ps[:, :], ps, [(xl(kk), wv[:, kk, :]) for kk in range(8)], [wv, x])
                    self.A(lambda e: e.activation(out=vg[:], in_=ps[:, :], func=AF.Gelu_apprx_tanh), [ps], [vg])
                    for g in range(4):
                        self.V(lambda e, g=g: e.bn_stats(out=st[:, g, :], in_=vg[:, g * 128:(g + 1) * 128]), [vg], [st])
                    for g in range(4):
                        self.V(lambda e, g=g: e.bn_aggr(out=mv[:, g, :], in_=st[:, g, :]), [st], [mv])
                    self.A(lambda e: e.activation(out=rs[:], in_=mv[:, :, 1], func=AF.Sqrt, bias=LN_EPS, scale=1.0), [mv], [rs])
                    self.V(lambda e: e.reciprocal(out=rs[:], in_=rs[:]), [rs], [rs])
                    for g in range(4):
                        self.V(lambda e, g=g: e.tensor_scalar(out=vg[:, g * 128:(g + 1) * 128], in0=vg[:, g * 128:(g + 1) * 128],
                                                              scalar1=mv[:, g, 0:1], scalar2=rs[:, g:g + 1], op0=ALU.subtract, op1=ALU.mult),
                               [vg, mv, rs], [vg])
                    self.V(lambda e: e.tensor_tensor(out=vg[:], in0=vg[:], in1=lng[:], op=ALU.mult), [vg, lng], [vg])
                    self.V(lambda e: e.tensor_tensor(out=vn[:], in0=vg[:], in1=lnb[:], op=ALU.add), [vg, lnb], [vn])
                    psm = self.rot()
                    for g in range(4):
                        self.P(lambda e, g=g: e.matmul(psm[:, g * 128:(g + 1) * 128], lhsT=vn[:, g * 128:(g + 1) * 128], rhs=wm[:, g, :],
                                                       start=True, stop=True), [vn, wm], [psm])
                    self.V(lambda e: e.tensor_tensor(out=sg1[:], in0=psm[:, :].rearrange("p (g t) -> p g t", g=4), in1=bb[:], op=ALU.add), [psm, bb], [sg1])
                    self.V(lambda e: e.tensor_tensor(out=ob[:, :, jt * 128:(jt + 1) * 128], in0=sg1[:], in1=ug[:, :, jt * 128:(jt + 1) * 128], op=ALU.mult),
                           [sg1, ug], [ob])
                    pst = self.rot()
                    self.mm_acc(pst[:, 0:256], pst, [(xl(kk), wvt[:, kk, 0:256]) for kk in range(8)], [wvt, x])
                    self.mm_acc(pst[:, 256:280], pst, [(xl(kk), wvt[:, kk, 256:280]) for kk in range(8)], [wvt, x])
                    self.A(lambda e: e.activation(out=vs_st[:, jt, :, 0:64], in_=pst[:, 0:128].rearrange("p (g d) -> p g d", g=2), func=AF.Copy), [pst], [vs_st])
                    self.A(lambda e: e.activation(out=vw_st[:, jt, :, 0:64], in_=pst[:, 128:256].rearrange("p (g d) -> p g d", g=2), func=AF.Copy), [pst], [vw_st])
                    self.A(lambda e: e.activation(out=gt_st[:, jt, :], in_=pst[:, 256:280], func=AF.Sigmoid), [pst], [gt_st])
                self.st(d["mixT"].t[4:8, :, tsl].rearrange("c p t -> p c t"), ob[:], [ob], [d["mixT"]], disjoint=True)
                self.st(d["vs"].t[tsl, :, :].rearrange("(j p) g d -> p j g d", p=128), vs_st[:], [vs_st], [d["vs"]], disjoint=True)
                self.st(d["vw"].t[tsl, :, :].rearrange("(j p) g d -> p j g d", p=128), vw_st[:], [vw_st], [d["vw"]], disjoint=True)
                self.st(d["gates"].t[tsl, :].rearrange("(j p) n -> p j n", p=128), gt_st[:], [gt_st], [d["gates"]], disjoint=True)


IN_SHAPES = {
    "x": ([S, D], F32), "mem": ([256, D], F32), "pos": ([1, S], I32),
    "w_in_even": ([1, D, 2328], F32), "cmp_posT": ([1, 2, 128, 32], F32), "nsa_cmp_w1": ([1, 2, 2048, 256], F32),
    "nsa_cmp_w2": ([1, 2, 256, 64], F32), "sgu_ln_g": ([1, 4, 128], F32), "sgu_ln_b": ([1, 4, 128], F32),
    "sgu_wT": ([1, 4, 128, 128], F32), "sgu_b": ([1, 4, 128], F32), "w_out_even": ([1, D, D], F32),
    "w_in_odd": ([1, D, 3584], F32), "hgrn_lbT": ([128, 2, 4], F32), "hgrn_norm_g": ([1, 4, 128], F32),
    "conv_wT": ([1, 128, 4, 3], F32), "conv_bT": ([1, 128, 4], F32), "w_out_odd": ([1, D, D], F32),
    "xattn_w_q": ([2, D, D], F32), "xattn_w_kv": ([2, D, 2 * D], F32), "xattn_w_o": ([2, D, D], F32),
    "ln_g": ([2, 3, D], F32), "ln_b": ([2, 3, D], F32), "router_w": ([2, D, 32], F32), "router_b": ([2, 32], F32),
    "expert_w_gu": ([2, 32, D, 2 * D], F32), "bguT": ([2, 128, 32, 16], F32), "expert_w_dn": ([2, 32, D, D], F32),
    "expert_b_dn": ([2, 32, D], F32),
    "c_ident_f": ([128, 128], F32), "c_ident_b": ([128, 128], BF16), "c_rotm": ([128, 128], BF16), "c_invf": ([128, 1], F32),
    "c_tri": ([128, 128], F32), "c_band": ([128, 8, 512], BF16), "c_cmpb": ([128, 2, S], BF16), "c_mcmp": ([128, 2, 64], BF16),
    "c_E": ([64, S], BF16), "c_cmul": ([128, 32, 64], F32), "c_cadd": ([128, 32, 64], F32),
    "c_tri64": ([64, 64], F32), "c_lts": ([128, 128], F32), "c_ebase": ([128, 32], F32), "c_dumpoff": ([128, 32], F32),
}


class LazyInputs(dict):
    def __init__(self, b):
        super().__init__()
        self.b = b

    def __missing__(self, name):
        shape, dt = IN_SHAPES[name]
        ap = self.b.nc.dram_tensor(name, list(shape), dt, kind="ExternalInput").ap()
        self[name] = ap
        return ap


def host_consts():
    bf = ml_dtypes.bfloat16
    c = {}
    c["c_ident_f"] = np.eye(128, dtype=np.float32)
    c["c_ident_b"] = np.eye(128, dtype=np.float32).astype(bf)
    rot = np.zeros((128, 128), np.float32)
    for m in range(128):
        if m % 64 < 32:
            rot[m + 32, m] = -1.0
        else:
            rot[m - 32, m] = 1.0
    c["c_rotm"] = rot.astype(bf)
    inv = 1.0 / (10000.0 ** (np.arange(0, 64, 2, dtype=np.float32) / 64.0))
    c["c_invf"] = np.tile(inv.astype(np.float32), 4).reshape(128, 1)
    s_ = np.arange(128)[:, None]
    t_ = np.arange(128)[None, :]
    c["c_tri"] = (s_ <= t_).astype(np.float32)
    c["c_lts"] = (s_ < t_).astype(np.float32)
    c["c_ebase"] = np.tile((np.arange(32, dtype=np.float32) * CAP)[None, :], (128, 1))
    c["c_dumpoff"] = np.tile(((31 - np.arange(32, dtype=np.float32)) * CAP)[None, :], (128, 1))
    c["c_tri64"] = (np.arange(64)[:, None] <= np.arange(64)[None, :]).astype(np.float32)
    kl = np.arange(128)[:, None, None]
    o = np.arange(8)[None, :, None]
    ql = np.arange(512)[None, None, :]
    dist = ql - kl + (4 - o) * 128
    c["c_band"] = np.where((dist >= 0) & (dist < 512), 0.0, NEGB).astype(bf)
    n = (np.arange(2)[None, :, None] * 128 + np.arange(128)[:, None, None])
    t = np.arange(S)[None, None, :]
    c["c_cmpb"] = np.where((16 * n + 31 <= t) & (n < 255), 0.0, NEGB).astype(bf)
    cs = np.arange(255)[:, None] * 16
    ss = np.arange(64)[None, :] * 64
    ov = np.clip(np.minimum(cs + 32, ss + 64) - np.maximum(cs, ss), 0, None) / 32.0
    m = np.zeros((256, 64), np.float32)
    m[:255] = ov
    c["c_mcmp"] = m.reshape(2, 128, 64).transpose(1, 0, 2).astype(bf)
    c["c_E"] = (np.arange(64)[:, None] == (np.arange(S)[None, :] // 64)).astype(np.float32).astype(bf)
    tt = np.arange(32)[None, :, None] * 128 + np.arange(128)[:, None, None]
    cur = tt // 64
    jj = np.arange(64)[None, None, :]
    f0 = (jj == 0)
    f1 = (jj == cur)
    f2 = (jj == cur - 1)
    forced = f0 | f1 | f2
    valid = jj <= cur
    cadd = np.where(forced, 1e4 + 2.0 * f0 + 1.0 * (f1 & ~f0) , np.where(valid, 0.0, -1.0 - 0.001 * jj))
    c["c_cmul"] = (valid & ~forced).astype(np.float32)
    c["c_cadd"] = cadd.astype(np.float32)
    return c


def host_layout(inputs, b):
    f = lambda a: np.ascontiguousarray(a)
    m = {}
    m["x"] = f(inputs["x"][b])
    m["mem"] = f(inputs["mem"][b])
    m["pos"] = f(inputs["positions"][b:b + 1]).astype(np.int32)
    for k in ("w_in_even", "nsa_cmp_w1", "nsa_cmp_w2", "sgu_ln_g", "sgu_ln_b", "sgu_b", "w_out_even", "w_in_odd",
              "hgrn_norm_g", "w_out_odd", "xattn_w_q", "xattn_w_kv", "xattn_w_o", "ln_g", "ln_b", "router_w", "router_b",
              "expert_w_gu", "expert_w_dn", "expert_b_dn"):
        m[k] = inputs[k]
    cp = np.transpose(inputs["nsa_cmp_pos"], (0, 1, 3, 2))
    m["cmp_posT"] = f(np.concatenate([cp, cp], axis=2))
    m["sgu_wT"] = f(np.transpose(inputs["sgu_w"], (0, 1, 3, 2)))
    m["hgrn_lbT"] = f(inputs["hgrn_lb_logits"].reshape(2, 4, 128).transpose(2, 0, 1))
    m["conv_wT"] = f(inputs["conv_w"].reshape(1, 3, 4, 128).transpose(0, 3, 2, 1))
    m["conv_bT"] = f(inputs["conv_b"].reshape(1, 4, 128).transpose(0, 2, 1))
    m["bguT"] = f(inputs["expert_b_gu"].reshape(2, 32, 16, 128).transpose(0, 3, 1, 2))
    return m


def phase_compress(self, j, d):
    inp = self.inp
    with self.phase() as es:
        tT = self.sb(es, "cp_t", [128, S], BF16)
        w1 = self.sb(es, "cp_w1", [128, 32, 256], BF16)
        w2 = self.sb(es, "cp_w2", [128, 2, 64], BF16)
        posf = self.sb(es, "cp_posf", [128, 32], F32)
        posb = self.sb(es, "cp_posb", [128, 32], BF16)
        hT = self.sb(es, "cp_hT", [128, 2, 256], BF16)
        cb = self.sb(es, "cp_cb", [128, 1], F32)
        kc_sb = self.sb(es, "cp_kc", [128, 2, 256], BF16)
        vcm = self.sb(es, "cp_vcm", [128, 2, 2, 129], BF16)
        self.V(lambda e: e.memset(hT[:], 0.0), [], [hT])
        self.V(lambda e: e.memset(vcm[:], 1.0), [], [vcm])
        for kt in range(2):
            for g in range(2):
                self.ld(vcm[:, kt, g, 65:129], inp["c_mcmp"][:, kt, :], [], [vcm], disjoint=True)
        tv = tT[:].rearrange("p (n r) -> p n r", r=16)
        for which, src in ((0, d["kc"]), (1, d["vc"])):
            self.ld(tT[:], src.t, [src], [tT])
            for half in range(2):
                self.ldw(w1[half * 64:(half + 1) * 64, :, :], inp["nsa_cmp_w1"][j, which].rearrange("(l d) h -> d l h", d=64), [w1], disjoint=True)
            self.ldw(w2[:], inp["nsa_cmp_w2"][j, which].rearrange("(c p) d -> p c d", p=128), [w2])
            self.ld(posf[:], inp["cmp_posT"][j, which], [], [posf])
            self.V(lambda e: e.tensor_copy(out=posb[:], in_=posf[:]), [posf], [posb])
            for g in range(2):
                gp = slice(g * 64, (g + 1) * 64)
                for hc in range(2):
                    ps = self.rot()
                    self.mm_acc(ps[:, 0:255], ps, [(w1[gp, l, hc * 128:(hc + 1) * 128], tv[gp, (l // 16):(l // 16) + 255, l % 16]) for l in range(32)], [w1, tT])
                    ps2 = self.rot()
                    self.mm_acc(ps2[:, 0:1], ps2, [(w1[gp, l, hc * 128:(hc + 1) * 128], posb[gp, l:l + 1]) for l in range(32)], [w1, posb])
                    self.A(lambda e: e.activation(out=cb[:], in_=ps2[:, 0:1], func=AF.Copy), [ps2], [cb])
                    self.A(lambda e: e.activation(out=hT[:, hc, 0:255], in_=ps[:, 0:255], func=AF.Gelu_apprx_tanh, bias=cb[:, 0:1], scale=1.0), [ps, cb], [hT])
                if which == 0:
                    for half in range(2):
                        pso = self.rot()
                        hs = slice(half * 64, (half + 1) * 64)
                        self.mm_acc(pso[hs, 0:256], pso, [(w2[:, hc, :], hT[:, hc, :]) for hc in range(2)], [w2, hT])
                        self.A(lambda e: e.activation(out=kc_sb[hs, g, :], in_=pso[hs, 0:256], func=AF.Copy), [pso], [kc_sb])
                else:
                    for kt in range(2):
                        pso = self.rot()
                        self.mm_acc(pso[:, 0:64], pso, [(hT[:, hc, kt * 128:(kt + 1) * 128], w2[:, hc, :]) for hc in range(2)], [w2, hT])
                        self.A(lambda e: e.activation(out=vcm[:, kt, g, 0:64], in_=pso[:, 0:64], func=AF.Copy), [pso], [vcm])
        self.st(d["kcd"].t, kc_sb[:], [kc_sb], [d["kcd"]])
        self.st(d["vcm"].t, vcm[:], [vcm], [d["vcm"]])


def phase_attn(self, j, d):
    inp = self.inp
    SC, ACC, MISC = [0, 1], [2, 3, 4, 5], [6, 7]
    with self.phase() as es:
        kc_dup = self.sb(es, "at_kc", [128, 2, 256], BF16)
        vcm = self.sb(es, "at_vcm", [128, 2, 2, 129], BF16)
        kcomb = self.sb(es, "at_kcomb", [128, 2, 2, S], BF16)
        kw_dup = self.sb(es, "at_kw", [128, 2, S], BF16)
        vs_sb = self.sb(es, "at_vs", [128, 32, 2, 65], BF16)
        vw_sb = self.sb(es, "at_vw", [128, 32, 2, 65], BF16)
        cmpb = self.sb(es, "at_cmpb", [128, 2, S], BF16)
        band = self.sb(es, "at_band", [128, 8, 512], BF16)
        cmul = self.sb(es, "at_cmul", [128, 32, 64], F32)
        cadd = self.sb(es, "at_cadd", [128, 32, 64], F32)
        gts = self.sb(es, "at_gates", [128, 32, 24], F32)
        qsel = [self.sb(es, "at_qsel%d" % i, [128, 512], BF16) for i in range(4)]
        self.ld(kc_dup[:], d["kcd"].t, [d["kcd"]], [kc_dup])
        self.ld(vcm[:], d["vcm"].t, [d["vcm"]], [vcm])
        for g in range(2):
            for half in range(2):
                hs = slice(half * 64, (half + 1) * 64)
                os_ = slice((1 - half) * 64, (2 - half) * 64)
                self.ld(kcomb[hs, half, g, :], d["ks"].t[g * 64:(g + 1) * 64, :], [d["ks"]], [kcomb], disjoint=True)
                self.ld(kcomb[os_, half, g, :], inp["c_E"], [], [kcomb], disjoint=True)
                self.ld(kw_dup[hs, g, :], d["kw"].t[g * 64:(g + 1) * 64, :], [d["kw"]], [kw_dup], disjoint=True)
        self.ld(vs_sb[:], d["vs"].t.rearrange("(k p) g d -> p k g d", p=128), [d["vs"]], [vs_sb])
        self.ld(vw_sb[:], d["vw"].t.rearrange("(k p) g d -> p k g d", p=128), [d["vw"]], [vw_sb])
        self.ld(cmpb[:], inp["c_cmpb"], [], [cmpb])
        self.ld(band[:], inp["c_band"], [], [band])
        self.ld(cmul[:], inp["c_cmul"], [], [cmul])
        self.ld(cadd[:], inp["c_cadd"], [], [cadd])
        self.ld(gts[:], d["gates"].t.rearrange("(t p) n -> p t n", p=128), [d["gates"]], [gts])
        qs = [self.sb(es, "at_q%d" % i, [128, 4, 512], BF16) for i in range(2)]
        qrs = [self.sb(es, "at_qr%d" % i, [128, 4, 512], BF16) for i in range(2)]
        o_acc = self.sb(es, "at_oacc", [128, 4, 512], F32)
        imp = self.sb(es, "at_imp", [128, 4, 2, 64], F32)
        pts = [self.sb(es, "at_pt%d" % i, [128, 512], BF16) for i in range(4)]
        pti = [0]
        rz = self.sb(es, "at_rz", [128, 1], F32)
        gz = self.sb(es, "at_gz", [128, 1], F32)
        sc1 = self.sb(es, "at_sc1", [128, 64], F32)
        sc2 = self.sb(es, "at_sc2", [128, 64], F32)
        m1 = self.sb(es, "at_m1", [128, 8], F32)
        m2 = self.sb(es, "at_m2", [128, 8], F32)
        sbias = self.sb(es, "at_sbias", [128, 128], F32)
        oT = self.sb(es, "at_oT", [128, 4, 512], BF16)
        acc = [self.psb[i] for i in ACC]

        def next_pt():
            p = pts[pti[0] % len(pts)]
            pti[0] += 1
            return p

        def finish(jt, qt, h, br, first):
            a = acc[jt]
            self.V(lambda e: e.tensor_scalar(out=rz[:], in0=a[:, 64:65], scalar1=1e-30, scalar2=None, op0=ALU.max), [a], [rz])
            self.V(lambda e: e.reciprocal(out=rz[:], in_=rz[:]), [rz], [rz])
            self.V(lambda e: e.tensor_tensor(out=gz[:], in0=rz[:], in1=gts[:, qt, h * 3 + br:h * 3 + br + 1], op=ALU.mult), [rz, gts], [gz])
            osl = o_acc[:, jt, h * 64:(h + 1) * 64]
            if first:
                self.V(lambda e: e.tensor_scalar(out=osl, in0=a[:, 0:64], scalar1=gz[:, 0:1], scalar2=None, op0=ALU.mult), [a, gz], [o_acc])
            else:
                self.V(lambda e: e.scalar_tensor_tensor(out=osl, in0=a[:, 0:64], scalar=gz[:, 0:1], in1=osl, op0=ALU.mult, op1=ALU.add), [a, gz, o_acc], [o_acc])

        for Q in range(NG):
            qsb, qrsb = qs[Q % 2], qrs[Q % 2]
            tsl = slice(Q * 512, (Q + 1) * 512)
            self.ld(qsb[:], d["q"].t[:, :, tsl].rearrange("c p t -> p c t"), [d["q"]], [qsb])
            self.ld(qrsb[:], d["qr"].t[:, :, tsl].rearrange("c p t -> p c t"), [d["qr"]], [qrsb])
            for g in range(2):
                nkt = 2 if Q >= 4 else 1
                for h in range(4 * g, 4 * g + 4):
                    c, hp = h // 2, slice((h % 2) * 64, (h % 2) * 64 + 64)
                    ptc = []
                    for kt in range(nkt):
                        sc = self.rot(SC)
                        self.P(lambda e: e.matmul(sc[:, :], lhsT=kc_dup[hp, g, kt * 128:(kt + 1) * 128], rhs=qsb[hp, c, :], start=True, stop=False), [kc_dup, qsb], [sc])
                        self.P(lambda e: e.matmul(sc[:, :], lhsT=self.ident_b[:], rhs=cmpb[:, kt, tsl], start=False, stop=True), [self.ident_b, cmpb], [sc])
                        pt = next_pt()
                        self.A(lambda e: e.activation(out=pt[:], in_=sc[:, :], func=AF.Exp, scale=0.125), [sc], [pt])
                        ptc.append(pt)
                    for jt in range(4):
                        a = acc[jt]
                        for kt in range(nkt):
                            self.P(lambda e: e.matmul(a[:, 0:129], lhsT=ptc[kt][:, jt * 128:(jt + 1) * 128], rhs=vcm[:, kt, g, :], start=(kt == 0), stop=(kt == nkt - 1)), [ptc[kt], vcm], [a])
                        finish(jt, 4 * Q + jt, h, 0, True)
                        isl = imp[:, jt, g, :]
                        if h % 4 == 0:
                            self.V(lambda e: e.tensor_scalar(out=isl, in0=a[:, 65:129], scalar1=rz[:, 0:1], scalar2=None, op0=ALU.mult), [a, rz], [imp])
                        else:
                            self.V(lambda e: e.scalar_tensor_tensor(out=isl, in0=a[:, 65:129], scalar=rz[:, 0:1], in1=isl, op0=ALU.mult, op1=ALU.add), [a, rz, imp], [imp])
                tp = self.rot(MISC)
                for jt in range(4):
                    qt = 4 * Q + jt
                    self.V(lambda e: e.tensor_tensor(out=sc1[:], in0=imp[:, jt, g, :], in1=cmul[:, qt, :], op=ALU.mult), [imp, cmul], [sc1])
                    self.V(lambda e: e.tensor_tensor(out=sc1[:], in0=sc1[:], in1=cadd[:, qt, :], op=ALU.add), [sc1, cadd], [sc1])
                    self.V(lambda e: e.max(out=m1[:], in_=sc1[:]), [sc1], [m1])
                    self.V(lambda e: e.match_replace(out=sc2[:], in_to_replace=m1[:], in_values=sc1[:], imm_value=-1e30), [sc1, m1], [sc2])
                    self.V(lambda e: e.max(out=m2[:], in_=sc2[:]), [sc2], [m2])
                    for hf in range(2):
                        self.V(lambda e, hf=hf: e.tensor_scalar(out=sbias[:, hf * 64:(hf + 1) * 64], in0=sc1[:], scalar1=m2[:, 7:8], scalar2=NEGB, op0=ALU.is_lt, op1=ALU.mult), [sc1, m2], [sbias])
                    self.P(lambda e: e.transpose(out=tp[:, jt * 128:(jt + 1) * 128], in_=sbias[:], identity=self.ident_f[:]), [sbias, self.ident_f], [tp])
                for hh in range(4):
                    v_ = hh % 2
                    c_ = (4 * g + hh) // 2
                    hp_ = slice(v_ * 64, v_ * 64 + 64)
                    ot_ = slice((1 - v_) * 64, (2 - v_) * 64)
                    self.A(lambda e, hh=hh, ot_=ot_: e.activation(out=qsel[hh][ot_, :], in_=tp[ot_, :], func=AF.Copy), [tp], [qsel[hh]])
                    self.V(lambda e, hh=hh, hp_=hp_, c_=c_: e.tensor_copy(out=qsel[hh][hp_, :], in_=qrsb[hp_, c_, :]), [qrsb, qsel[hh]], [qsel[hh]])
                for h in range(4 * g, 4 * g + 4):
                    c, hp = h // 2, slice((h % 2) * 64, (h % 2) * 64 + 64)
                    for br in (1, 2):
                        vsb = vs_sb if br == 1 else vw_sb
                        kt_lo = 0 if br == 1 else max(0, 4 * Q - 4)
                        def qk_exp(kt):
                            sc = self.rot(SC)
                            need_band = (br == 2) or (kt >= 4 * Q)
                            if br == 1:
                                qs_ = qsel[h % 4]
                                self.P(lambda e: e.matmul(sc[:, :], lhsT=kcomb[:, h % 2, g, kt * 128:(kt + 1) * 128], rhs=qs_[:, :], start=True, stop=not need_band), [kcomb, qs_], [sc])
                            else:
                                self.P(lambda e: e.matmul(sc[:, :], lhsT=kw_dup[hp, g, kt * 128:(kt + 1) * 128], rhs=qrsb[hp, c, :], start=True, stop=not need_band), [kw_dup, qrsb], [sc])
                            if need_band:
                                o_ = kt - 4 * Q + 4
                                jb = o_ % 4
                                self.P(lambda e: e.matmul(sc[:, jb * 128:(jb + 1) * 128], lhsT=self.ident_b[:], rhs=band[:, o_, jb * 128:(jb + 1) * 128], start=False, stop=True), [self.ident_b, band], [sc])
                            pt = next_pt()
                            self.A(lambda e: e.activation(out=pt[:], in_=sc[:, :], func=AF.Exp, scale=0.125), [sc], [pt])
                            return pt

                        def pv(kt, pt):
                            for jt in range(4):
                                qt = 4 * Q + jt
                                lo = 0 if br == 1 else max(0, qt - 4)
                                if kt < lo or kt > qt:
                                    continue
                                a = acc[jt]
                                self.P(lambda e: e.matmul(a[:, 0:65], lhsT=pt[:, jt * 128:(jt + 1) * 128], rhs=vsb[:, kt, g, :], start=(kt == lo), stop=(kt == qt)), [pt, vsb], [a])
                                if kt == qt:
                                    finish(jt, qt, h, br, False)
                        prev = None
                        for kt in range(kt_lo, 4 * Q + 4):
                            pt = qk_exp(kt)
                            if prev is not None:
                                pv(*prev)
                            prev = (kt, pt)
                        pv(*prev)
            for c in range(4):
                tp = self.rot(MISC)
                for jt in range(4):
                    self.P(lambda e: e.transpose(out=tp[:, jt * 128:(jt + 1) * 128], in_=o_acc[:, jt, c * 128:(c + 1) * 128], identity=self.ident_f[:]), [o_acc, self.ident_f], [tp])
                self.A(lambda e: e.activation(out=oT[:, c, :], in_=tp[:, :], func=AF.Copy), [tp], [oT])
            self.st(d["mixT"].t[0:4, :, tsl].rearrange("c p t -> p c t"), oT[:], [oT], [d["mixT"]], disjoint=True)


Builder.phase_compress = phase_compress
Builder.phase_attn = phase_attn


def phase_xattn(self, li, mem_ap, xT, xres_src, xres_dst, xT_dst, md):
    inp = self.inp
    d_gk, d_dest, d_xg = md["gk"], md["dest"], md["xg"]
    SC, ACC, MISC = [0, 1], [2, 3], [4, 5, 6, 7]
    with self.phase() as es:
        wq = self.sb(es, "xa_wq", [128, 8, D], BF16)
        wo = self.sb(es, "xa_wo", [128, 8, D], BF16)
        kT = self.sb(es, "xa_kT", [128, 8, 256], BF16)
        v_sb = self.sb(es, "xa_v", [128, 2, 4, 257], BF16)
        for kk in range(8):
            self.ldw(wq[:, kk, :], inp["xattn_w_q"][li, kk * 128:(kk + 1) * 128, :], [wq], disjoint=True)
            self.ldw(wo[:, kk, :], inp["xattn_w_o"][li, kk * 128:(kk + 1) * 128, :], [wo], disjoint=True)
        self.V(lambda e: e.memset(v_sb[:], 1.0), [], [v_sb])
        with self.phase() as es2:
            memT = self.sb(es2, "xa_memT", [128, 8, 256], BF16)
            mt = self.sb(es2, "xa_mt", [128, D], F32)
            wkv = self.sb(es2, "xa_wkv", [128, 8, D], BF16)
            for t in range(2):
                self.ld(mt[:], mem_ap[t * 128:(t + 1) * 128, :], [], [mt])
                for half in range(2):
                    tp = self.rot(MISC)
                    for jj in range(4):
                        ch = half * 4 + jj
                        self.P(lambda e: e.transpose(out=tp[:, jj * 128:(jj + 1) * 128], in_=mt[:, ch * 128:(ch + 1) * 128], identity=self.ident_f[:]), [mt, self.ident_f], [tp])
                    self.A(lambda e: e.activation(out=memT[:, half * 4:(half + 1) * 4, t * 128:(t + 1) * 128],
                                                  in_=tp[:, :].rearrange("p (a b) -> p a b", a=4), func=AF.Copy), [tp], [memT])
            for part in range(2):
                for kk in range(8):
                    self.ldw(wkv[:, kk, :], inp["xattn_w_kv"][li, kk * 128:(kk + 1) * 128, part * D:(part + 1) * D], [wkv], disjoint=(kk > 0))
                if part == 0:
                    for c in range(8):
                        ps = self.rot(MISC)
                        self.mm_acc(ps[:, 0:256], ps, [(wkv[:, kk, c * 128:(c + 1) * 128], memT[:, kk, :]) for kk in range(8)], [wkv, memT])
                        self.A(lambda e: e.activation(out=kT[:, c, :], in_=ps[:, 0:256], func=AF.Copy), [ps], [kT])
                else:
                    for kt in range(2):
                        for half in range(2):
                            ps = self.rot(MISC)
                            self.mm_acc(ps[:, :], ps, [(memT[:, kk, kt * 128:(kt + 1) * 128], wkv[:, kk, half * 512:(half + 1) * 512]) for kk in range(8)], [wkv, memT])
                            self.A(lambda e: e.activation(out=v_sb[:, kt, 2 * half:2 * half + 2, 0:256], in_=ps[:, :].rearrange("p (h d) -> p h d", h=2), func=AF.Copy), [ps], [v_sb])
        L = self.ln_setup(es, li, 1, "xa")
        L["stq"] = "pool"
        wr = self.sb(es, "xa_wr", [128, 8, 32], F32)
        self.ld(wr[:], inp["router_w"][li].rearrange("(c p) e -> p c e", p=128), [], [wr])
        br = self.sb(es, "xa_br", [128, 32], F32)
        self.ld(br[:], inp["router_b"][li:li + 1, :].broadcast_to([128, 32]), [], [br])
        lts = self.sb(es, "xa_lts", [128, 128], F32)
        self.ld(lts[:], inp["c_lts"], [], [lts])
        ones = self.sb(es, "xa_ones", [128, 128], F32)
        self.V(lambda e: e.memset(ones[:], 1.0), [], [ones])
        ebase = self.sb(es, "xa_ebase", [128, 32], F32)
        self.ld(ebase[:], inp["c_ebase"], [], [ebase])
        dumpoff = self.sb(es, "xa_dumpoff", [128, 32], F32)
        self.ld(dumpoff[:], inp["c_dumpoff"], [], [dumpoff])
        cnt = self.sb(es, "xa_cnt", [128, 32], F32)
        self.V(lambda e: e.memset(cnt[:], 0.0), [], [cnt])
        xTf = self.sb(es, "xa_xTf", [128, 8, 128], F32)
        lg = self.sb(es, "xa_lg", [128, 32], F32)
        m8 = self.sb(es, "xa_m8", [128, 8], F32)
        nm = self.sb(es, "xa_nm", [128, 1], F32)
        e4 = self.sb(es, "xa_e4", [128, 4], F32)
        sm = self.sb(es, "xa_sm", [128, 1], F32)
        gk = self.sb(es, "xa_gk", [128, 4], F32)
        mk = self.sb(es, "xa_mk", [128, 32], F32)
        rowid = self.sb(es, "xa_rowid", [128, 32], F32)
        ov = self.sb(es, "xa_ov", [128, 32], F32)
        tmp32 = self.sb(es, "xa_tmp32", [128, 32], F32)
        destf = self.sb(es, "xa_destf", [128, 4], F32)
        destu = [self.sb(es, "xa_destu%d" % i, [128, 4], U32) for i in range(2)]

        def router_hook(ti, pts, xn):
            du = destu[ti % 2]
            for half in range(2):
                self.A(lambda e: e.activation(out=xTf[:, half * 4:(half + 1) * 4, :], in_=pts[half][:, :].rearrange("p (a b) -> p a b", a=4), func=AF.Copy), [pts[half]], [xTf])
            ps = self.rot(MISC)
            self.mm_acc(ps[:, 0:32], ps, [(xTf[:, c, :], wr[:, c, :]) for c in range(8)], [xTf, wr])
            self.V(lambda e: e.tensor_tensor(out=lg[:], in0=ps[:, 0:32], in1=br[:], op=ALU.add), [ps, br], [lg])
            self.V(lambda e: e.max(out=m8[:], in_=lg[:]), [lg], [m8])
            self.V(lambda e: e.tensor_scalar(out=nm[:], in0=m8[:, 0:1], scalar1=-1.0, scalar2=None, op0=ALU.mult), [m8], [nm])
            self.A(lambda e: e.activation(out=e4[:], in_=m8[:, 0:4], func=AF.Exp, bias=nm[:, 0:1], scale=1.0), [m8, nm], [e4])
            self.V(lambda e: e.reduce_sum(out=sm[:], in_=e4[:], axis=mybir.AxisListType.X), [e4], [sm])
            self.V(lambda e: e.reciprocal(out=sm[:], in_=sm[:]), [sm], [sm])
            self.V(lambda e: e.tensor_scalar(out=gk[:], in0=e4[:], scalar1=sm[:, 0:1], scalar2=None, op0=ALU.mult), [e4, sm], [gk])
            self.c.dma("pool", d_gk.t[ti * 128:(ti + 1) * 128, :], gk[:], [gk], [d_gk], disjoint=True)
            self.V(lambda e: e.tensor_scalar(out=mk[:], in0=lg[:], scalar1=m8[:, 3:4], scalar2=None, op0=ALU.is_ge), [lg, m8], [mk])
            pw = self.rot(MISC)
            self.P(lambda e: e.matmul(pw[:, 0:32], lhsT=lts[:], rhs=mk[:], start=True, stop=True), [lts, mk], [pw])
            self.P(lambda e: e.matmul(pw[:, 32:64], lhsT=ones[:], rhs=mk[:], start=True, stop=True), [ones, mk], [pw])
            self.V(lambda e: e.tensor_tensor(out=rowid[:], in0=pw[:, 0:32], in1=cnt[:], op=ALU.add), [pw, cnt], [rowid])
            self.V(lambda e: e.tensor_tensor(out=cnt[:], in0=pw[:, 32:64], in1=cnt[:], op=ALU.add), [pw, cnt], [cnt])
            self.V(lambda e: e.scalar_tensor_tensor(out=ov[:], in0=rowid[:], scalar=float(CAP), in1=dumpoff[:], op0=ALU.is_ge, op1=ALU.mult), [rowid, dumpoff], [ov])
            self.V(lambda e: e.tensor_tensor(out=rowid[:], in0=rowid[:], in1=ebase[:], op=ALU.add), [rowid, ebase], [rowid])
            self.V(lambda e: e.tensor_tensor(out=rowid[:], in0=rowid[:], in1=ov[:], op=ALU.add), [rowid, ov], [rowid])
            for k in range(4):
                self.V(lambda e: e.scalar_tensor_tensor(out=tmp32[:], in0=lg[:], scalar=m8[:, k:k + 1], in1=rowid[:], op0=ALU.is_equal, op1=ALU.mult), [lg, m8, rowid], [tmp32])
                self.V(lambda e: e.reduce_sum(out=destf[:, k:k + 1], in_=tmp32[:], axis=mybir.AxisListType.X), [tmp32], [destf])
            self.V(lambda e: e.tensor_copy(out=du[:], in_=destf[:]), [destf], [du])
            self.c.dma("pool", d_dest.t[ti * 128:(ti + 1) * 128, :], du[:], [du], [d_dest], disjoint=True)
            for k in range(4):
                self.c.idma(d_xg.t, bass.IndirectOffsetOnAxis(du[:, k:k + 1], 0), xn[:], None, [xn, du], [d_xg], disjoint=True)

        xs = [self.sb(es, "xa_x%d" % i, [128, 8, 512], BF16) for i in range(2)]
        qT = self.sb(es, "xa_qT", [128, 8, 512], BF16)
        o_sb = self.sb(es, "xa_o", [128, 4, D], F32)
        pts_ = [self.sb(es, "xa_pt%d" % i, [128, 512], BF16) for i in range(4)]
        pti = [0]
        rz = self.sb(es, "xa_rz", [128, 1], F32)
        oT = [self.sb(es, "xa_oT%d" % i, [128, 8, 128], BF16) for i in range(2)]
        pend = [None]

        def flush():
            if pend[0] is not None:
                pend[0]()
                pend[0] = None
        for Q in range(NG):
            x = xs[Q % 2]
            tsl = slice(Q * 512, (Q + 1) * 512)
            if Q == 0:
                self.ld(x[:], xT.t[:, :, tsl].rearrange("c p t -> p c t"), [xT], [x])
            if Q + 1 < NG:
                self.ld(xs[(Q + 1) % 2][:], xT.t[:, :, (Q + 1) * 512:(Q + 2) * 512].rearrange("c p t -> p c t"), [xT], [xs[(Q + 1) % 2]])
            for c in range(8):
                ps = self.rot(MISC)
                self.mm_acc(ps[:, :], ps, [(wq[:, kk, c * 128:(c + 1) * 128], x[:, kk, :]) for kk in range(8)], [wq, x])
                self.A(lambda e: e.activation(out=qT[:, c, :], in_=ps[:, :], func=AF.Copy), [ps], [qT])
                if c == 3:
                    flush()
            def xa_qk(h):
                pk = []
                for kt in range(2):
                    sc = self.rot(SC)
                    self.mm_acc(sc[:, :], sc, [(kT[:, 2 * h + cc, kt * 128:(kt + 1) * 128], qT[:, 2 * h + cc, :]) for cc in range(2)], [kT, qT])
                    pt = pts_[pti[0] % 4]
                    pti[0] += 1
                    self.A(lambda e: e.activation(out=pt[:], in_=sc[:, :], func=AF.Exp, scale=1.0 / 16.0), [sc], [pt])
                    pk.append(pt)
                return pk

            def xa_pv(h, pk):
                for jt in range(4):
                    a = self.rot(ACC)
                    self.mm_acc(a[:, 0:257], a, [(pk[kt][:, jt * 128:(jt + 1) * 128], v_sb[:, kt, h, :]) for kt in range(2)], pk + [v_sb])
                    self.V(lambda e: e.reciprocal(out=rz[:], in_=a[:, 256:257]), [a], [rz])
                    self.V(lambda e: e.tensor_scalar(out=o_sb[:, jt, h * 256:(h + 1) * 256], in0=a[:, 0:256], scalar1=rz[:, 0:1], scalar2=None, op0=ALU.mult), [a, rz], [o_sb])
            prev = None
            for h in range(4):
                pk = xa_qk(h)
                if prev is not None:
                    xa_pv(*prev)
                prev = (h, pk)
            xa_pv(*prev)
            def xa_pre(jt):
                ti = Q * 4 + jt
                ot = oT[ti % 2]
                xr = self.ln_preload(L, ti, xres_src)
                for half in range(2):
                    tp = self.rot(MISC)
                    for jj in range(4):
                        ch = half * 4 + jj
                        self.P(lambda e: e.transpose(out=tp[:, jj * 128:(jj + 1) * 128], in_=o_sb[:, jt, ch * 128:(ch + 1) * 128], identity=self.ident_f[:]), [o_sb, self.ident_f], [tp])
                    self.A(lambda e: e.activation(out=ot[:, half * 4:(half + 1) * 4, :], in_=tp[:, :].rearrange("p (a b) -> p a b", a=4), func=AF.Copy), [tp], [ot])
                ps = [self.psb[0], self.psb[1]] if jt % 2 == 0 else [self.psb[2], self.psb[3]]
                for hh in range(2):
                    self.mm_acc(ps[hh][:, :], ps[hh], [(ot[:, kk, :], wo[:, kk, hh * 512:(hh + 1) * 512]) for kk in range(8)], [ot, wo])
                return ps, xr
            nxt = xa_pre(0)
            for jt in range(4):
                cur = nxt
                if jt + 1 < 4:
                    nxt = xa_pre(jt + 1)
                ps, xr = cur
                pb = self.ln_tile(L, Q * 4 + jt, [ps[0][:, :], ps[1][:, :]], ps, xres_src, xres_dst, xT_dst, hook=router_hook, psum_sub=MISC, xr_pre=xr, defer=True)
                flush()
                pend[0] = pb
        flush()


def phase_moe(self, li, xT, gates_d, xres_src, xres_dst, xT_dst, final_out=None):
    inp = self.inp
    with self.phase() as es:
        L = self.ln_setup(es, li, 2, "mo")
        bgu = self.sb(es, "mo_bgu", [128, 32, 16], F32)
        self.ld(bgu[:], inp["bguT"][li], [], [bgu])
        bdn = self.sb(es, "mo_bdn", [32, D], F32)
        self.ld(bdn[:], inp["expert_b_dn"][li], [], [bdn])
        x = self.sb(es, "mo_x", [128, 8, 1024], BF16)
        gts = self.sb(es, "mo_g", [128, 8, 32], F32)
        gT = self.sb(es, "mo_gT", [32, 128], F32)
        acc = self.sb(es, "mo_acc", [128, 8, D], F32)
        wgu = self.sb(es, "mo_wgu", [128, 8, 2 * D], BF16)
        wdn = self.sb(es, "mo_wdn", [128, 8, D], BF16)
        act = self.sb(es, "mo_act", [128, 8, 512], BF16)
        g_sb = [self.sb(es, "mo_gs%d" % i, [128, 512], F32) for i in range(2)]
        s_sb = [self.sb(es, "mo_ss%d" % i, [128, 512], F32) for i in range(2)]
        u_sb = [self.sb(es, "mo_us%d" % i, [128, 512], F32) for i in range(2)]
        for SG in range(4):
            self.ld(x[:], xT.t[:, :, SG * 1024:(SG + 1) * 1024].rearrange("c p t -> p c t"), [xT], [x])
            self.ld(gts[:], gates_d.t[SG * 1024:(SG + 1) * 1024, :].rearrange("(t p) e -> p t e", p=128), [gates_d], [gts])
            for t in range(8):
                tp = self.rot()
                self.P(lambda e: e.transpose(out=tp[0:32, 0:128], in_=gts[:, t, :], identity=self.ident_f[:]), [gts, self.ident_f], [tp])
                self.A(lambda e: e.activation(out=gT[:], in_=tp[0:32, 0:128], func=AF.Copy), [tp], [gT])
                for half in range(2):
                    ps = self.rot()
                    self.P(lambda e: e.matmul(ps[:, :], lhsT=gT[:], rhs=bdn[:, half * 512:(half + 1) * 512], start=True, stop=True), [gT, bdn], [ps])
                    self.A(lambda e: e.activation(out=acc[:, t, half * 512:(half + 1) * 512], in_=ps[:, :], func=AF.Copy), [ps], [acc])
            for ex in range(32):
                for kk in range(8):
                    self.ldw(wgu[:, kk, :], inp["expert_w_gu"][li, ex, kk * 128:(kk + 1) * 128, :], [wgu], disjoint=(kk > 0))
                for kk in range(8):
                    self.ldw(wdn[:, kk, :], inp["expert_w_dn"][li, ex, kk * 128:(kk + 1) * 128, :], [wdn], disjoint=(kk > 0))
                for tg in range(2):
                    tsl = slice(tg * 512, (tg + 1) * 512)
                    for jp in range(8):
                        k2 = jp % 2
                        gs, ss, us = g_sb[k2], s_sb[k2], u_sb[k2]
                        hg = self.rot()
                        self.mm_acc(hg[:, :], hg, [(wgu[:, kk, jp * 128:(jp + 1) * 128], x[:, kk, tsl]) for kk in range(8)], [wgu, x])
                        hu = self.rot()
                        self.mm_acc(hu[:, :], hu, [(wgu[:, kk, D + jp * 128:D + (jp + 1) * 128], x[:, kk, tsl]) for kk in range(8)], [wgu, x])
                        self.V(lambda e: e.tensor_scalar(out=gs[:], in0=hg[:, :], scalar1=bgu[:, ex, jp:jp + 1], scalar2=7.0, op0=ALU.add, op1=ALU.min), [hg, bgu], [gs])
                        self.A(lambda e: e.activation(out=ss[:], in_=gs[:], func=AF.Sigmoid, scale=1.702), [gs], [ss])
                        self.V(lambda e: e.tensor_scalar(out=us[:], in0=hu[:, :], scalar1=bgu[:, ex, 8 + jp:9 + jp], scalar2=7.0, op0=ALU.add, op1=ALU.min), [hu, bgu], [us])
                        self.V(lambda e: e.tensor_scalar(out=us[:], in0=us[:], scalar1=-7.0, scalar2=1.0, op0=ALU.max, op1=ALU.add), [us], [us])
                        self.V(lambda e: e.tensor_tensor(out=gs[:], in0=gs[:], in1=ss[:], op=ALU.mult), [gs, ss], [gs])
                        self.V(lambda e: e.tensor_tensor(out=act[:, jp, :], in0=us[:], in1=gs[:], op=ALU.mult), [us, gs], [act])
                    for t in range(4):
                        tt = tg * 4 + t
                        for half in range(2):
                            y = self.rot()
                            self.mm_acc(y[:, :], y, [(act[:, jf, t * 128:(t + 1) * 128], wdn[:, jf, half * 512:(half + 1) * 512]) for jf in range(8)], [act, wdn])
                            asl = acc[:, tt, half * 512:(half + 1) * 512]
                            self.V(lambda e: e.scalar_tensor_tensor(out=asl, in0=y[:, :], scalar=gts[:, tt, ex:ex + 1], in1=asl, op0=ALU.mult, op1=ALU.add), [y, gts, acc], [acc])
            for t in range(8):
                self.ln_tile(L, SG * 8 + t, [acc[:, t, 0:512], acc[:, t, 512:1024]], [acc], xres_src, xres_dst, xT_dst, final_out=final_out)


Builder.phase_xattn = phase_xattn
Builder.phase_moe = phase_moe


def phase_moe_sparse(self, li, md, xres_src, xres_dst, xT_dst, final_out=None):
    inp = self.inp
    d_gk, d_dest, d_xg, d_yg = md["gk"], md["dest"], md["xg"], md["yg"]
    NTL = CAP // 128
    GROUPS = [(g0, min(512, CAP - g0)) for g0 in range(0, CAP, 512)]
    with self.phase() as es:
        with self.phase() as es1:
            bgu = self.sb(es1, "ms_bgu", [128, 32, 16], F32)
            self.ld(bgu[:], inp["bguT"][li], [], [bgu])
            wgu = [self.sb(es1, "ms_wgu%d" % i, [128, 8, 2 * D], BF16) for i in range(2)]
            wdn = [self.sb(es1, "ms_wdn%d" % i, [128, 8, D], BF16) for i in range(2)]
            bdn = [self.sb(es1, "ms_bdn%d" % i, [128, D], F32) for i in range(2)]
            xr = [self.sb(es1, "ms_xr%d" % i, [128, D], F32) for i in range(NTL)]

            def prefetch_rows(ex_):
                for t_ in range(NTL):
                    r__ = xr[t_]
                    self.ld(r__[:], d_xg.t[ex_ * CAP + t_ * 128:ex_ * CAP + (t_ + 1) * 128, :], [d_xg], [r__])
            xgT = self.sb(es1, "ms_xgT", [128, 8, CAP], BF16)
            act = self.sb(es1, "ms_act", [128, 8, 512], BF16)
            g_sb = [self.sb(es1, "ms_gs%d" % i, [128, 512], F32) for i in range(2)]
            s_sb = [self.sb(es1, "ms_ss%d" % i, [128, 512], F32) for i in range(2)]
            u_sb = [self.sb(es1, "ms_us%d" % i, [128, 512], F32) for i in range(2)]
            y_sb = [self.sb(es1, "ms_y%d" % i, [128, D], F32) for i in range(2)]
            yi = [0]
            zt = y_sb[0]
            self.V(lambda e: e.memset(zt[:], 0.0), [], [zt])
            for t_ in range(NTL):
                self.st(d_yg.t[32 * CAP + t_ * 128:32 * CAP + (t_ + 1) * 128, :], zt[:], [zt], [d_yg], disjoint=True)
            act2 = [act, self.sb(es1, "ms_act2", [128, 8, 512], BF16)]

            def st_load_w(ex):
                wg, wd, bd = wgu[ex % 2], wdn[ex % 2], bdn[ex % 2]
                for kk in range(8):
                    self.ldw(wg[:, kk, :], inp["expert_w_gu"][li, ex, kk * 128:(kk + 1) * 128, :], [wg], disjoint=(kk > 0))
                for kk in range(8):
                    self.ldw(wd[:, kk, :], inp["expert_w_dn"][li, ex, kk * 128:(kk + 1) * 128, :], [wd], disjoint=(kk > 0))
                self.ld(bd[:], inp["expert_b_dn"][li, ex:ex + 1, :].broadcast_to([128, D]), [], [bd])

            def st_T(ex):
                for t in range(NTL):
                    r_ = xr[t]
                    for half in range(2):
                        tp = self.rot()
                        for jj in range(4):
                            ch = half * 4 + jj
                            self.P(lambda e: e.transpose(out=tp[:, jj * 128:(jj + 1) * 128], in_=r_[:, ch * 128:(ch + 1) * 128], identity=self.ident_f[:]), [r_, self.ident_f], [tp])
                        self.A(lambda e: e.activation(out=xgT[:, half * 4:(half + 1) * 4, t * 128:(t + 1) * 128], in_=tp[:, :].rearrange("p (a b) -> p a b", a=4), func=AF.Copy), [tp], [xgT])
                if ex + 1 < 32:
                    prefetch_rows(ex + 1)

            def st_GU(ex, gi):
                wg = wgu[ex % 2]
                g0, gn = GROUPS[gi]
                a_ = act2[gi % 2]
                tsl = slice(g0, g0 + gn)
                for jp in range(8):
                    k2 = jp % 2
                    gs, ss, us = g_sb[k2], s_sb[k2], u_sb[k2]
                    hg = self.rot()
                    self.mm_acc(hg[:, 0:gn], hg, [(wg[:, kk, jp * 128:(jp + 1) * 128], xgT[:, kk, tsl]) for kk in range(8)], [wg, xgT])
                    hu = self.rot()
                    self.mm_acc(hu[:, 0:gn], hu, [(wg[:, kk, D + jp * 128:D + (jp + 1) * 128], xgT[:, kk, tsl]) for kk in range(8)], [wg, xgT])
                    self.V(lambda e: e.tensor_scalar(out=gs[:, 0:gn], in0=hg[:, 0:gn], scalar1=bgu[:, ex, jp:jp + 1], scalar2=7.0, op0=ALU.add, op1=ALU.min), [hg, bgu], [gs])
                    self.A(lambda e: e.activation(out=ss[:, 0:gn], in_=gs[:, 0:gn], func=AF.Sigmoid, scale=1.702), [gs], [ss])
                    self.V(lambda e: e.tensor_scalar(out=us[:, 0:gn], in0=hu[:, 0:gn], scalar1=bgu[:, ex, 8 + jp:9 + jp], scalar2=7.0, op0=ALU.add, op1=ALU.min), [hu, bgu], [us])
                    self.V(lambda e: e.tensor_scalar(out=us[:, 0:gn], in0=us[:, 0:gn], scalar1=-7.0, scalar2=1.0, op0=ALU.max, op1=ALU.add), [us], [us])
                    self.V(lambda e: e.tensor_tensor(out=gs[:, 0:gn], in0=gs[:, 0:gn], in1=ss[:, 0:gn], op=ALU.mult), [gs, ss], [gs])
                    self.V(lambda e: e.tensor_tensor(out=a_[:, jp, 0:gn], in0=us[:, 0:gn], in1=gs[:, 0:gn], op=ALU.mult), [us, gs], [a_])

            def st_DN(ex, gi):
                wd, bd = wdn[ex % 2], bdn[ex % 2]
                g0, gn = GROUPS[gi]
                a_ = act2[gi % 2]
                for t in range(gn // 128):
                    ys = y_sb[yi[0] % 2]
                    yi[0] += 1
                    for half in range(2):
                        y = self.rot()
                        self.mm_acc(y[:, :], y, [(a_[:, jf, t * 128:(t + 1) * 128], wd[:, jf, half * 512:(half + 1) * 512]) for jf in range(8)], [a_, wd])
                        self.V(lambda e: e.tensor_tensor(out=ys[:, half * 512:(half + 1) * 512], in0=y[:, :], in1=bd[:, half * 512:(half + 1) * 512], op=ALU.add), [y, bd], [ys])
                    r0 = ex * CAP + g0 + t * 128
                    self.st(d_yg.t[r0:r0 + 128, :], ys[:], [ys], [d_yg], disjoint=True)

            NGp = len(GROUPS)
            prefetch_rows(0)
            st_load_w(0)
            st_T(0)
            st_GU(0, 0)
            for ex in range(32):
                if ex + 1 < 32:
                    st_load_w(ex + 1)
                for gi in range(NGp):
                    if gi + 1 < NGp:
                        st_GU(ex, gi + 1)
                    elif ex + 1 < 32:
                        st_T(ex + 1)
                        st_GU(ex + 1, 0)
                    st_DN(ex, gi)
        L = self.ln_setup(es, li, 2, "ms")
        du = [self.sb(es, "ms_du%d" % i, [128, 4], U32) for i in range(2)]
        gk = [self.sb(es, "ms_gk%d" % i, [128, 4], F32) for i in range(2)]
        yk = [[self.sb(es, "ms_yk%d_%d" % (i, k), [128, D], F32) for k in range(4)] for i in range(2)]
        accs = [self.sb(es, "ms_acc%d" % i, [128, D], F32) for i in range(2)]
        def cb_pre(ti):
            b2 = ti % 2
            self.ld(du[b2][:], d_dest.t[ti * 128:(ti + 1) * 128, :], [d_dest], [du[b2]])
            self.ld(gk[b2][:], d_gk.t[ti * 128:(ti + 1) * 128, :], [d_gk], [gk[b2]])
            for k in range(4):
                self.c.idma(yk[b2][k][:], None, d_yg.t, bass.IndirectOffsetOnAxis(du[b2][:, k:k + 1], 0), [d_yg, du[b2]], [yk[b2][k]])
            return self.ln_preload(L, ti, xres_src)
        nxt = cb_pre(0)
        for ti in range(NT):
            b2 = ti % 2
            xr = nxt
            if ti + 1 < NT:
                nxt = cb_pre(ti + 1)
            acc = accs[b2]
            self.V(lambda e: e.tensor_scalar(out=acc[:], in0=yk[b2][0][:], scalar1=gk[b2][:, 0:1], scalar2=None, op0=ALU.mult), [yk[b2][0], gk[b2]], [acc])
            for k in range(1, 4):
                self.V(lambda e: e.scalar_tensor_tensor(out=acc[:], in0=yk[b2][k][:], scalar=gk[b2][:, k:k + 1], in1=acc[:], op0=ALU.mult, op1=ALU.add), [yk[b2][k], gk[b2], acc], [acc])
            self.ln_tile(L, ti, [acc[:, 0:512], acc[:, 512:1024]], [acc], xres_src, xres_dst, xT_dst, final_out=final_out, xr_pre=xr)


Builder.phase_moe_sparse = phase_moe_sparse


def declare_scratch(b):
    d = {}
    for nm in ("q", "qr"):
        d[nm] = b.dscratch("s_" + nm, [4, 128, S], BF16)
    for nm in ("kc", "vc", "ks", "kw"):
        d[nm] = b.dscratch("s_" + nm, [128, S], BF16)
    for nm in ("vs", "vw"):
        d[nm] = b.dscratch("s_" + nm, [S, 2, 65], BF16)
    d["gates"] = b.dscratch("s_gates", [S, 24], F32)
    d["mixT"] = b.dscratch("s_mixT", [8, 128, S], BF16)
    d["kcd"] = b.dscratch("s_kcd", [128, 2, 256], BF16)
    d["vcm"] = b.dscratch("s_vcm", [128, 2, 2, 129], BF16)
    d["R0"] = b.dscratch("s_R0", [S, D], F32)
    d["R1"] = b.dscratch("s_R1", [S, D], F32)
    d["T0"] = b.dscratch("s_T0", [8, 128, S], BF16)
    d["T1"] = b.dscratch("s_T1", [8, 128, S], BF16)
    d["md"] = dict(gk=b.dscratch("s_gk", [S, 4], F32), dest=b.dscratch("s_dest", [S, 4], U32),
                   xg=b.dscratch("s_xg", [33 * CAP, D], F32), yg=b.dscratch("s_yg", [33 * CAP, D], F32))
    return d


def build_program(dbg=None, upto=99):
    nc = bass.Bass("TRN2", target_bir_lowering=False)
    b = Builder(nc, dbg=dbg)
    d = declare_scratch(b)
    x_in = T(b.inp["x"], "x_in")
    out = T(nc.dram_tensor("out", [S, D], F32, kind="ExternalOutput").ap(), "out")
    steps = []
    with ExitStack() as es:
        b.init_psum(es)
        b.load_consts(es)
        steps = [
            lambda: b.phase_t0(x_in, d["T0"]),
            lambda: b.phase_even_proj(0, d["T0"], b.inp["pos"], d),
            lambda: b.phase_compress(0, d),
            lambda: b.phase_attn(0, d),
            lambda: b.phase_outproj_ln(d["mixT"], b.inp["w_out_even"][0], 0, 0, x_in, d["R0"], d["T1"]),
            lambda: b.phase_xattn(0, b.inp["mem"], d["T1"], d["R0"], d["R1"], d["T0"], d["md"]),
            lambda: b.phase_moe_sparse(0, d["md"], d["R1"], d["R0"], d["T1"]),
            lambda: b.phase_odd_proj(0, d["T1"], d),
            lambda: b.phase_outproj_ln(d["mixT"], b.inp["w_out_odd"][0], 1, 0, d["R0"], d["R1"], d["T0"]),
            lambda: b.phase_xattn(1, b.inp["mem"], d["T0"], d["R1"], d["R0"], d["T1"], d["md"]),
            lambda: b.phase_moe_sparse(1, d["md"], d["R0"], None, None, final_out=out),
        ]
        for i, s_ in enumerate(steps):
            if i < upto:
                s_()
        allr = [v for v in d.values() if isinstance(v, T)] + list(d["md"].values()) + [out]
        b.c.wait_all("sp", allr)
    return nc, b


def phase_odd_proj(self, j, xT, d):
    inp = self.inp
    W = inp["w_in_odd"][j]
    with self.phase() as es:
        wfm = self.sb(es, "od_wfm", [128, 8, 2560], BF16)
        wtm = self.sb(es, "od_wtm", [128, 8, 1024], BF16)
        for kk in range(8):
            rs_ = slice(kk * 128, (kk + 1) * 128)
            self.ldw(wfm[:, kk, 0:1024], W[rs_, 0:1024], [wfm], disjoint=True)
            self.ldw(wfm[:, kk, 1024:2560], W[rs_, 2048:3584], [wfm], disjoint=True)
            self.ldw(wtm[:, kk, :], W[rs_, 1024:2048], [wtm], disjoint=True)
        lbl = self.sb(es, "od_lbl", [128, 2, 4], F32)
        lb = self.sb(es, "od_lb", [128, 4], F32)
        oml = self.sb(es, "od_oml", [128, 4], F32)
        self.ld(lbl[:], inp["hgrn_lbT"], [], [lbl])
        self.V(lambda e: e.tensor_tensor(out=lb[:], in0=lbl[:, 1, :], in1=lbl[:, 0, :], op=ALU.subtract), [lbl], [lb])
        self.A(lambda e: e.activation(out=lb[:], in_=lb[:], func=AF.Sigmoid), [lb], [lb])
        self.V(lambda e: e.tensor_scalar(out=oml[:], in0=lb[:], scalar1=-1.0, scalar2=1.0, op0=ALU.mult, op1=ALU.add), [lb], [oml])
        ng = self.sb(es, "od_ng", [128, 512], F32)
        self.ld(ng[:], inp["hgrn_norm_g"][j:j + 1].rearrange("o h v -> o (h v)").broadcast_to([128, 512]), [], [ng])
        cw = self.sb(es, "od_cw", [128, 4, 3], F32)
        cbv = self.sb(es, "od_cb", [128, 4], F32)
        self.ld(cw[:], inp["conv_wT"][j], [], [cw])
        self.ld(cbv[:], inp["conv_bT"][j], [], [cbv])
        tri = self.sb(es, "od_tri", [64, 64], F32)
        self.ld(tri[:], inp["c_tri64"], [], [tri])
        rmask = self.sb(es, "od_rmask", [128, 512], F32)
        self.V(lambda e: e.memset(rmask[:], 1.0), [], [rmask])
        self.V(lambda e: e.memset(rmask[:].rearrange("p (a b) -> p a b", b=64)[:, :, 0:1], 0.0), [], [rmask])
        zbuf = self.sb(es, "od_z", [128, 4, 514], F32)
        self.V(lambda e: e.memset(zbuf[:], 0.0), [], [zbuf])
        St = self.sb(es, "od_S", [128, 4, 128], F32)
        Sb = self.sb(es, "od_Sb", [128, 4, 128], BF16)
        self.V(lambda e: e.memset(St[:], 0.0), [], [St])
        self.V(lambda e: e.memset(Sb[:], 0.0), [], [Sb])
        xs = [self.sb(es, "od_x%d" % i, [128, 8, 512], BF16) for i in range(2)]
        sg = self.sb(es, "od_sg", [128, 512], F32)
        lf = self.sb(es, "od_lf", [128, 512], F32)
        kf = self.sb(es, "od_kf", [128, 512], F32)
        bt = self.sb(es, "od_b", [128, 512], F32)
        eb = self.sb(es, "od_eb", [128, 512], F32)
        enb = self.sb(es, "od_enb", [128, 512], F32)
        brel = self.sb(es, "od_brel", [128, 512], F32)
        ebl = self.sb(es, "od_ebl", [128, 4, 8], F32)
        A_sb = self.sb(es, "od_A", [128, 4, 512], BF16)
        Bm_sb = self.sb(es, "od_Bm", [128, 4, 512], BF16)
        Kt_sb = self.sb(es, "od_Kt", [128, 4, 512], F32)
        c_sb = self.sb(es, "od_c", [128, 512], F32)
        y_sb = self.sb(es, "od_y", [128, 512], F32)
        od_sb = self.sb(es, "od_od", [128, 4, 512], BF16)
        v_sb = self.sb(es, "od_v", [64, 8, 512], BF16)
        gsn = self.sb(es, "od_gsn", [64, 8, 512], F32)
        atm = self.sb(es, "od_atm", [64, 64], BF16)
        KtT = self.sb(es, "od_KtT", [64, 128], BF16)
        junk = self.sb(es, "od_junk", [64, 128], F32)
        ss = self.sb(es, "od_ss", [64, 1], F32)
        oc = self.sb(es, "od_oc", [64, 8, 512], F32)
        ocT = self.sb(es, "od_ocT", [128, 4, 512], BF16)
        for Q in range(NG):
            x = xs[Q % 2]
            tsl = slice(Q * 512, (Q + 1) * 512)
            if Q == 0:
                self.ld(x[:], xT.t[:, :, tsl].rearrange("c p t -> p c t"), [xT], [x])
            if Q + 1 < NG:
                self.ld(xs[(Q + 1) % 2][:], xT.t[:, :, (Q + 1) * 512:(Q + 2) * 512].rearrange("c p t -> p c t"), [xT], [xs[(Q + 1) % 2]])

            def fm(col0):
                ps = self.rot()
                self.mm_acc(ps[:, :], ps, [(wfm[:, kk, col0:col0 + 128], x[:, kk, :]) for kk in range(8)], [wfm, x])
                return ps
            for hd in range(4):
                psf = fm(512 + hd * 128)
                self.A(lambda e: e.activation(out=sg[:], in_=psf[:, :], func=AF.Sigmoid), [psf], [sg])
                self.V(lambda e: e.tensor_scalar(out=sg[:], in0=sg[:], scalar1=oml[:, hd:hd + 1], scalar2=lb[:, hd:hd + 1], op0=ALU.mult, op1=ALU.add), [sg, oml, lb], [sg])
                self.A(lambda e: e.activation(out=lf[:], in_=sg[:], func=AF.Ln), [sg], [lf])
                self.V(lambda e: e.tensor_scalar(out=kf[:], in0=sg[:], scalar1=-1.0, scalar2=1.0, op0=ALU.mult, op1=ALU.add), [sg], [kf])
                self.V(lambda e: e.tensor_tensor_scan(out=bt[:], data0=rmask[:], data1=lf[:], initial=0.0, op0=ALU.mult, op1=ALU.add), [rmask, lf], [bt])
                self.A(lambda e: e.activation(out=eb[:], in_=bt[:], func=AF.Exp), [bt], [eb])
                self.A(lambda e: e.activation(out=enb[:], in_=bt[:], func=AF.Exp, scale=-1.0), [bt], [enb])
                bv = bt[:].rearrange("p (a b) -> p a b", b=64)
                self.V(lambda e: e.tensor_tensor(out=brel[:].rearrange("p (a b) -> p a b", b=64), in0=bv[:, :, 63:64].broadcast_to([128, 8, 64]), in1=bv, op=ALU.subtract), [bt], [brel])
                self.A(lambda e: e.activation(out=brel[:], in_=brel[:], func=AF.Exp), [brel], [brel])
                self.A(lambda e: e.activation(out=ebl[:, hd, :], in_=bv[:, :, 63], func=AF.Exp), [bt], [ebl])
                self.V(lambda e: e.tensor_tensor(out=Bm_sb[:, hd, :], in0=kf[:], in1=enb[:], op=ALU.mult), [kf, enb], [Bm_sb])
                self.V(lambda e: e.tensor_tensor(out=Kt_sb[:, hd, :], in0=kf[:], in1=brel[:], op=ALU.mult), [kf, brel], [Kt_sb])
                psq = fm(hd * 128)
                self.V(lambda e: e.tensor_tensor(out=A_sb[:, hd, :], in0=psq[:, :], in1=eb[:], op=ALU.mult), [psq, eb], [A_sb])
            for cc in range(4):
                psc = fm(1024 + 1024 + cc * 128)
                self.A(lambda e: e.activation(out=c_sb[:], in_=psc[:, :], func=AF.Copy), [psc], [c_sb])
                psh = fm(1024 + cc * 128)
                self.V(lambda e: e.tensor_tensor(out=zbuf[:, cc, 2:514], in0=psh[:, :], in1=c_sb[:], op=ALU.mult), [psh, c_sb], [zbuf])
                self.V(lambda e: e.tensor_scalar(out=y_sb[:], in0=zbuf[:, cc, 2:514], scalar1=cw[:, cc, 2:3], scalar2=cbv[:, cc:cc + 1], op0=ALU.mult, op1=ALU.add), [zbuf, cw, cbv], [y_sb])
                self.V(lambda e: e.scalar_tensor_tensor(out=y_sb[:], in0=zbuf[:, cc, 1:513], scalar=cw[:, cc, 1:2], in1=y_sb[:], op0=ALU.mult, op1=ALU.add), [zbuf, cw, y_sb], [y_sb])
                self.V(lambda e: e.scalar_tensor_tensor(out=y_sb[:], in0=zbuf[:, cc, 0:512], scalar=cw[:, cc, 0:1], in1=y_sb[:], op0=ALU.mult, op1=ALU.add), [zbuf, cw, y_sb], [y_sb])
                psb_ = fm(1024 + 512 + cc * 128)
                self.V(lambda e: e.tensor_tensor(out=od_sb[:, cc, :], in0=psb_[:, :], in1=y_sb[:], op=ALU.mult), [psb_, y_sb], [od_sb])
                self.V(lambda e: e.tensor_copy(out=zbuf[:, cc, 0:2], in_=zbuf[:, cc, 512:514]), [zbuf], [zbuf])
            self.st(d["mixT"].t[4:8, :, tsl].rearrange("c p t -> p c t"), od_sb[:], [od_sb], [d["mixT"]], disjoint=True)
            for ck in range(8):
                xl = lambda kk: x[:, kk, ck * 64:(ck + 1) * 64]
                ps = self.rot()
                self.mm_acc(ps[0:64, :], ps, [(xl(kk), wtm[:, kk, 0:512]) for kk in range(8)], [wtm, x])
                self.A(lambda e: e.activation(out=v_sb[:, ck, :], in_=ps[0:64, :], func=AF.Copy), [ps], [v_sb])
                ps = self.rot()
                self.mm_acc(ps[0:64, :], ps, [(xl(kk), wtm[:, kk, 512:1024]) for kk in range(8)], [wtm, x])
                self.A(lambda e: e.activation(out=gsn[:, ck, :], in_=ps[0:64, :], func=AF.Silu), [ps], [gsn])
                self.V(lambda e: e.tensor_tensor(out=gsn[:, ck, :], in0=gsn[:, ck, :], in1=ng[0:64, :], op=ALU.mult), [gsn, ng], [gsn])
            for ck in range(8):
                csl = slice(ck * 64, (ck + 1) * 64)
                for hd in range(4):
                    vsl = v_sb[:, ck, hd * 128:(hd + 1) * 128]
                    at = self.rot()
                    self.P(lambda e: e.matmul(at[0:64, 0:64], lhsT=Bm_sb[:, hd, csl], rhs=A_sb[:, hd, csl], start=True, stop=True), [Bm_sb, A_sb], [at])
                    self.V(lambda e: e.tensor_tensor(out=atm[:], in0=at[0:64, 0:64], in1=tri[:], op=ALU.mult), [at, tri], [atm])
                    tp = self.rot()
                    self.P(lambda e: e.transpose(out=tp[0:64, 0:128], in_=Kt_sb[:, hd, csl], identity=self.ident_f[:]), [Kt_sb, self.ident_f], [tp])
                    self.A(lambda e: e.activation(out=KtT[:], in_=tp[0:64, 0:128], func=AF.Copy), [tp], [KtT])
                    o = self.rot()
                    self.P(lambda e: e.matmul(o[0:64, 0:128], lhsT=atm[:], rhs=vsl, start=True, stop=False), [atm, v_sb], [o])
                    self.P(lambda e: e.matmul(o[0:64, 0:128], lhsT=A_sb[:, hd, csl], rhs=Sb[:, hd, :], start=False, stop=True), [A_sb, Sb], [o])
                    dS = self.rot()
                    self.P(lambda e: e.matmul(dS[:, 0:128], lhsT=KtT[:], rhs=vsl, start=True, stop=True), [KtT, v_sb], [dS])
                    self.V(lambda e: e.scalar_tensor_tensor(out=St[:, hd, :], in0=St[:, hd, :], scalar=ebl[:, hd, ck:ck + 1], in1=dS[:, 0:128], op0=ALU.mult, op1=ALU.add), [St, ebl, dS], [St])
                    self.A(lambda e: e.activation(out=Sb[:, hd, :], in_=St[:, hd, :], func=AF.Copy), [St], [Sb])
                    self.A(lambda e: e.activation(out=junk[:], in_=o[0:64, 0:128], func=AF.Square, accum_out=ss[:, 0:1]), [o], [junk, ss])
                    self.V(lambda e: e.tensor_scalar(out=ss[:], in0=ss[:], scalar1=1.0 / 128.0, scalar2=RMS_EPS, op0=ALU.mult, op1=ALU.add), [ss], [ss])
                    self.A(lambda e: e.activation(out=ss[:], in_=ss[:], func=AF.Sqrt), [ss], [ss])
                    self.V(lambda e: e.reciprocal(out=ss[:], in_=ss[:]), [ss], [ss])
                    self.V(lambda e: e.scalar_tensor_tensor(out=oc[:, ck, hd * 128:(hd + 1) * 128], in0=o[0:64, 0:128], scalar=ss[:, 0:1], in1=gsn[:, ck, hd * 128:(hd + 1) * 128], op0=ALU.mult, op1=ALU.mult), [o, ss, gsn], [oc])
            for hd in range(4):
                tp = self.rot()
                for ck in range(8):
                    self.P(lambda e: e.transpose(out=tp[:, ck * 64:(ck + 1) * 64], in_=oc[:, ck, hd * 128:(hd + 1) * 128], identity=self.ident_f[0:64, 0:64]), [oc, self.ident_f], [tp])
                self.A(lambda e: e.activation(out=ocT[:, hd, :], in_=tp[:, :], func=AF.Copy), [tp], [ocT])
            self.st(d["mixT"].t[0:4, :, tsl].rearrange("c p t -> p c t"), ocT[:], [ocT], [d["mixT"]], disjoint=True)


Builder.phase_odd_proj = phase_odd_proj


_PROG = None


def kernel(**inputs):
    global _PROG
    inputs = {k: np.asarray(v) for k, v in inputs.items()}
    if _PROG is None:
        _PROG = build_program()
    nc, b = _PROG
    consts = host_consts()
    in_maps = []
    for core in range(8):
        m = host_layout(inputs, core)
        m.update(consts)
        in_maps.append({k: m[k] for k in b.inp.keys()})
    res = run_bass_kernel_spmd(nc, in_maps, core_ids=list(range(8)))
    out = np.stack([np.asarray(r["out"]) for r in res.results], axis=0)
    return out.astype(np.float32)
```

```python
import numpy as np
import ml_dtypes
from contextlib import ExitStack
import concourse.bass as bass
import concourse.mybir as mybir
from concourse.bass_utils import run_bass_kernel_spmd

F32 = mybir.dt.float32
BF16 = mybir.dt.bfloat16
I32 = mybir.dt.int32
U32 = mybir.dt.uint32
AF = mybir.ActivationFunctionType
ALU = mybir.AluOpType

S = 4096
D = 1024
NT = S // 128
NG = S // 512
ALPHA = 4 ** 0.25
LN_EPS = 1e-5
RMS_EPS = 1e-6
NEGB = -32768.0
CAP = 768


class Res:
    __slots__ = ("name", "w", "r", "dsem", "excl")

    def __init__(self, name):
        self.name = name
        self.excl = False
        self.w = {}
        self.r = {}
        self.dsem = None


class T:
    __slots__ = ("t", "r")

    def __init__(self, t, name):
        self.t = t
        self.r = Res(name)

    def __getitem__(self, k):
        return self.t[k]


def _r(x):
    return x.r if isinstance(x, T) else x


class Sem:
    __slots__ = ("h", "count", "uid")
    _n = 0

    def __init__(self, h):
        self.h = h
        self.count = 0
        Sem._n += 1
        self.uid = Sem._n


class Ctx:
    def __init__(self, nc, same_engine_sync=True):
        self.nc = nc
        self.eng = {"pe": nc.tensor, "dve": nc.vector, "act": nc.scalar, "pool": nc.gpsimd, "sp": nc.sync}
        self.esem = {k: Sem(nc.alloc_semaphore("es_" + k)) for k in self.eng}
        self.waited = {}
        self.same = same_engine_sync
        self.free_dsems = []
        self.all_dsems = []
        self.barrier = {}
        self.n_dsem = 0
        self.n_inst = 0

    def _dsem(self, r):
        if r.dsem is None:
            if self.free_dsems:
                r.dsem = self.free_dsems.pop()
            else:
                r.dsem = Sem(self.nc.alloc_semaphore("ds%d" % self.n_dsem))
                self.all_dsems.append(r.dsem)
                self.n_dsem += 1
        return r.dsem

    def set_barrier(self):
        bar = {s.uid: (s, s.count) for s in self.esem.values() if s.count > 0}
        for ds in self.all_dsems:
            if ds.count > 0:
                bar[ds.uid] = (ds, 16 * ds.count)
        self.barrier = bar

    def release(self, r):
        if r.dsem is not None:
            self.free_dsems.append(r.dsem)
            r.dsem = None

    def _collect(self, reads, writes, skip=None):
        deps = {}

        def add(d):
            for k, (s, v) in d.items():
                if skip is not None and s is skip:
                    continue
                if k not in deps or deps[k][1] < v:
                    deps[k] = (s, v)
        for r in reads:
            add(r.w)
        for w in writes:
            add(w.w)
            add(w.r)
        return deps

    def _waits(self, ename, deps):
        e = self.eng[ename]
        own = self.esem[ename].uid
        for k, (s, v) in deps.items():
            if k == own and (ename == "pe" or not self.same):
                continue
            key = (ename, k)
            if self.waited.get(key, 0) >= v:
                continue
            e.wait_ge(s.h, v)
            self.waited[key] = v
            self.n_inst += 1

    def op(self, ename, fn, reads=(), writes=()):
        reads = [_r(x) for x in reads]
        writes = [_r(x) for x in writes]
        writes = writes + [x for x in reads if x.excl and x not in writes]
        reads = [x for x in reads if not x.excl]
        self._waits(ename, self._collect(reads, writes))
        ins = fn(self.eng[ename])
        s = self.esem[ename]
        ins.then_inc(s.h, 1)
        s.count += 1
        v = s.count
        self.n_inst += 1
        for w in writes:
            w.w = {s.uid: (s, v)}
            w.r = {}
        for r in reads:
            if r not in writes:
                r.r[s.uid] = (s, v)
        return ins

    def dma(self, q, out, in_, reads=(), writes=(), disjoint=False, **kw):
        reads = [_r(x) for x in reads]
        writes = [_r(x) for x in writes]
        dst = writes[0]
        ds = self._dsem(dst)
        self._waits(q, self._collect(reads, writes, skip=ds if disjoint else None))
        ins = self.eng[q].dma_start(out=out, in_=in_, **kw)
        ins.then_inc(ds.h, 16)
        ds.count += 1
        v = 16 * ds.count
        self.n_inst += 1
        for w in writes:
            if disjoint:
                w.w[ds.uid] = (ds, v)
            else:
                w.w = {ds.uid: (ds, v)}
                w.r = {}
        for r in reads:
            r.r[ds.uid] = (ds, v)
        return ins

    def idma(self, out, out_off, in_, in_off, reads=(), writes=(), disjoint=False, **kw):
        reads = [_r(x) for x in reads]
        writes = [_r(x) for x in writes]
        dst = writes[0]
        ds = self._dsem(dst)
        self._waits("pool", self._collect(reads, writes, skip=ds if disjoint else None))
        ins = self.eng["pool"].indirect_dma_start(out=out, out_offset=out_off, in_=in_, in_offset=in_off, **kw)
        ins.then_inc(ds.h, 16)
        ds.count += 1
        v = 16 * ds.count
        self.n_inst += 1
        for w in writes:
            if disjoint:
                w.w[ds.uid] = (ds, v)
            else:
                w.w = {ds.uid: (ds, v)}
                w.r = {}
        for r in reads:
            r.r[ds.uid] = (ds, v)
        return ins

    def wait_all(self, ename, rs):
        self._waits(ename, self._collect([_r(x) for x in rs], []))


class Builder:
    def __init__(self, nc, dbg=None):
        self.nc = nc
        self.c = Ctx(nc)
        self.dbg = dbg or {}
        self.inp = LazyInputs(self)
        self.psb = []
        self.rot_i = 0

    def din(self, name, shape, dt):
        ap = self.nc.dram_tensor(name, list(shape), dt, kind="ExternalInput").ap()
        self.inp[name] = ap
        return ap

    def dscratch(self, name, shape, dt):
        kind = "ExternalOutput" if name in self.dbg else "Internal"
        t = self.nc.dram_tensor(name, list(shape), dt, kind=kind)
        return T(t.ap(), name)

    def sb(self, es, name, shape, dt):
        self.n_sb = getattr(self, "n_sb", 0) + 1
        name = "%s_%d" % (name, self.n_sb)
        t = es.enter_context(self.nc.sbuf_tensor(name, list(shape), dt))
        tt = T(t, name)
        tt.r.w = dict(self.c.barrier)
        es.callback(self.c.release, tt.r)
        return tt

    def phase(self):
        b = self

        class _Ph(ExitStack):
            def __exit__(self, *a):
                r = super().__exit__(*a)
                b.c.set_barrier()
                return r
        return _Ph()

    def init_psum(self, es):
        for i in range(8):
            t = es.enter_context(self.nc.psum_tensor("psb%d" % i, [128, 512], F32))
            self.psb.append(T(t, "psb%d" % i))
            self.psb[-1].r.excl = True

    def rot(self, subset=None):
        subset = subset or list(range(8))
        b = self.psb[subset[self.rot_i % len(subset)]]
        self.rot_i += 1
        return b

    def V(self, fn, r, w):
        return self.c.op("dve", fn, r, w)

    def A(self, fn, r, w):
        return self.c.op("act", fn, r, w)

    def G(self, fn, r, w):
        return self.c.op("pool", fn, r, w)

    def P(self, fn, r, w):
        return self.c.op("pe", fn, r, w)

    def ld(self, out, in_, r, w, **kw):
        return self.c.dma("sp", out, in_, r, w, **kw)

    def st(self, out, in_, r, w, **kw):
        return self.c.dma("sp", out, in_, r, w, **kw)

    def sta(self, out, in_, r, w, **kw):
        return self.c.dma("act", out, in_, r, w, **kw)

    def ldw(self, out, in_, w, **kw):
        return self.c.dma("pool", out, in_, [], w, **kw)

    def mm_acc(self, ps_ap, ps_T, pairs, reads):
        n = len(pairs)
        for i, (l, rh) in enumerate(pairs):
            self.P(lambda e, l=l, rh=rh, i=i: e.matmul(ps_ap, lhsT=l, rhs=rh, start=(i == 0), stop=(i == n - 1)),
                   reads, [ps_T])

    def load_consts(self, es):
        nc = self.nc
        self.ident_f = self.sb(es, "ident_f", [128, 128], F32)
        self.ld(self.ident_f[:], self.inp["c_ident_f"], [], [self.ident_f])
        self.ident_b = self.sb(es, "ident_b", [128, 128], BF16)
        self.ld(self.ident_b[:], self.inp["c_ident_b"], [], [self.ident_b])

    def ln_setup(self, es, li, which, tag):
        g = self.sb(es, "lng" + tag, [128, D], F32)
        b = self.sb(es, "lnb" + tag, [128, D], F32)
        self.ld(g[:], self.inp["ln_g"][li, which:which + 1, :].broadcast_to([128, D]), [], [g])
        self.ld(b[:], self.inp["ln_b"][li, which:which + 1, :].broadcast_to([128, D]), [], [b])
        tmp = dict(
            g=g, b=b,
            st=self.sb(es, "lnst" + tag, [128, 2, 6], F32),
            mv=self.sb(es, "lnmv" + tag, [128, 2], F32),
            rs=self.sb(es, "lnrs" + tag, [128, 1], F32),
            xn=[self.sb(es, "lnxn%d" % i + tag, [128, D], F32) for i in range(2)],
            xTt=[self.sb(es, "lnxT%d" % i + tag, [128, 8, 128], BF16) for i in range(2)],
            xr=[self.sb(es, "lnxr%d" % i + tag, [128, D], F32) for i in range(2)],
            y=self.sb(es, "lny" + tag, [128, D], F32),
            i=0,
        )
        return tmp

    def ln_preload(self, L, ti, xres_src):
        k = L.setdefault("ip", 0) % 2
        L["ip"] += 1
        xr = L["xr"][k]
        self.ld(xr[:], xres_src.t[ti * 128:(ti + 1) * 128, :], [xres_src], [xr])
        return xr

    def ln_tile(self, L, ti, f_aps, f_res, xres_src, xres_dst, xT_dst, final_out=None, hook=None, psum_sub=None, xr_pre=None, defer=False):
        k = L["i"] % 2
        L["i"] += 1
        xr, xn, xTt, y = L["xr"][k], L["xn"][k], L["xTt"][k], L["y"]
        if xr_pre is not None:
            xr = xr_pre
        else:
            self.ld(xr[:], xres_src.t[ti * 128:(ti + 1) * 128, :], [xres_src], [xr])
        for h in range(2):
            self.V(lambda e, h=h: e.scalar_tensor_tensor(out=y[:, h * 512:(h + 1) * 512], in0=xr[:, h * 512:(h + 1) * 512],
                                                         scalar=float(ALPHA), in1=f_aps[h], op0=ALU.mult, op1=ALU.add),
                   [xr] + list(f_res), [y])
        for h in range(2):
            self.V(lambda e, h=h: e.bn_stats(out=L["st"][:, h, :], in_=y[:, h * 512:(h + 1) * 512]), [y], [L["st"]])
        self.V(lambda e: e.bn_aggr(out=L["mv"][:], in_=L["st"][:].rearrange("p a b -> p (a b)")), [L["st"]], [L["mv"]])
        self.A(lambda e: e.activation(out=L["rs"][:], in_=L["mv"][:, 1:2], func=AF.Ln, bias=LN_EPS, scale=1.0), [L["mv"]], [L["rs"]])
        self.A(lambda e: e.activation(out=L["rs"][:], in_=L["rs"][:], func=AF.Exp, scale=-0.5), [L["rs"]], [L["rs"]])
        self.V(lambda e: e.tensor_scalar(out=xn[:], in0=y[:], scalar1=L["mv"][:, 0:1], scalar2=L["rs"][:, 0:1],
                                         op0=ALU.subtract, op1=ALU.mult), [y, L["mv"], L["rs"]], [xn])
        self.V(lambda e: e.tensor_tensor(out=xn[:], in0=xn[:], in1=L["g"][:], op=ALU.mult), [xn, L["g"]], [xn])
        self.V(lambda e: e.tensor_tensor(out=xn[:], in0=xn[:], in1=L["b"][:], op=ALU.add), [xn, L["b"]], [xn])
        if final_out is not None:
            self.sta(final_out.t[ti * 128:(ti + 1) * 128, :], xn[:], [xn], [final_out], disjoint=True)
            return
        self.c.dma(L.get("stq", "act"), xres_dst.t[ti * 128:(ti + 1) * 128, :], xn[:], [xn], [xres_dst], disjoint=True)

        def part_b():
            self.transpose_store(xn, xTt, ti, xT_dst, hook=hook, psum_sub=psum_sub)
        if defer:
            return part_b
        part_b()

    def transpose_store(self, xn, xTt, ti, xT_dst, hook=None, psum_sub=None):
        pts = []
        for half in range(2):
            pt = self.rot(psum_sub)
            for j in range(4):
                ch = half * 4 + j
                self.P(lambda e, j=j, ch=ch: e.transpose(out=pt[:, j * 128:(j + 1) * 128], in_=xn[:, ch * 128:(ch + 1) * 128],
                                                         identity=self.ident_f[:]), [xn, self.ident_f], [pt])
            self.A(lambda e, half=half: e.activation(out=xTt[:, half * 4:(half + 1) * 4, :],
                                                      in_=pt[:].rearrange("p (a b) -> p a b", a=4), func=AF.Copy), [pt], [xTt])
            pts.append(pt)
        if hook is not None:
            hook(ti, pts, xn)
        self.sta(xT_dst.t[:, :, ti * 128:(ti + 1) * 128].rearrange("c p t -> p c t"), xTt[:], [xTt], [xT_dst], disjoint=True)

    def phase_t0(self, x_in, xT_dst):
        with self.phase() as es:
            xs = [self.sb(es, "t0x%d" % i, [128, D], F32) for i in range(2)]
            xTt = [self.sb(es, "t0xT%d" % i, [128, 8, 128], BF16) for i in range(2)]
            self.ld(xs[0][:], x_in.t[0:128, :], [x_in], [xs[0]])
            for ti in range(NT):
                k = ti % 2
                if ti + 1 < NT:
                    self.ld(xs[1 - k][:], x_in.t[(ti + 1) * 128:(ti + 2) * 128, :], [x_in], [xs[1 - k]])
                self.transpose_store(xs[k], xTt[k], ti, xT_dst)

    def phase_outproj_ln(self, mixT, w_ap, li, which, xres_src, xres_dst, xT_dst):
        with self.phase() as es:
            w = self.sb(es, "opw", [128, 8, D], BF16)
            for kk in range(8):
                self.ldw(w[:, kk, :], w_ap[kk * 128:(kk + 1) * 128, :], [w], disjoint=True)
            L = self.ln_setup(es, li, which, "op")
            mt = [self.sb(es, "opm%d" % i, [128, 8, 128], BF16) for i in range(2)]

            def pre(ti):
                k = ti % 2
                self.ld(mt[k][:], mixT.t[:, :, ti * 128:(ti + 1) * 128].rearrange("c p t -> p c t"), [mixT], [mt[k]])
                xr = self.ln_preload(L, ti, xres_src)
                ps = [self.rot(), self.rot()]
                for h in range(2):
                    self.mm_acc(ps[h][:, :], ps[h], [(mt[k][:, kk, :], w[:, kk, h * 512:(h + 1) * 512]) for kk in range(8)], [mt[k], w])
                return ps, xr
            nxt = pre(0)
            for ti in range(NT):
                cur = nxt
                if ti + 1 < NT:
                    nxt = pre(ti + 1)
                ps, xr = cur
                self.ln_tile(L, ti, [ps[0][:, :], ps[1][:, :]], ps, xres_src, xres_dst, xT_dst, xr_pre=xr)

    def rope_tables(self, es, pos_ap):
        cos = self.sb(es, "ropecos", [128, S], F32)
        sin = self.sb(es, "ropesin", [128, S], F32)
        invf = self.sb(es, "invf", [128, 1], F32)
        self.ld(invf[:], self.inp["c_invf"], [], [invf])
        twopi = 2 * np.pi
        c1 = float(np.float32(6.28125))
        c2 = float(twopi - 6.28125)
        with self.phase() as es2:
            pi_ = self.sb(es2, "rp_pi", [128, 1024], I32)
            ang = self.sb(es2, "rp_ang", [128, 1024], F32)
            ki = self.sb(es2, "rp_ki", [128, 1024], I32)
            kf = self.sb(es2, "rp_kf", [128, 1024], F32)
            rr = self.sb(es2, "rp_rr", [128, 1024], F32)
            w0 = self.sb(es2, "rp_w0", [128, 1024], F32)
            for q in range(4):
                sl = slice(q * 1024, (q + 1) * 1024)
                self.ld(pi_[:], pos_ap[0:1, sl].broadcast_to([128, 1024]), [], [pi_])
                self.V(lambda e: e.tensor_copy(out=ang[:], in_=pi_[:]), [pi_], [ang])
                self.V(lambda e: e.tensor_scalar(out=ang[:], in0=ang[:], scalar1=invf[:, 0:1], scalar2=None, op0=ALU.mult), [ang, invf], [ang])
                self.V(lambda e: e.tensor_scalar(out=ki[:], in0=ang[:], scalar1=float(1 / twopi), scalar2=None, op0=ALU.mult), [ang], [ki])
                self.V(lambda e: e.tensor_copy(out=kf[:], in_=ki[:]), [ki], [kf])
                self.V(lambda e: e.scalar_tensor_tensor(out=rr[:], in0=kf[:], scalar=-c1, in1=ang[:], op0=ALU.mult, op1=ALU.add), [kf, ang], [rr])
                self.V(lambda e: e.scalar_tensor_tensor(out=rr[:], in0=kf[:], scalar=-c2, in1=rr[:], op0=ALU.mult, op1=ALU.add), [kf, rr], [rr])
                for (dst, shift) in ((sin, 0.0), (cos, np.pi / 2)):
                    self.V(lambda e: e.tensor_scalar(out=w0[:], in0=rr[:], scalar1=float(shift), scalar2=float(np.pi), op0=ALU.add, op1=ALU.is_gt), [rr], [w0])
                    self.V(lambda e: e.tensor_scalar(out=w0[:], in0=w0[:], scalar1=float(-twopi), scalar2=float(shift), op0=ALU.mult, op1=ALU.add), [w0], [w0])
                    self.V(lambda e: e.tensor_tensor(out=w0[:], in0=w0[:], in1=rr[:], op=ALU.add), [w0, rr], [w0])
                    self.V(lambda e: e.tensor_scalar(out=w0[:], in0=w0[:], scalar1=float(-np.pi), scalar2=float(np.pi), op0=ALU.max, op1=ALU.min), [w0], [w0])
                    self.A(lambda e, dst=dst: e.activation(out=dst[:, sl], in_=w0[:], func=AF.Sin), [w0], [dst])
        return cos, sin

    def phase_even_proj(self, j, xT, pos_ap, d):
        inp = self.inp
        W = inp["w_in_even"][j]
        with self.phase() as es:
            cos, sin = self.rope_tables(es, pos_ap)
            rotm = self.sb(es, "rotm", [128, 128], BF16)
            self.ld(rotm[:], inp["c_rotm"], [], [rotm])

            def wload(name, cols, a):
                t = self.sb(es, name, [128, 8, cols], BF16)
                for kk in range(8):
                    self.ldw(t[:, kk, :], W[kk * 128:(kk + 1) * 128, a:a + cols], [t], disjoint=True)
                return t
            wq = wload("wq", 512, 0)
            wk4 = self.sb(es, "wk4", [128, 8, 4, 128], BF16)
            for i, a in enumerate((512, 640, 768, 1024)):
                for kk in range(8):
                    self.ldw(wk4[:, kk, i, :], W[kk * 128:(kk + 1) * 128, a:a + 128], [wk4], disjoint=True)
            wvt = self.sb(es, "wvt", [128, 8, 280], BF16)
            for (o, a, n) in ((0, 896, 128), (128, 1152, 128), (256, 1280, 24)):
                for kk in range(8):
                    self.ldw(wvt[:, kk, o:o + n], W[kk * 128:(kk + 1) * 128, a:a + n], [wvt], disjoint=True)
            wu = wload("wu", 512, 1304)
            wv = wload("wv", 512, 1816)
            lng = self.sb(es, "sgu_g", [128, 512], F32)
            lnb = self.sb(es, "sgu_bt", [128, 512], F32)
            self.ld(lng[:], inp["sgu_ln_g"][j:j + 1].rearrange("o g c -> o (g c)").broadcast_to([128, 512]), [], [lng])
            self.ld(lnb[:], inp["sgu_ln_b"][j:j + 1].rearrange("o g c -> o (g c)").broadcast_to([128, 512]), [], [lnb])
            bb = self.sb(es, "sgu_bb", [128, 4, 128], F32)
            self.ld(bb[:].rearrange("p g t -> p (g t)"), inp["sgu_b"][j:j + 1].rearrange("o g c -> o (g c)").broadcast_to([128, 512]), [], [bb])
            wmf = self.sb(es, "sgu_wf", [128, 4, 128], F32)
            tri = self.sb(es, "sgu_tri", [128, 128], F32)
            self.ld(wmf[:], inp["sgu_wT"][j].rearrange("g s t -> s g t"), [], [wmf])
            self.ld(tri[:], inp["c_tri"], [], [tri])
            wm = self.sb(es, "sgu_wm", [128, 4, 128], BF16)
            for g in range(4):
                self.V(lambda e, g=g: e.tensor_tensor(out=wm[:, g, :], in0=wmf[:, g, :], in1=tri[:], op=ALU.mult), [wmf, tri], [wm])
            xs = [self.sb(es, "ep_x%d" % i, [128, 8, 512], BF16) for i in range(2)]
            q_sb = self.sb(es, "ep_q", [128, 4, 512], BF16)
            qr_sb = self.sb(es, "ep_qr", [128, 4, 512], BF16)
            k_sb = self.sb(es, "ep_k", [128, 4, 512], BF16)
            kt_sb = self.sb(es, "ep_kt", [128, 512], BF16)
            ug = self.sb(es, "ep_ug", [128, 4, 512], BF16)
            ob = self.sb(es, "ep_ob", [128, 4, 512], BF16)
            t1 = self.sb(es, "ep_t1", [128, 512], F32)
            t2 = self.sb(es, "ep_t2", [128, 512], F32)
            vg = self.sb(es, "ep_vg", [128, 512], F32)
            vn = self.sb(es, "ep_vn", [128, 512], BF16)
            st = self.sb(es, "ep_st", [128, 4, 6], F32)
            mv = self.sb(es, "ep_mv", [128, 4, 2], F32)
            rs = self.sb(es, "ep_rs", [128, 4], F32)
            sg1 = self.sb(es, "ep_sg1", [128, 4, 128], F32)
            vs_st = self.sb(es, "ep_vs", [128, 4, 2, 65], BF16)
            vw_st = self.sb(es, "ep_vw", [128, 4, 2, 65], BF16)
            gt_st = self.sb(es, "ep_gt", [128, 4, 24], F32)
            self.G(lambda e: e.memset(vs_st[:], 1.0), [], [vs_st])
            self.G(lambda e: e.memset(vw_st[:], 1.0), [], [vw_st])

            def rope(ps, src_bf, dst_ap, dstT, Q):
                tsl = slice(Q * 512, (Q + 1) * 512)
                p2 = self.rot()
                self.P(lambda e: e.matmul(p2[:, :], lhsT=rotm[:], rhs=src_bf[0], start=True, stop=True), [rotm, src_bf[1]], [p2])
                self.V(lambda e: e.tensor_tensor(out=t1[:], in0=ps[:, :], in1=cos[:, tsl], op=ALU.mult), [ps, cos], [t1])
                self.V(lambda e: e.tensor_tensor(out=t2[:], in0=p2[:, :], in1=sin[:, tsl], op=ALU.mult), [p2, sin], [t2])
                self.V(lambda e: e.tensor_tensor(out=dst_ap, in0=t1[:], in1=t2[:], op=ALU.add), [t1, t2], [dstT])

            for Q in range(NG):
                x = xs[Q % 2]
                tsl = slice(Q * 512, (Q + 1) * 512)
                if Q == 0:
                    self.ld(x[:], xT.t[:, :, tsl].rearrange("c p t -> p c t"), [xT], [x])
                if Q + 1 < NG:
                    self.ld(xs[(Q + 1) % 2][:], xT.t[:, :, (Q + 1) * 512:(Q + 2) * 512].rearrange("c p t -> p c t"), [xT], [xs[(Q + 1) % 2]])
                for cq in range(4):
                    ps = self.rot()
                    self.mm_acc(ps[:, :], ps, [(wq[:, kk, cq * 128:(cq + 1) * 128], x[:, kk, :]) for kk in range(8)], [wq, x])
                    self.A(lambda e: e.activation(out=q_sb[:, cq, :], in_=ps[:, :], func=AF.Copy), [ps], [q_sb])
                    rope(ps, (q_sb[:, cq, :], q_sb), qr_sb[:, cq, :], qr_sb, Q)
                self.st(d["q"].t[:, :, tsl].rearrange("c p t -> p c t"), q_sb[:], [q_sb], [d["q"]], disjoint=True)
                self.st(d["qr"].t[:, :, tsl].rearrange("c p t -> p c t"), qr_sb[:], [qr_sb], [d["qr"]], disjoint=True)
                for i in range(4):
                    ps = self.rot()
                    self.mm_acc(ps[:, :], ps, [(wk4[:, kk, i, :], x[:, kk, :]) for kk in range(8)], [wk4, x])
                    if i < 2:
                        self.A(lambda e: e.activation(out=k_sb[:, i, :], in_=ps[:, :], func=AF.Copy), [ps], [k_sb])
                    else:
                        self.A(lambda e: e.activation(out=kt_sb[:], in_=ps[:, :], func=AF.Copy), [ps], [kt_sb])
                        rope(ps, (kt_sb[:], kt_sb), k_sb[:, i, :], k_sb, Q)
                for i, nm in enumerate(("kc", "vc", "ks", "kw")):
                    self.st(d[nm].t[:, tsl], k_sb[:, i, :], [k_sb], [d[nm]], disjoint=True)
                for cu in range(4):
                    ps = self.rot()
                    self.mm_acc(ps[:, :], ps, [(wu[:, kk, cu * 128:(cu + 1) * 128], x[:, kk, :]) for kk in range(8)], [wu, x])
                    self.A(lambda e: e.activation(out=ug[:, cu, :], in_=ps[:, :], func=AF.Gelu_apprx_tanh), [ps], [ug])
                for jt in range(4):
                    xl = lambda kk: x[:, kk, jt * 128:(jt + 1) * 128]
                    ps = self.rot()
                    self.mm_acc(ps[:, :], ps, [(xl(kk), wv[:, kk, :]) for kk in range(8)], [wv, x])
                    self.A(lambda e: e.activation(out=vg[:], in_=ps[:, :], func=AF.Gelu_apprx_tanh), [ps], [vg])
                    for g in range(4):
                        self.V(lambda e, g=g: e.bn_stats(out=st[:, g, :], in_=vg[:, g * 128:(g + 1) * 128]), [vg], [st])
                    for g in range(4):
                        self.V(lambda e, g=g: e.bn_aggr(out=mv[:, g, :], in_=st[:, g, :]), [st], [mv])
                    self.A(lambda e: e.activation(out=rs[:], in_=mv[:, :, 1], func=AF.Sqrt, bias=LN_EPS, scale=1.0), [mv], [rs])
                    self.V(lambda e: e.reciprocal(out=rs[:], in_=rs[:]), [rs], [rs])
                    for g in range(4):
                        self.V(lambda e, g=g: e.tensor_scalar(out=vg[:, g * 128:(g + 1) * 128], in0=vg[:, g * 128:(g + 1) * 128],
                                                              scalar1=mv[:, g, 0:1], scalar2=rs[:, g:g + 1], op0=ALU.subtract, op1=ALU.mult),
                               [vg, mv, rs], [vg])
                    self.V(lambda e: e.tensor_tensor(out=vg[:], in0=vg[:], in1=lng[:], op=ALU.mult), [vg, lng], [vg])
                    self.V(lambda e: e.tensor_tensor(out=vn[:], in0=vg[:], in1=lnb[:], op=ALU.add), [vg, lnb], [vn])
                    psm = self.rot()
                    for g in range(4):
                        self.P(lambda e, g=g: e.matmul(psm[:, g * 128:(g + 1) * 128], lhsT=vn[:, g * 128:(g + 1) * 128], rhs=wm[:, g, :],
                                                       start=True, stop=True), [vn, wm], [psm])
                    self.V(lambda e: e.tensor_tensor(out=sg1[:], in0=psm[:, :].rearrange("p (g t) -> p g t", g=4), in1=bb[:], op=ALU.add), [psm, bb], [sg1])
                    self.V(lambda e: e.tensor_tensor(out=ob[:, :, jt * 128:(jt + 1) * 128], in0=sg1[:], in1=ug[:, :, jt * 128:(jt + 1) * 128], op=ALU.mult),
                           [sg1, ug], [ob])
                    pst = self.rot()
                    self.mm_acc(pst[:, 0:256], pst, [(xl(kk), wvt[:, kk, 0:256]) for kk in range(8)], [wvt, x])
                    self.mm_acc(pst[:, 256:280], pst, [(xl(kk), wvt[:, kk, 256:280]) for kk in range(8)], [wvt, x])
                    self.A(lambda e: e.activation(out=vs_st[:, jt, :, 0:64], in_=pst[:, 0:128].rearrange("p (g d) -> p g d", g=2), func=AF.Copy), [pst], [vs_st])
                    self.A(lambda e: e.activation(out=vw_st[:, jt, :, 0:64], in_=pst[:, 128:256].rearrange("p (g d) -> p g d", g=2), func=AF.Copy), [pst], [vw_st])
                    self.A(lambda e: e.activation(out=gt_st[:, jt, :], in_=pst[:, 256:280], func=AF.Sigmoid), [pst], [gt_st])
                self.st(d["mixT"].t[4:8, :, tsl].rearrange("c p t -> p c t"), ob[:], [ob], [d["mixT"]], disjoint=True)
                self.st(d["vs"].t[tsl, :, :].rearrange("(j p) g d -> p j g d", p=128), vs_st[:], [vs_st], [d["vs"]], disjoint=True)
                self.st(d["vw"].t[tsl, :, :].rearrange("(j p) g d -> p j g d", p=128), vw_st[:], [vw_st], [d["vw"]], disjoint=True)
                self.st(d["gates"].t[tsl, :].rearrange("(j p) n -> p j n", p=128), gt_st[:], [gt_st], [d["gates"]], disjoint=True)


IN_SHAPES = {
    "x": ([S, D], F32), "mem": ([256, D], F32), "pos": ([1, S], I32),
    "w_in_even": ([1, D, 2328], F32), "cmp_posT": ([1, 2, 128, 32], F32), "nsa_cmp_w1": ([1, 2, 2048, 256], F32),
    "nsa_cmp_w2": ([1, 2, 256, 64], F32), "sgu_ln_g": ([1, 4, 128], F32), "sgu_ln_b": ([1, 4, 128], F32),
    "sgu_wT": ([1, 4, 128, 128], F32), "sgu_b": ([1, 4, 128], F32), "w_out_even": ([1, D, D], F32),
    "w_in_odd": ([1, D, 3584], F32), "hgrn_lbT": ([128, 2, 4], F32), "hgrn_norm_g": ([1, 4, 128], F32),
    "conv_wT": ([1, 128, 4, 3], F32), "conv_bT": ([1, 128, 4], F32), "w_out_odd": ([1, D, D], F32),
    "xattn_w_q": ([2, D, D], F32), "xattn_w_kv": ([2, D, 2 * D], F32), "xattn_w_o": ([2, D, D], F32),
    "ln_g": ([2, 3, D], F32), "ln_b": ([2, 3, D], F32), "router_w": ([2, D, 32], F32), "router_b": ([2, 32], F32),
    "expert_w_gu": ([2, 32, D, 2 * D], F32), "bguT": ([2, 128, 32, 16], F32), "expert_w_dn": ([2, 32, D, D], F32),
    "expert_b_dn": ([2, 32, D], F32),
    "c_ident_f": ([128, 128], F32), "c_ident_b": ([128, 128], BF16), "c_rotm": ([128, 128], BF16), "c_invf": ([128, 1], F32),
    "c_tri": ([128, 128], F32), "c_band": ([128, 8, 512], BF16), "c_cmpb": ([128, 2, S], BF16), "c_mcmp": ([128, 2, 64], BF16),
    "c_E": ([64, S], BF16), "c_cmul": ([128, 32, 64], F32), "c_cadd": ([128, 32, 64], F32),
    "c_tri64": ([64, 64], F32), "c_lts": ([128, 128], F32), "c_ebase": ([128, 32], F32), "c_dumpoff": ([128, 32], F32),
}


class LazyInputs(dict):
    def __init__(self, b):
        super().__init__()
        self.b = b

    def __missing__(self, name):
        shape, dt = IN_SHAPES[name]
        ap = self.b.nc.dram_tensor(name, list(shape), dt, kind="ExternalInput").ap()
        self[name] = ap
        return ap


def host_consts():
    bf = ml_dtypes.bfloat16
    c = {}
    c["c_ident_f"] = np.eye(128, dtype=np.float32)
    c["c_ident_b"] = np.eye(128, dtype=np.float32).astype(bf)
    rot = np.zeros((128, 128), np.float32)
    for m in range(128):
        if m % 64 < 32:
            rot[m + 32, m] = -1.0
        else:
            rot[m - 32, m] = 1.0
    c["c_rotm"] = rot.astype(bf)
    inv = 1.0 / (10000.0 ** (np.arange(0, 64, 2, dtype=np.float32) / 64.0))
    c["c_invf"] = np.tile(inv.astype(np.float32), 4).reshape(128, 1)
    s_ = np.arange(128)[:, None]
    t_ = np.arange(128)[None, :]
    c["c_tri"] = (s_ <= t_).astype(np.float32)
    c["c_lts"] = (s_ < t_).astype(np.float32)
    c["c_ebase"] = np.tile((np.arange(32, dtype=np.float32) * CAP)[None, :], (128, 1))
    c["c_dumpoff"] = np.tile(((31 - np.arange(32, dtype=np.float32)) * CAP)[None, :], (128, 1))
    c["c_tri64"] = (np.arange(64)[:, None] <= np.arange(64)[None, :]).astype(np.float32)
    kl = np.arange(128)[:, None, None]
    o = np.arange(8)[None, :, None]
    ql = np.arange(512)[None, None, :]
    dist = ql - kl + (4 - o) * 128
    c["c_band"] = np.where((dist >= 0) & (dist < 512), 0.0, NEGB).astype(bf)
    n = (np.arange(2)[None, :, None] * 128 + np.arange(128)[:, None, None])
    t = np.arange(S)[None, None, :]
    c["c_cmpb"] = np.where((16 * n + 31 <= t) & (n < 255), 0.0, NEGB).astype(bf)
    cs = np.arange(255)[:, None] * 16
    ss = np.arange(64)[None, :] * 64
    ov = np.clip(np.minimum(cs + 32, ss + 64) - np.maximum(cs, ss), 0, None) / 32.0
    m = np.zeros((256, 64), np.float32)
    m[:255] = ov
    c["c_mcmp"] = m.reshape(2, 128, 64).transpose(1, 0, 2).astype(bf)
    c["c_E"] = (np.arange(64)[:, None] == (np.arange(S)[None, :] // 64)).astype(np.float32).astype(bf)
    tt = np.arange(32)[None, :, None] * 128 + np.arange(128)[:, None, None]
    cur = tt // 64
    jj = np.arange(64)[None, None, :]
    f0 = (jj == 0)
    f1 = (jj == cur)
    f2 = (jj == cur - 1)
    forced = f0 | f1 | f2
    valid = jj <= cur
    cadd = np.where(forced, 1e4 + 2.0 * f0 + 1.0 * (f1 & ~f0) , np.where(valid, 0.0, -1.0 - 0.001 * jj))
    c["c_cmul"] = (valid & ~forced).astype(np.float32)
    c["c_cadd"] = cadd.astype(np.float32)
    return c


def host_layout(inputs, b):
    f = lambda a: np.ascontiguousarray(a)
    m = {}
    m["x"] = f(inputs["x"][b])
    m["mem"] = f(inputs["mem"][b])
    m["pos"] = f(inputs["positions"][b:b + 1]).astype(np.int32)
    for k in ("w_in_even", "nsa_cmp_w1", "nsa_cmp_w2", "sgu_ln_g", "sgu_ln_b", "sgu_b", "w_out_even", "w_in_odd",
              "hgrn_norm_g", "w_out_odd", "xattn_w_q", "xattn_w_kv", "xattn_w_o", "ln_g", "ln_b", "router_w", "router_b",
              "expert_w_gu", "expert_w_dn", "expert_b_dn"):
        m[k] = inputs[k]
    cp = np.transpose(inputs["nsa_cmp_pos"], (0, 1, 3, 2))
    m["cmp_posT"] = f(np.concatenate([cp, cp], axis=2))
    m["sgu_wT"] = f(np.transpose(inputs["sgu_w"], (0, 1, 3, 2)))
    m["hgrn_lbT"] = f(inputs["hgrn_lb_logits"].reshape(2, 4, 128).transpose(2, 0, 1))
    m["conv_wT"] = f(inputs["conv_w"].reshape(1, 3, 4, 128).transpose(0, 3, 2, 1))
    m["conv_bT"] = f(inputs["conv_b"].reshape(1, 4, 128).transpose(0, 2, 1))
    m["bguT"] = f(inputs["expert_b_gu"].reshape(2, 32, 16, 128).transpose(0, 3, 1, 2))
    return m


def phase_compress(self, j, d):
    inp = self.inp
    with self.phase() as es:
        tT = self.sb(es, "cp_t", [128, S], BF16)
        w1 = self.sb(es, "cp_w1", [128, 32, 256], BF16)
        w2 = self.sb(es, "cp_w2", [128, 2, 64], BF16)
        posf = self.sb(es, "cp_posf", [128, 32], F32)
        posb = self.sb(es, "cp_posb", [128, 32], BF16)
        hT = self.sb(es, "cp_hT", [128, 2, 256], BF16)
        cb = self.sb(es, "cp_cb", [128, 1], F32)
        kc_sb = self.sb(es, "cp_kc", [128, 2, 256], BF16)
        vcm = self.sb(es, "cp_vcm", [128, 2, 2, 129], BF16)
        self.V(lambda e: e.memset(hT[:], 0.0), [], [hT])
        self.V(lambda e: e.memset(vcm[:], 1.0), [], [vcm])
        for kt in range(2):
            for g in range(2):
                self.ld(vcm[:, kt, g, 65:129], inp["c_mcmp"][:, kt, :], [], [vcm], disjoint=True)
        tv = tT[:].rearrange("p (n r) -> p n r", r=16)
        for which, src in ((0, d["kc"]), (1, d["vc"])):
            self.ld(tT[:], src.t, [src], [tT])
            for half in range(2):
                self.ldw(w1[half * 64:(half + 1) * 64, :, :], inp["nsa_cmp_w1"][j, which].rearrange("(l d) h -> d l h", d=64), [w1], disjoint=True)
            self.ldw(w2[:], inp["nsa_cmp_w2"][j, which].rearrange("(c p) d -> p c d", p=128), [w2])
            self.ld(posf[:], inp["cmp_posT"][j, which], [], [posf])
            self.V(lambda e: e.tensor_copy(out=posb[:], in_=posf[:]), [posf], [posb])
            for g in range(2):
                gp = slice(g * 64, (g + 1) * 64)
                for hc in range(2):
                    ps = self.rot()
                    self.mm_acc(ps[:, 0:255], ps, [(w1[gp, l, hc * 128:(hc + 1) * 128], tv[gp, (l // 16):(l // 16) + 255, l % 16]) for l in range(32)], [w1, tT])
                    ps2 = self.rot()
                    self.mm_acc(ps2[:, 0:1], ps2, [(w1[gp, l, hc * 128:(hc + 1) * 128], posb[gp, l:l + 1]) for l in range(32)], [w1, posb])
                    self.A(lambda e: e.activation(out=cb[:], in_=ps2[:, 0:1], func=AF.Copy), [ps2], [cb])
                    self.A(lambda e: e.activation(out=hT[:, hc, 0:255], in_=ps[:, 0:255], func=AF.Gelu_apprx_tanh, bias=cb[:, 0:1], scale=1.0), [ps, cb], [hT])
                if which == 0:
                    for half in range(2):
                        pso = self.rot()
                        hs = slice(half * 64, (half + 1) * 64)
                        self.mm_acc(pso[hs, 0:256], pso, [(w2[:, hc, :], hT[:, hc, :]) for hc in range(2)], [w2, hT])
                        self.A(lambda e: e.activation(out=kc_sb[hs, g, :], in_=pso[hs, 0:256], func=AF.Copy), [pso], [kc_sb])
                else:
                    for kt in range(2):
                        pso = self.rot()
                        self.mm_acc(pso[:, 0:64], pso, [(hT[:, hc, kt * 128:(kt + 1) * 128], w2[:, hc, :]) for hc in range(2)], [w2, hT])
                        self.A(lambda e: e.activation(out=vcm[:, kt, g, 0:64], in_=pso[:, 0:64], func=AF.Copy), [pso], [vcm])
        self.st(d["kcd"].t, kc_sb[:], [kc_sb], [d["kcd"]])
        self.st(d["vcm"].t, vcm[:], [vcm], [d["vcm"]])


def phase_attn(self, j, d):
    inp = self.inp
    SC, ACC, MISC = [0, 1], [2, 3, 4, 5], [6, 7]
    with self.phase() as es:
        kc_dup = self.sb(es, "at_kc", [128, 2, 256], BF16)
        vcm = self.sb(es, "at_vcm", [128, 2, 2, 129], BF16)
        kcomb = self.sb(es, "at_kcomb", [128, 2, 2, S], BF16)
        kw_dup = self.sb(es, "at_kw", [128, 2, S], BF16)
        vs_sb = self.sb(es, "at_vs", [128, 32, 2, 65], BF16)
        vw_sb = self.sb(es, "at_vw", [128, 32, 2, 65], BF16)
        cmpb = self.sb(es, "at_cmpb", [128, 2, S], BF16)
        band = self.sb(es, "at_band", [128, 8, 512], BF16)
        cmul = self.sb(es, "at_cmul", [128, 32, 64], F32)
        cadd = self.sb(es, "at_cadd", [128, 32, 64], F32)
        gts = self.sb(es, "at_gates", [128, 32, 24], F32)
        qsel = [self.sb(es, "at_qsel%d" % i, [128, 512], BF16) for i in range(4)]
        self.ld(kc_dup[:], d["kcd"].t, [d["kcd"]], [kc_dup])
        self.ld(vcm[:], d["vcm"].t, [d["vcm"]], [vcm])
        for g in range(2):
            for half in range(2):
                hs = slice(half * 64, (half + 1) * 64)
                os_ = slice((1 - half) * 64, (2 - half) * 64)
                self.ld(kcomb[hs, half, g, :], d["ks"].t[g * 64:(g + 1) * 64, :], [d["ks"]], [kcomb], disjoint=True)
                self.ld(kcomb[os_, half, g, :], inp["c_E"], [], [kcomb], disjoint=True)
                self.ld(kw_dup[hs, g, :], d["kw"].t[g * 64:(g + 1) * 64, :], [d["kw"]], [kw_dup], disjoint=True)
        self.ld(vs_sb[:], d["vs"].t.rearrange("(k p) g d -> p k g d", p=128), [d["vs"]], [vs_sb])
        self.ld(vw_sb[:], d["vw"].t.rearrange("(k p) g d -> p k g d", p=128), [d["vw"]], [vw_sb])
        self.ld(cmpb[:], inp["c_cmpb"], [], [cmpb])
        self.ld(band[:], inp["c_band"], [], [band])
        self.ld(cmul[:], inp["c_cmul"], [], [cmul])
        self.ld(cadd[:], inp["c_cadd"], [], [cadd])
        self.ld(gts[:], d["gates"].t.rearrange("(t p) n -> p t n", p=128), [d["gates"]], [gts])
        qs = [self.sb(es, "at_q%d" % i, [128, 4, 512], BF16) for i in range(2)]
        qrs = [self.sb(es, "at_qr%d" % i, [128, 4, 512], BF16) for i in range(2)]
        o_acc = self.sb(es, "at_oacc", [128, 4, 512], F32)
        imp = self.sb(es, "at_imp", [128, 4, 2, 64], F32)
        pts = [self.sb(es, "at_pt%d" % i, [128, 512], BF16) for i in range(4)]
        pti = [0]
        rz = self.sb(es, "at_rz", [128, 1], F32)
        gz = self.sb(es, "at_gz", [128, 1], F32)
        sc1 = self.sb(es, "at_sc1", [128, 64], F32)
        sc2 = self.sb(es, "at_sc2", [128, 64], F32)
        m1 = self.sb(es, "at_m1", [128, 8], F32)
        m2 = self.sb(es, "at_m2", [128, 8], F32)
        sbias = self.sb(es, "at_sbias", [128, 128], F32)
        oT = self.sb(es, "at_oT", [128, 4, 512], BF16)
        acc = [self.psb[i] for i in ACC]

        def next_pt():
            p = pts[pti[0] % len(pts)]
            pti[0] += 1
            return p

        def finish(jt, qt, h, br, first):
            a = acc[jt]
            self.V(lambda e: e.tensor_scalar(out=rz[:], in0=a[:, 64:65], scalar1=1e-30, scalar2=None, op0=ALU.max), [a], [rz])
            self.V(lambda e: e.reciprocal(out=rz[:], in_=rz[:]), [rz], [rz])
            self.V(lambda e: e.tensor_tensor(out=gz[:], in0=rz[:], in1=gts[:, qt, h * 3 + br:h * 3 + br + 1], op=ALU.mult), [rz, gts], [gz])
            osl = o_acc[:, jt, h * 64:(h + 1) * 64]
            if first:
                self.V(lambda e: e.tensor_scalar(out=osl, in0=a[:, 0:64], scalar1=gz[:, 0:1], scalar2=None, op0=ALU.mult), [a, gz], [o_acc])
            else:
                self.V(lambda e: e.scalar_tensor_tensor(out=osl, in0=a[:, 0:64], scalar=gz[:, 0:1], in1=osl, op0=ALU.mult, op1=ALU.add), [a, gz, o_acc], [o_acc])

        for Q in range(NG):
            qsb, qrsb = qs[Q % 2], qrs[Q % 2]
            tsl = slice(Q * 512, (Q + 1) * 512)
            self.ld(qsb[:], d["q"].t[:, :, tsl].rearrange("c p t -> p c t"), [d["q"]], [qsb])
            self.ld(qrsb[:], d["qr"].t[:, :, tsl].rearrange("c p t -> p c t"), [d["qr"]], [qrsb])
            for g in range(2):
                nkt = 2 if Q >= 4 else 1
                for h in range(4 * g, 4 * g + 4):
                    c, hp = h // 2, slice((h % 2) * 64, (h % 2) * 64 + 64)
                    ptc = []
                    for kt in range(nkt):
                        sc = self.rot(SC)
                        self.P(lambda e: e.matmul(sc[:, :], lhsT=kc_dup[hp, g, kt * 128:(kt + 1) * 128], rhs=qsb[hp, c, :], start=True, stop=False), [kc_dup, qsb], [sc])
                        self.P(lambda e: e.matmul(sc[:, :], lhsT=self.ident_b[:], rhs=cmpb[:, kt, tsl], start=False, stop=True), [self.ident_b, cmpb], [sc])
                        pt = next_pt()
                        self.A(lambda e: e.activation(out=pt[:], in_=sc[:, :], func=AF.Exp, scale=0.125), [sc], [pt])
                        ptc.append(pt)
                    for jt in range(4):
                        a = acc[jt]
                        for kt in range(nkt):
                            self.P(lambda e: e.matmul(a[:, 0:129], lhsT=ptc[kt][:, jt * 128:(jt + 1) * 128], rhs=vcm[:, kt, g, :], start=(kt == 0), stop=(kt == nkt - 1)), [ptc[kt], vcm], [a])
                        finish(jt, 4 * Q + jt, h, 0, True)
                        isl = imp[:, jt, g, :]
                        if h % 4 == 0:
                            self.V(lambda e: e.tensor_scalar(out=isl, in0=a[:, 65:129], scalar1=rz[:, 0:1], scalar2=None, op0=ALU.mult), [a, rz], [imp])
                        else:
                            self.V(lambda e: e.scalar_tensor_tensor(out=isl, in0=a[:, 65:129], scalar=rz[:, 0:1], in1=isl, op0=ALU.mult, op1=ALU.add), [a, rz, imp], [imp])
                tp = self.rot(MISC)
                for jt in range(4):
                    qt = 4 * Q + jt
                    self.V(lambda e: e.tensor_tensor(out=sc1[:], in0=imp[:, jt, g, :], in1=cmul[:, qt, :], op=ALU.mult), [imp, cmul], [sc1])
                    self.V(lambda e: e.tensor_tensor(out=sc1[:], in0=sc1[:], in1=cadd[:, qt, :], op=ALU.add), [sc1, cadd], [sc1])
                    self.V(lambda e: e.max(out=m1[:], in_=sc1[:]), [sc1], [m1])
                    self.V(lambda e: e.match_replace(out=sc2[:], in_to_replace=m1[:], in_values=sc1[:], imm_value=-1e30), [sc1, m1], [sc2])
                    self.V(lambda e: e.max(out=m2[:], in_=sc2[:]), [sc2], [m2])
                    for hf in range(2):
                        self.V(lambda e, hf=hf: e.tensor_scalar(out=sbias[:, hf * 64:(hf + 1) * 64], in0=sc1[:], scalar1=m2[:, 7:8], scalar2=NEGB, op0=ALU.is_lt, op1=ALU.mult), [sc1, m2], [sbias])
                    self.P(lambda e: e.transpose(out=tp[:, jt * 128:(jt + 1) * 128], in_=sbias[:], identity=self.ident_f[:]), [sbias, self.ident_f], [tp])
                for hh in range(4):
                    v_ = hh % 2
                    c_ = (4 * g + hh) // 2
                    hp_ = slice(v_ * 64, v_ * 64 + 64)
                    ot_ = slice((1 - v_) * 64, (2 - v_) * 64)
                    self.A(lambda e, hh=hh, ot_=ot_: e.activation(out=qsel[hh][ot_, :], in_=tp[ot_, :], func=AF.Copy), [tp], [qsel[hh]])
                    self.V(lambda e, hh=hh, hp_=hp_, c_=c_: e.tensor_copy(out=qsel[hh][hp_, :], in_=qrsb[hp_, c_, :]), [qrsb, qsel[hh]], [qsel[hh]])
                for h in range(4 * g, 4 * g + 4):
                    c, hp = h // 2, slice((h % 2) * 64, (h % 2) * 64 + 64)
                    for br in (1, 2):
                        vsb = vs_sb if br == 1 else vw_sb
                        kt_lo = 0 if br == 1 else max(0, 4 * Q - 4)
                        def qk_exp(kt):
                            sc = self.rot(SC)
                            need_band = (br == 2) or (kt >= 4 * Q)
                            if br == 1:
                                qs_ = qsel[h % 4]
                                self.P(lambda e: e.matmul(sc[:, :], lhsT=kcomb[:, h % 2, g, kt * 128:(kt + 1) * 128], rhs=qs_[:, :], start=True, stop=not need_band), [kcomb, qs_], [sc])
                            else:
                                self.P(lambda e: e.matmul(sc[:, :], lhsT=kw_dup[hp, g, kt * 128:(kt + 1) * 128], rhs=qrsb[hp, c, :], start=True, stop=not need_band), [kw_dup, qrsb], [sc])
                            if need_band:
                                o_ = kt - 4 * Q + 4
                                jb = o_ % 4
                                self.P(lambda e: e.matmul(sc[:, jb * 128:(jb + 1) * 128], lhsT=self.ident_b[:], rhs=band[:, o_, jb * 128:(jb + 1) * 128], start=False, stop=True), [self.ident_b, band], [sc])
                            pt = next_pt()
                            self.A(lambda e: e.activation(out=pt[:], in_=sc[:, :], func=AF.Exp, scale=0.125), [sc], [pt])
                            return pt

                        def pv(kt, pt):
                            for jt in range(4):
                                qt = 4 * Q + jt
                                lo = 0 if br == 1 else max(0, qt - 4)
                                if kt < lo or kt > qt:
                                    continue
                                a = acc[jt]
                                self.P(lambda e: e.matmul(a[:, 0:65], lhsT=pt[:, jt * 128:(jt + 1) * 128], rhs=vsb[:, kt, g, :], start=(kt == lo), stop=(kt == qt)), [pt, vsb], [a])
                                if kt == qt:
                                    finish(jt, qt, h, br, False)
                        prev = None
                        for kt in range(kt_lo, 4 * Q + 4):
                            pt = qk_exp(kt)
                            if prev is not None:
                                pv(*prev)
                            prev = (kt, pt)
                        pv(*prev)
            for c in range(4):
                tp = self.rot(MISC)
                for jt in range(4):
                    self.P(lambda e: e.transpose(out=tp[:, jt * 128:(jt + 1) * 128], in_=o_acc[:, jt, c * 128:(c + 1) * 128], identity=self.ident_f[:]), [o_acc, self.ident_f], [tp])
                self.A(lambda e: e.activation(out=oT[:, c, :], in_=tp[:, :], func=AF.Copy), [tp], [oT])
            self.st(d["mixT"].t[0:4, :, tsl].rearrange("c p t -> p c t"), oT[:], [oT], [d["mixT"]], disjoint=True)


Builder.phase_compress = phase_compress
Builder.phase_attn = phase_attn


def phase_xattn(self, li, mem_ap, xT, xres_src, xres_dst, xT_dst, md):
    inp = self.inp
    d_gk, d_dest, d_xg = md["gk"], md["dest"], md["xg"]
    SC, ACC, MISC = [0, 1], [2, 3], [4, 5, 6, 7]
    with self.phase() as es:
        wq = self.sb(es, "xa_wq", [128, 8, D], BF16)
        wo = self.sb(es, "xa_wo", [128, 8, D], BF16)
        kT = self.sb(es, "xa_kT", [128, 8, 256], BF16)
        v_sb = self.sb(es, "xa_v", [128, 2, 4, 257], BF16)
        for kk in range(8):
            self.ldw(wq[:, kk, :], inp["xattn_w_q"][li, kk * 128:(kk + 1) * 128, :], [wq], disjoint=True)
            self.ldw(wo[:, kk, :], inp["xattn_w_o"][li, kk * 128:(kk + 1) * 128, :], [wo], disjoint=True)
        self.V(lambda e: e.memset(v_sb[:], 1.0), [], [v_sb])
        with self.phase() as es2:
            memT = self.sb(es2, "xa_memT", [128, 8, 256], BF16)
            mt = self.sb(es2, "xa_mt", [128, D], F32)
            wkv = self.sb(es2, "xa_wkv", [128, 8, D], BF16)
            for t in range(2):
                self.ld(mt[:], mem_ap[t * 128:(t + 1) * 128, :], [], [mt])
                for half in range(2):
                    tp = self.rot(MISC)
                    for jj in range(4):
                        ch = half * 4 + jj
                        self.P(lambda e: e.transpose(out=tp[:, jj * 128:(jj + 1) * 128], in_=mt[:, ch * 128:(ch + 1) * 128], identity=self.ident_f[:]), [mt, self.ident_f], [tp])
                    self.A(lambda e: e.activation(out=memT[:, half * 4:(half + 1) * 4, t * 128:(t + 1) * 128],
                                                  in_=tp[:, :].rearrange("p (a b) -> p a b", a=4), func=AF.Copy), [tp], [memT])
            for part in range(2):
                for kk in range(8):
                    self.ldw(wkv[:, kk, :], inp["xattn_w_kv"][li, kk * 128:(kk + 1) * 128, part * D:(part + 1) * D], [wkv], disjoint=(kk > 0))
                if part == 0:
                    for c in range(8):
                        ps = self.rot(MISC)
                        self.mm_acc(ps[:, 0:256], ps, [(wkv[:, kk, c * 128:(c + 1) * 128], memT[:, kk, :]) for kk in range(8)], [wkv, memT])
                        self.A(lambda e: e.activation(out=kT[:, c, :], in_=ps[:, 0:256], func=AF.Copy), [ps], [kT])
                else:
                    for kt in range(2):
                        for half in range(2):
                            ps = self.rot(MISC)
                            self.mm_acc(ps[:, :], ps, [(memT[:, kk, kt * 128:(kt + 1) * 128], wkv[:, kk, half * 512:(half + 1) * 512]) for kk in range(8)], [wkv, memT])
                            self.A(lambda e: e.activation(out=v_sb[:, kt, 2 * half:2 * half + 2, 0:256], in_=ps[:, :].rearrange("p (h d) -> p h d", h=2), func=AF.Copy), [ps], [v_sb])
        L = self.ln_setup(es, li, 1, "xa")
        L["stq"] = "pool"
        wr = self.sb(es, "xa_wr", [128, 8, 32], F32)
        self.ld(wr[:], inp["router_w"][li].rearrange("(c p) e -> p c e", p=128), [], [wr])
        br = self.sb(es, "xa_br", [128, 32], F32)
        self.ld(br[:], inp["router_b"][li:li + 1, :].broadcast_to([128, 32]), [], [br])
        lts = self.sb(es, "xa_lts", [128, 128], F32)
        self.ld(lts[:], inp["c_lts"], [], [lts])
        ones = self.sb(es, "xa_ones", [128, 128], F32)
        self.V(lambda e: e.memset(ones[:], 1.0), [], [ones])
        ebase = self.sb(es, "xa_ebase", [128, 32], F32)
        self.ld(ebase[:], inp["c_ebase"], [], [ebase])
        dumpoff = self.sb(es, "xa_dumpoff", [128, 32], F32)
        self.ld(dumpoff[:], inp["c_dumpoff"], [], [dumpoff])
        cnt = self.sb(es, "xa_cnt", [128, 32], F32)
        self.V(lambda e: e.memset(cnt[:], 0.0), [], [cnt])
        xTf = self.sb(es, "xa_xTf", [128, 8, 128], F32)
        lg = self.sb(es, "xa_lg", [128, 32], F32)
        m8 = self.sb(es, "xa_m8", [128, 8], F32)
        nm = self.sb(es, "xa_nm", [128, 1], F32)
        e4 = self.sb(es, "xa_e4", [128, 4], F32)
        sm = self.sb(es, "xa_sm", [128, 1], F32)
        gk = self.sb(es, "xa_gk", [128, 4], F32)
        mk = self.sb(es, "xa_mk", [128, 32], F32)
        rowid = self.sb(es, "xa_rowid", [128, 32], F32)
        ov = self.sb(es, "xa_ov", [128, 32], F32)
        tmp32 = self.sb(es, "xa_tmp32", [128, 32], F32)
        destf = self.sb(es, "xa_destf", [128, 4], F32)
        destu = [self.sb(es, "xa_destu%d" % i, [128, 4], U32) for i in range(2)]

        def router_hook(ti, pts, xn):
            du = destu[ti % 2]
            for half in range(2):
                self.A(lambda e: e.activation(out=xTf[:, half * 4:(half + 1) * 4, :], in_=pts[half][:, :].rearrange("p (a b) -> p a b", a=4), func=AF.Copy), [pts[half]], [xTf])
            ps = self.rot(MISC)
            self.mm_acc(ps[:, 0:32], ps, [(xTf[:, c, :], wr[:, c, :]) for c in range(8)], [xTf, wr])
            self.V(lambda e: e.tensor_tensor(out=lg[:], in0=ps[:, 0:32], in1=br[:], op=ALU.add), [ps, br], [lg])
            self.V(lambda e: e.max(out=m8[:], in_=lg[:]), [lg], [m8])
            self.V(lambda e: e.tensor_scalar(out=nm[:], in0=m8[:, 0:1], scalar1=-1.0, scalar2=None, op0=ALU.mult), [m8], [nm])
            self.A(lambda e: e.activation(out=e4[:], in_=m8[:, 0:4], func=AF.Exp, bias=nm[:, 0:1], scale=1.0), [m8, nm], [e4])
            self.V(lambda e: e.reduce_sum(out=sm[:], in_=e4[:], axis=mybir.AxisListType.X), [e4], [sm])
            self.V(lambda e: e.reciprocal(out=sm[:], in_=sm[:]), [sm], [sm])
            self.V(lambda e: e.tensor_scalar(out=gk[:], in0=e4[:], scalar1=sm[:, 0:1], scalar2=None, op0=ALU.mult), [e4, sm], [gk])
            self.c.dma("pool", d_gk.t[ti * 128:(ti + 1) * 128, :], gk[:], [gk], [d_gk], disjoint=True)
            self.V(lambda e: e.tensor_scalar(out=mk[:], in0=lg[:], scalar1=m8[:, 3:4], scalar2=None, op0=ALU.is_ge), [lg, m8], [mk])
            pw = self.rot(MISC)
            self.P(lambda e: e.matmul(pw[:, 0:32], lhsT=lts[:], rhs=mk[:], start=True, stop=True), [lts, mk], [pw])
            self.P(lambda e: e.matmul(pw[:, 32:64], lhsT=ones[:], rhs=mk[:], start=True, stop=True), [ones, mk], [pw])
            self.V(lambda e: e.tensor_tensor(out=rowid[:], in0=pw[:, 0:32], in1=cnt[:], op=ALU.add), [pw, cnt], [rowid])
            self.V(lambda e: e.tensor_tensor(out=cnt[:], in0=pw[:, 32:64], in1=cnt[:], op=ALU.add), [pw, cnt], [cnt])
            self.V(lambda e: e.scalar_tensor_tensor(out=ov[:], in0=rowid[:], scalar=float(CAP), in1=dumpoff[:], op0=ALU.is_ge, op1=ALU.mult), [rowid, dumpoff], [ov])
            self.V(lambda e: e.tensor_tensor(out=rowid[:], in0=rowid[:], in1=ebase[:], op=ALU.add), [rowid, ebase], [rowid])
            self.V(lambda e: e.tensor_tensor(out=rowid[:], in0=rowid[:], in1=ov[:], op=ALU.add), [rowid, ov], [rowid])
            for k in range(4):
                self.V(lambda e: e.scalar_tensor_tensor(out=tmp32[:], in0=lg[:], scalar=m8[:, k:k + 1], in1=rowid[:], op0=ALU.is_equal, op1=ALU.mult), [lg, m8, rowid], [tmp32])
                self.V(lambda e: e.reduce_sum(out=destf[:, k:k + 1], in_=tmp32[:], axis=mybir.AxisListType.X), [tmp32], [destf])
            self.V(lambda e: e.tensor_copy(out=du[:], in_=destf[:]), [destf], [du])
            self.c.dma("pool", d_dest.t[ti * 128:(ti + 1) * 128, :], du[:], [du], [d_dest], disjoint=True)
            for k in range(4):
                self.c.idma(d_xg.t, bass.IndirectOffsetOnAxis(du[:, k:k + 1], 0), xn[:], None, [xn, du], [d_xg], disjoint=True)

        xs = [self.sb(es, "xa_x%d" % i, [128, 8, 512], BF16) for i in range(2)]
        qT = self.sb(es, "xa_qT", [128, 8, 512], BF16)
        o_sb = self.sb(es, "xa_o", [128, 4, D], F32)
        pts_ = [self.sb(es, "xa_pt%d" % i, [128, 512], BF16) for i in range(4)]
        pti = [0]
        rz2 = [self.sb(es, "xa_rz%d" % i, [128, 1], F32) for i in range(2)]
        oT = [self.sb(es, "xa_oT%d" % i, [128, 8, 128], BF16) for i in range(2)]
        pend = [None]

        def flush():
            if pend[0] is not None:
                pend[0]()
                pend[0] = None
        for Q in range(NG):
            x = xs[Q % 2]
            tsl = slice(Q * 512, (Q + 1) * 512)
            if Q == 0:
                self.ld(x[:], xT.t[:, :, tsl].rearrange("c p t -> p c t"), [xT], [x])
            if Q + 1 < NG:
                self.ld(xs[(Q + 1) % 2][:], xT.t[:, :, (Q + 1) * 512:(Q + 2) * 512].rearrange("c p t -> p c t"), [xT], [xs[(Q + 1) % 2]])
            for c in range(8):
                ps = self.rot(MISC)
                self.mm_acc(ps[:, :], ps, [(wq[:, kk, c * 128:(c + 1) * 128], x[:, kk, :]) for kk in range(8)], [wq, x])
                self.A(lambda e: e.activation(out=qT[:, c, :], in_=ps[:, :], func=AF.Copy), [ps], [qT])
                if c == 3:
                    flush()
            def xa_qk(h):
                pk = []
                for kt in range(2):
                    sc = self.rot(SC)
                    self.mm_acc(sc[:, :], sc, [(kT[:, 2 * h + cc, kt * 128:(kt + 1) * 128], qT[:, 2 * h + cc, :]) for cc in range(2)], [kT, qT])
                    pt = pts_[pti[0] % 4]
                    pti[0] += 1
                    self.A(lambda e: e.activation(out=pt[:], in_=sc[:, :], func=AF.Exp, scale=1.0 / 16.0), [sc], [pt])
                    pk.append(pt)
                return pk

            def xa_pv(h, pk):
                for jt in range(4):
                    a = self.rot(ACC)
                    self.mm_acc(a[:, 0:257], a, [(pk[kt][:, jt * 128:(jt + 1) * 128], v_sb[:, kt, h, :]) for kt in range(2)], pk + [v_sb])
                    rzj = rz2[jt % 2]
                    self.V(lambda e: e.reciprocal(out=rzj[:], in_=a[:, 256:257]), [a], [rzj])
                    if jt % 2 == 0:
                        self.V(lambda e: e.tensor_scalar(out=o_sb[:, jt, h * 256:(h + 1) * 256], in0=a[:, 0:256], scalar1=rzj[:, 0:1], scalar2=None, op0=ALU.mult), [a, rzj], [o_sb])
                    else:
                        self.A(lambda e: e.activation(out=o_sb[:, jt, h * 256:(h + 1) * 256], in_=a[:, 0:256], func=AF.Identity, scale=rzj[:, 0:1]), [a, rzj], [o_sb])
            prev = None
            for h in range(4):
                pk = xa_qk(h)
                if prev is not None:
                    xa_pv(*prev)
                prev = (h, pk)
            xa_pv(*prev)
            def xa_pre(jt):
                ti = Q * 4 + jt
                ot = oT[ti % 2]
                xr = self.ln_preload(L, ti, xres_src)
                for half in range(2):
                    tp = self.rot(MISC)
                    for jj in range(4):
                        ch = half * 4 + jj
                        self.P(lambda e: e.transpose(out=tp[:, jj * 128:(jj + 1) * 128], in_=o_sb[:, jt, ch * 128:(ch + 1) * 128], identity=self.ident_f[:]), [o_sb, self.ident_f], [tp])
                    self.A(lambda e: e.activation(out=ot[:, half * 4:(half + 1) * 4, :], in_=tp[:, :].rearrange("p (a b) -> p a b", a=4), func=AF.Copy), [tp], [ot])
                ps = [self.psb[0], self.psb[1]] if jt % 2 == 0 else [self.psb[2], self.psb[3]]
                for hh in range(2):
                    self.mm_acc(ps[hh][:, :], ps[hh], [(ot[:, kk, :], wo[:, kk, hh * 512:(hh + 1) * 512]) for kk in range(8)], [ot, wo])
                return ps, xr
            nxt = xa_pre(0)
            for jt in range(4):
                ps, xr = nxt
                pb = self.ln_tile(L, Q * 4 + jt, [ps[0][:, :], ps[1][:, :]], ps, xres_src, xres_dst, xT_dst, hook=router_hook, psum_sub=MISC, xr_pre=xr, defer=True)
                flush()
                pend[0] = pb
                if jt + 1 < 4:
                    nxt = xa_pre(jt + 1)
        flush()


def phase_moe(self, li, xT, gates_d, xres_src, xres_dst, xT_dst, final_out=None):
    inp = self.inp
    with self.phase() as es:
        L = self.ln_setup(es, li, 2, "mo")
        bgu = self.sb(es, "mo_bgu", [128, 32, 16], F32)
        self.ld(bgu[:], inp["bguT"][li], [], [bgu])
        bdn = self.sb(es, "mo_bdn", [32, D], F32)
        self.ld(bdn[:], inp["expert_b_dn"][li], [], [bdn])
        x = self.sb(es, "mo_x", [128, 8, 1024], BF16)
        gts = self.sb(es, "mo_g", [128, 8, 32], F32)
        gT = self.sb(es, "mo_gT", [32, 128], F32)
        acc = self.sb(es, "mo_acc", [128, 8, D], F32)
        wgu = self.sb(es, "mo_wgu", [128, 8, 2 * D], BF16)
        wdn = self.sb(es, "mo_wdn", [128, 8, D], BF16)
        act = self.sb(es, "mo_act", [128, 8, 512], BF16)
        g_sb = [self.sb(es, "mo_gs%d" % i, [128, 512], F32) for i in range(2)]
        s_sb = [self.sb(es, "mo_ss%d" % i, [128, 512], F32) for i in range(2)]
        u_sb = [self.sb(es, "mo_us%d" % i, [128, 512], F32) for i in range(2)]
        for SG in range(4):
            self.ld(x[:], xT.t[:, :, SG * 1024:(SG + 1) * 1024].rearrange("c p t -> p c t"), [xT], [x])
            self.ld(gts[:], gates_d.t[SG * 1024:(SG + 1) * 1024, :].rearrange("(t p) e -> p t e", p=128), [gates_d], [gts])
            for t in range(8):
                tp = self.rot()
                self.P(lambda e: e.transpose(out=tp[0:32, 0:128], in_=gts[:, t, :], identity=self.ident_f[:]), [gts, self.ident_f], [tp])
                self.A(lambda e: e.activation(out=gT[:], in_=tp[0:32, 0:128], func=AF.Copy), [tp], [gT])
                for half in range(2):
                    ps = self.rot()
                    self.P(lambda e: e.matmul(ps[:, :], lhsT=gT[:], rhs=bdn[:, half * 512:(half + 1) * 512], start=True, stop=True), [gT, bdn], [ps])
                    self.A(lambda e: e.activation(out=acc[:, t, half * 512:(half + 1) * 512], in_=ps[:, :], func=AF.Copy), [ps], [acc])
            for ex in range(32):
                for kk in range(8):
                    self.ldw(wgu[:, kk, :], inp["expert_w_gu"][li, ex, kk * 128:(kk + 1) * 128, :], [wgu], disjoint=(kk > 0))
                for kk in range(8):
                    self.ldw(wdn[:, kk, :], inp["expert_w_dn"][li, ex, kk * 128:(kk + 1) * 128, :], [wdn], disjoint=(kk > 0))
                for tg in range(2):
                    tsl = slice(tg * 512, (tg + 1) * 512)
                    for jp in range(8):
                        k2 = jp % 2
                        gs, ss, us = g_sb[k2], s_sb[k2], u_sb[k2]
                        hg = self.rot()
                        self.mm_acc(hg[:, :], hg, [(wgu[:, kk, jp * 128:(jp + 1) * 128], x[:, kk, tsl]) for kk in range(8)], [wgu, x])
                        hu = self.rot()
                        self.mm_acc(hu[:, :], hu, [(wgu[:, kk, D + jp * 128:D + (jp + 1) * 128], x[:, kk, tsl]) for kk in range(8)], [wgu, x])
                        self.V(lambda e: e.tensor_scalar(out=gs[:], in0=hg[:, :], scalar1=bgu[:, ex, jp:jp + 1], scalar2=7.0, op0=ALU.add, op1=ALU.min), [hg, bgu], [gs])
                        self.A(lambda e: e.activation(out=ss[:], in_=gs[:], func=AF.Sigmoid, scale=1.702), [gs], [ss])
                        self.V(lambda e: e.tensor_scalar(out=us[:], in0=hu[:, :], scalar1=bgu[:, ex, 8 + jp:9 + jp], scalar2=7.0, op0=ALU.add, op1=ALU.min), [hu, bgu], [us])
                        self.V(lambda e: e.tensor_scalar(out=us[:], in0=us[:], scalar1=-7.0, scalar2=1.0, op0=ALU.max, op1=ALU.add), [us], [us])
                        self.V(lambda e: e.tensor_tensor(out=gs[:], in0=gs[:], in1=ss[:], op=ALU.mult), [gs, ss], [gs])
                        self.V(lambda e: e.tensor_tensor(out=act[:, jp, :], in0=us[:], in1=gs[:], op=ALU.mult), [us, gs], [act])
                    for t in range(4):
                        tt = tg * 4 + t
                        for half in range(2):
                            y = self.rot()
                            self.mm_acc(y[:, :], y, [(act[:, jf, t * 128:(t + 1) * 128], wdn[:, jf, half * 512:(half + 1) * 512]) for jf in range(8)], [act, wdn])
                            asl = acc[:, tt, half * 512:(half + 1) * 512]
                            self.V(lambda e: e.scalar_tensor_tensor(out=asl, in0=y[:, :], scalar=gts[:, tt, ex:ex + 1], in1=asl, op0=ALU.mult, op1=ALU.add), [y, gts, acc], [acc])
            for t in range(8):
                self.ln_tile(L, SG * 8 + t, [acc[:, t, 0:512], acc[:, t, 512:1024]], [acc], xres_src, xres_dst, xT_dst, final_out=final_out)


Builder.phase_xattn = phase_xattn
Builder.phase_moe = phase_moe


def phase_moe_sparse(self, li, md, xres_src, xres_dst, xT_dst, final_out=None):
    inp = self.inp
    d_gk, d_dest, d_xg, d_yg = md["gk"], md["dest"], md["xg"], md["yg"]
    NTL = CAP // 128
    GROUPS = [(g0, min(512, CAP - g0)) for g0 in range(0, CAP, 512)]
    with self.phase() as es:
        with self.phase() as es1:
            bgu = self.sb(es1, "ms_bgu", [128, 32, 16], F32)
            self.ld(bgu[:], inp["bguT"][li], [], [bgu])
            wgu = [self.sb(es1, "ms_wgu%d" % i, [128, 8, 2 * D], BF16) for i in range(2)]
            wdn = [self.sb(es1, "ms_wdn%d" % i, [128, 8, D], BF16) for i in range(2)]
            bdn = [self.sb(es1, "ms_bdn%d" % i, [128, D], F32) for i in range(2)]
            xr = [self.sb(es1, "ms_xr%d" % i, [128, D], F32) for i in range(NTL)]

            def prefetch_rows(ex_):
                for t_ in range(NTL):
                    r__ = xr[t_]
                    self.ld(r__[:], d_xg.t[ex_ * CAP + t_ * 128:ex_ * CAP + (t_ + 1) * 128, :], [d_xg], [r__])
            xgT = self.sb(es1, "ms_xgT", [128, 8, CAP], BF16)
            act = self.sb(es1, "ms_act", [128, 8, 512], BF16)
            g_sb = [self.sb(es1, "ms_gs%d" % i, [128, 512], F32) for i in range(2)]
            s_sb = [self.sb(es1, "ms_ss%d" % i, [128, 512], F32) for i in range(2)]
            u_sb = [self.sb(es1, "ms_us%d" % i, [128, 512], F32) for i in range(2)]
            y_sb = [self.sb(es1, "ms_y%d" % i, [128, D], F32) for i in range(2)]
            yi = [0]
            zt = y_sb[0]
            self.V(lambda e: e.memset(zt[:], 0.0), [], [zt])
            for t_ in range(NTL):
                self.st(d_yg.t[32 * CAP + t_ * 128:32 * CAP + (t_ + 1) * 128, :], zt[:], [zt], [d_yg], disjoint=True)
            act2 = [act, self.sb(es1, "ms_act2", [128, 8, 512], BF16)]

            def st_load_w(ex):
                wg, wd, bd = wgu[ex % 2], wdn[ex % 2], bdn[ex % 2]
                for kk in range(8):
                    self.ldw(wg[:, kk, :], inp["expert_w_gu"][li, ex, kk * 128:(kk + 1) * 128, :], [wg], disjoint=(kk > 0))
                for kk in range(8):
                    self.ldw(wd[:, kk, :], inp["expert_w_dn"][li, ex, kk * 128:(kk + 1) * 128, :], [wd], disjoint=(kk > 0))
                self.ld(bd[:], inp["expert_b_dn"][li, ex:ex + 1, :].broadcast_to([128, D]), [], [bd])

            def st_T(ex):
                for t in range(NTL):
                    r_ = xr[t]
                    for half in range(2):
                        tp = self.rot()
                        for jj in range(4):
                            ch = half * 4 + jj
                            self.P(lambda e: e.transpose(out=tp[:, jj * 128:(jj + 1) * 128], in_=r_[:, ch * 128:(ch + 1) * 128], identity=self.ident_f[:]), [r_, self.ident_f], [tp])
                        self.A(lambda e: e.activation(out=xgT[:, half * 4:(half + 1) * 4, t * 128:(t + 1) * 128], in_=tp[:, :].rearrange("p (a b) -> p a b", a=4), func=AF.Copy), [tp], [xgT])
                if ex + 1 < 32:
                    prefetch_rows(ex + 1)

            def st_GU(ex, gi):
                wg = wgu[ex % 2]
                g0, gn = GROUPS[gi]
                a_ = act2[gi % 2]
                tsl = slice(g0, g0 + gn)
                for jp in range(8):
                    k2 = jp % 2
                    gs, ss, us = g_sb[k2], s_sb[k2], u_sb[k2]
                    hg = self.rot()
                    self.mm_acc(hg[:, 0:gn], hg, [(wg[:, kk, jp * 128:(jp + 1) * 128], xgT[:, kk, tsl]) for kk in range(8)], [wg, xgT])
                    hu = self.rot()
                    self.mm_acc(hu[:, 0:gn], hu, [(wg[:, kk, D + jp * 128:D + (jp + 1) * 128], xgT[:, kk, tsl]) for kk in range(8)], [wg, xgT])
                    self.V(lambda e: e.tensor_scalar(out=gs[:, 0:gn], in0=hg[:, 0:gn], scalar1=bgu[:, ex, jp:jp + 1], scalar2=7.0, op0=ALU.add, op1=ALU.min), [hg, bgu], [gs])
                    self.A(lambda e: e.activation(out=ss[:, 0:gn], in_=gs[:, 0:gn], func=AF.Sigmoid, scale=1.702), [gs], [ss])
                    self.V(lambda e: e.tensor_scalar(out=us[:, 0:gn], in0=hu[:, 0:gn], scalar1=bgu[:, ex, 8 + jp:9 + jp], scalar2=7.0, op0=ALU.add, op1=ALU.min), [hu, bgu], [us])
                    self.V(lambda e: e.tensor_scalar(out=us[:, 0:gn], in0=us[:, 0:gn], scalar1=-7.0, scalar2=1.0, op0=ALU.max, op1=ALU.add), [us], [us])
                    self.V(lambda e: e.tensor_tensor(out=gs[:, 0:gn], in0=gs[:, 0:gn], in1=ss[:, 0:gn], op=ALU.mult), [gs, ss], [gs])
                    self.V(lambda e: e.tensor_tensor(out=a_[:, jp, 0:gn], in0=us[:, 0:gn], in1=gs[:, 0:gn], op=ALU.mult), [us, gs], [a_])

            def st_DN(ex, gi):
                wd, bd = wdn[ex % 2], bdn[ex % 2]
                g0, gn = GROUPS[gi]
                a_ = act2[gi % 2]
                for t in range(gn // 128):
                    ys = y_sb[yi[0] % 2]
                    yi[0] += 1
                    for half in range(2):
                        y = self.rot()
                        self.mm_acc(y[:, :], y, [(a_[:, jf, t * 128:(t + 1) * 128], wd[:, jf, half * 512:(half + 1) * 512]) for jf in range(8)], [a_, wd])
                        self.V(lambda e: e.tensor_tensor(out=ys[:, half * 512:(half + 1) * 512], in0=y[:, :], in1=bd[:, half * 512:(half + 1) * 512], op=ALU.add), [y, bd], [ys])
                    r0 = ex * CAP + g0 + t * 128
                    self.st(d_yg.t[r0:r0 + 128, :], ys[:], [ys], [d_yg], disjoint=True)

            NGp = len(GROUPS)
            prefetch_rows(0)
            st_load_w(0)
            st_T(0)
            st_GU(0, 0)
            for ex in range(32):
                if ex + 1 < 32:
                    st_load_w(ex + 1)
                for gi in range(NGp):
                    if gi + 1 < NGp:
                        st_GU(ex, gi + 1)
                    elif ex + 1 < 32:
                        st_T(ex + 1)
                        st_GU(ex + 1, 0)
                    st_DN(ex, gi)
        L = self.ln_setup(es, li, 2, "ms")
        du = [self.sb(es, "ms_du%d" % i, [128, 4], U32) for i in range(2)]
        gk = [self.sb(es, "ms_gk%d" % i, [128, 4], F32) for i in range(2)]
        yk = [[self.sb(es, "ms_yk%d_%d" % (i, k), [128, D], F32) for k in range(4)] for i in range(2)]
        accs = [self.sb(es, "ms_acc%d" % i, [128, D], F32) for i in range(2)]
        def cb_pre(ti):
            b2 = ti % 2
            self.ld(du[b2][:], d_dest.t[ti * 128:(ti + 1) * 128, :], [d_dest], [du[b2]])
            self.ld(gk[b2][:], d_gk.t[ti * 128:(ti + 1) * 128, :], [d_gk], [gk[b2]])
            for k in range(4):
                self.c.idma(yk[b2][k][:], None, d_yg.t, bass.IndirectOffsetOnAxis(du[b2][:, k:k + 1], 0), [d_yg, du[b2]], [yk[b2][k]])
            return self.ln_preload(L, ti, xres_src)
        nxt = cb_pre(0)
        for ti in range(NT):
            b2 = ti % 2
            xr = nxt
            if ti + 1 < NT:
                nxt = cb_pre(ti + 1)
            acc = accs[b2]
            self.V(lambda e: e.tensor_scalar(out=acc[:], in0=yk[b2][0][:], scalar1=gk[b2][:, 0:1], scalar2=None, op0=ALU.mult), [yk[b2][0], gk[b2]], [acc])
            for k in range(1, 4):
                self.V(lambda e: e.scalar_tensor_tensor(out=acc[:], in0=yk[b2][k][:], scalar=gk[b2][:, k:k + 1], in1=acc[:], op0=ALU.mult, op1=ALU.add), [yk[b2][k], gk[b2], acc], [acc])
            self.ln_tile(L, ti, [acc[:, 0:512], acc[:, 512:1024]], [acc], xres_src, xres_dst, xT_dst, final_out=final_out, xr_pre=xr)


Builder.phase_moe_sparse = phase_moe_sparse


def declare_scratch(b):
    d = {}
    for nm in ("q", "qr"):
        d[nm] = b.dscratch("s_" + nm, [4, 128, S], BF16)
    for nm in ("kc", "vc", "ks", "kw"):
        d[nm] = b.dscratch("s_" + nm, [128, S], BF16)
    for nm in ("vs", "vw"):
        d[nm] = b.dscratch("s_" + nm, [S, 2, 65], BF16)
    d["gates"] = b.dscratch("s_gates", [S, 24], F32)
    d["mixT"] = b.dscratch("s_mixT", [8, 128, S], BF16)
    d["kcd"] = b.dscratch("s_kcd", [128, 2, 256], BF16)
    d["vcm"] = b.dscratch("s_vcm", [128, 2, 2, 129], BF16)
    d["R0"] = b.dscratch("s_R0", [S, D], F32)
    d["R1"] = b.dscratch("s_R1", [S, D], F32)
    d["T0"] = b.dscratch("s_T0", [8, 128, S], BF16)
    d["T1"] = b.dscratch("s_T1", [8, 128, S], BF16)
    d["md"] = dict(gk=b.dscratch("s_gk", [S, 4], F32), dest=b.dscratch("s_dest", [S, 4], U32),
                   xg=b.dscratch("s_xg", [33 * CAP, D], F32), yg=b.dscratch("s_yg", [33 * CAP, D], F32))
    return d


def build_program(dbg=None, upto=99):
    nc = bass.Bass("TRN2", target_bir_lowering=False)
    b = Builder(nc, dbg=dbg)
    d = declare_scratch(b)
    x_in = T(b.inp["x"], "x_in")
    out = T(nc.dram_tensor("out", [S, D], F32, kind="ExternalOutput").ap(), "out")
    steps = []
    with ExitStack() as es:
        b.init_psum(es)
        b.load_consts(es)
        steps = [
            lambda: b.phase_t0(x_in, d["T0"]),
            lambda: b.phase_even_proj(0, d["T0"], b.inp["pos"], d),
            lambda: b.phase_compress(0, d),
            lambda: b.phase_attn(0, d),
            lambda: b.phase_outproj_ln(d["mixT"], b.inp["w_out_even"][0], 0, 0, x_in, d["R0"], d["T1"]),
            lambda: b.phase_xattn(0, b.inp["mem"], d["T1"], d["R0"], d["R1"], d["T0"], d["md"]),
            lambda: b.phase_moe_sparse(0, d["md"], d["R1"], d["R0"], d["T1"]),
            lambda: b.phase_odd_proj(0, d["T1"], d),
            lambda: b.phase_outproj_ln(d["mixT"], b.inp["w_out_odd"][0], 1, 0, d["R0"], d["R1"], d["T0"]),
            lambda: b.phase_xattn(1, b.inp["mem"], d["T0"], d["R1"], d["R0"], d["T1"], d["md"]),
            lambda: b.phase_moe_sparse(1, d["md"], d["R0"], None, None, final_out=out),
        ]
        for i, s_ in enumerate(steps):
            if i < upto:
                s_()
        allr = [v for v in d.values() if isinstance(v, T)] + list(d["md"].values()) + [out]
        b.c.wait_all("sp", allr)
    return nc, b


def phase_odd_proj(self, j, xT, d):
    inp = self.inp
    W = inp["w_in_odd"][j]
    with self.phase() as es:
        wfm = self.sb(es, "od_wfm", [128, 8, 2560], BF16)
        wtm = self.sb(es, "od_wtm", [128, 8, 1024], BF16)
        for kk in range(8):
            rs_ = slice(kk * 128, (kk + 1) * 128)
            self.ldw(wfm[:, kk, 0:1024], W[rs_, 0:1024], [wfm], disjoint=True)
            self.ldw(wfm[:, kk, 1024:2560], W[rs_, 2048:3584], [wfm], disjoint=True)
            self.ldw(wtm[:, kk, :], W[rs_, 1024:2048], [wtm], disjoint=True)
        lbl = self.sb(es, "od_lbl", [128, 2, 4], F32)
        lb = self.sb(es, "od_lb", [128, 4], F32)
        oml = self.sb(es, "od_oml", [128, 4], F32)
        self.ld(lbl[:], inp["hgrn_lbT"], [], [lbl])
        self.V(lambda e: e.tensor_tensor(out=lb[:], in0=lbl[:, 1, :], in1=lbl[:, 0, :], op=ALU.subtract), [lbl], [lb])
        self.A(lambda e: e.activation(out=lb[:], in_=lb[:], func=AF.Sigmoid), [lb], [lb])
        self.V(lambda e: e.tensor_scalar(out=oml[:], in0=lb[:], scalar1=-1.0, scalar2=1.0, op0=ALU.mult, op1=ALU.add), [lb], [oml])
        ng = self.sb(es, "od_ng", [128, 512], F32)
        self.ld(ng[:], inp["hgrn_norm_g"][j:j + 1].rearrange("o h v -> o (h v)").broadcast_to([128, 512]), [], [ng])
        cw = self.sb(es, "od_cw", [128, 4, 3], F32)
        cbv = self.sb(es, "od_cb", [128, 4], F32)
        self.ld(cw[:], inp["conv_wT"][j], [], [cw])
        self.ld(cbv[:], inp["conv_bT"][j], [], [cbv])
        tri = self.sb(es, "od_tri", [64, 64], F32)
        self.ld(tri[:], inp["c_tri64"], [], [tri])
        rmask = self.sb(es, "od_rmask", [128, 512], F32)
        self.V(lambda e: e.memset(rmask[:], 1.0), [], [rmask])
        self.V(lambda e: e.memset(rmask[:].rearrange("p (a b) -> p a b", b=64)[:, :, 0:1], 0.0), [], [rmask])
        zbuf = self.sb(es, "od_z", [128, 4, 514], F32)
        self.V(lambda e: e.memset(zbuf[:], 0.0), [], [zbuf])
        St = self.sb(es, "od_S", [128, 4, 128], F32)
        Sb = self.sb(es, "od_Sb", [128, 4, 128], BF16)
        self.V(lambda e: e.memset(St[:], 0.0), [], [St])
        self.V(lambda e: e.memset(Sb[:], 0.0), [], [Sb])
        xs = [self.sb(es, "od_x%d" % i, [128, 8, 512], BF16) for i in range(2)]
        sg = self.sb(es, "od_sg", [128, 512], F32)
        lf = self.sb(es, "od_lf", [128, 512], F32)
        kf = self.sb(es, "od_kf", [128, 512], F32)
        bt = self.sb(es, "od_b", [128, 512], F32)
        eb = self.sb(es, "od_eb", [128, 512], F32)
        enb = self.sb(es, "od_enb", [128, 512], F32)
        brel = self.sb(es, "od_brel", [128, 512], F32)
        ebl = self.sb(es, "od_ebl", [128, 4, 8], F32)
        A_sb = self.sb(es, "od_A", [128, 4, 512], BF16)
        Bm_sb = self.sb(es, "od_Bm", [128, 4, 512], BF16)
        Kt_sb = self.sb(es, "od_Kt", [128, 4, 512], F32)
        c_sb = self.sb(es, "od_c", [128, 512], F32)
        y_sb = self.sb(es, "od_y", [128, 512], F32)
        od_sb = self.sb(es, "od_od", [128, 4, 512], BF16)
        v_sb = self.sb(es, "od_v", [64, 8, 512], BF16)
        gsn = self.sb(es, "od_gsn", [64, 8, 512], F32)
        atm = self.sb(es, "od_atm", [64, 64], BF16)
        KtT = self.sb(es, "od_KtT", [64, 128], BF16)
        junk = self.sb(es, "od_junk", [64, 128], F32)
        ss = self.sb(es, "od_ss", [64, 1], F32)
        oc = self.sb(es, "od_oc", [64, 8, 512], F32)
        ocT = self.sb(es, "od_ocT", [128, 4, 512], BF16)
        for Q in range(NG):
            x = xs[Q % 2]
            tsl = slice(Q * 512, (Q + 1) * 512)
            if Q == 0:
                self.ld(x[:], xT.t[:, :, tsl].rearrange("c p t -> p c t"), [xT], [x])
            if Q + 1 < NG:
                self.ld(xs[(Q + 1) % 2][:], xT.t[:, :, (Q + 1) * 512:(Q + 2) * 512].rearrange("c p t -> p c t"), [xT], [xs[(Q + 1) % 2]])

            def fm(col0):
                ps = self.rot()
                self.mm_acc(ps[:, :], ps, [(wfm[:, kk, col0:col0 + 128], x[:, kk, :]) for kk in range(8)], [wfm, x])
                return ps
            for hd in range(4):
                psf = fm(512 + hd * 128)
                self.A(lambda e: e.activation(out=sg[:], in_=psf[:, :], func=AF.Sigmoid), [psf], [sg])
                self.V(lambda e: e.tensor_scalar(out=sg[:], in0=sg[:], scalar1=oml[:, hd:hd + 1], scalar2=lb[:, hd:hd + 1], op0=ALU.mult, op1=ALU.add), [sg, oml, lb], [sg])
                self.A(lambda e: e.activation(out=lf[:], in_=sg[:], func=AF.Ln), [sg], [lf])
                self.V(lambda e: e.tensor_scalar(out=kf[:], in0=sg[:], scalar1=-1.0, scalar2=1.0, op0=ALU.mult, op1=ALU.add), [sg], [kf])
                self.V(lambda e: e.tensor_tensor_scan(out=bt[:], data0=rmask[:], data1=lf[:], initial=0.0, op0=ALU.mult, op1=ALU.add), [rmask, lf], [bt])
                self.A(lambda e: e.activation(out=eb[:], in_=bt[:], func=AF.Exp), [bt], [eb])
                self.A(lambda e: e.activation(out=enb[:], in_=bt[:], func=AF.Exp, scale=-1.0), [bt], [enb])
                bv = bt[:].rearrange("p (a b) -> p a b", b=64)
                self.V(lambda e: e.tensor_tensor(out=brel[:].rearrange("p (a b) -> p a b", b=64), in0=bv[:, :, 63:64].broadcast_to([128, 8, 64]), in1=bv, op=ALU.subtract), [bt], [brel])
                self.A(lambda e: e.activation(out=brel[:], in_=brel[:], func=AF.Exp), [brel], [brel])
                self.A(lambda e: e.activation(out=ebl[:, hd, :], in_=bv[:, :, 63], func=AF.Exp), [bt], [ebl])
                self.V(lambda e: e.tensor_tensor(out=Bm_sb[:, hd, :], in0=kf[:], in1=enb[:], op=ALU.mult), [kf, enb], [Bm_sb])
                self.V(lambda e: e.tensor_tensor(out=Kt_sb[:, hd, :], in0=kf[:], in1=brel[:], op=ALU.mult), [kf, brel], [Kt_sb])
                psq = fm(hd * 128)
                self.V(lambda e: e.tensor_tensor(out=A_sb[:, hd, :], in0=psq[:, :], in1=eb[:], op=ALU.mult), [psq, eb], [A_sb])
            for cc in range(4):
                psc = fm(1024 + 1024 + cc * 128)
                self.A(lambda e: e.activation(out=c_sb[:], in_=psc[:, :], func=AF.Copy), [psc], [c_sb])
                psh = fm(1024 + cc * 128)
                self.V(lambda e: e.tensor_tensor(out=zbuf[:, cc, 2:514], in0=psh[:, :], in1=c_sb[:], op=ALU.mult), [psh, c_sb], [zbuf])
                self.V(lambda e: e.tensor_scalar(out=y_sb[:], in0=zbuf[:, cc, 2:514], scalar1=cw[:, cc, 2:3], scalar2=cbv[:, cc:cc + 1], op0=ALU.mult, op1=ALU.add), [zbuf, cw, cbv], [y_sb])
                self.V(lambda e: e.scalar_tensor_tensor(out=y_sb[:], in0=zbuf[:, cc, 1:513], scalar=cw[:, cc, 1:2], in1=y_sb[:], op0=ALU.mult, op1=ALU.add), [zbuf, cw, y_sb], [y_sb])
                self.V(lambda e: e.scalar_tensor_tensor(out=y_sb[:], in0=zbuf[:, cc, 0:512], scalar=cw[:, cc, 0:1], in1=y_sb[:], op0=ALU.mult, op1=ALU.add), [zbuf, cw, y_sb], [y_sb])
                psb_ = fm(1024 + 512 + cc * 128)
                self.V(lambda e: e.tensor_tensor(out=od_sb[:, cc, :], in0=psb_[:, :], in1=y_sb[:], op=ALU.mult), [psb_, y_sb], [od_sb])
                self.V(lambda e: e.tensor_copy(out=zbuf[:, cc, 0:2], in_=zbuf[:, cc, 512:514]), [zbuf], [zbuf])
            self.st(d["mixT"].t[4:8, :, tsl].rearrange("c p t -> p c t"), od_sb[:], [od_sb], [d["mixT"]], disjoint=True)
            for ck in range(8):
                xl = lambda kk: x[:, kk, ck * 64:(ck + 1) * 64]
                ps = self.rot()
                self.mm_acc(ps[0:64, :], ps, [(xl(kk), wtm[:, kk, 0:512]) for kk in range(8)], [wtm, x])
                self.A(lambda e: e.activation(out=v_sb[:, ck, :], in_=ps[0:64, :], func=AF.Copy), [ps], [v_sb])
                ps = self.rot()
                self.mm_acc(ps[0:64, :], ps, [(xl(kk), wtm[:, kk, 512:1024]) for kk in range(8)], [wtm, x])
                self.A(lambda e: e.activation(out=gsn[:, ck, :], in_=ps[0:64, :], func=AF.Silu), [ps], [gsn])
                self.V(lambda e: e.tensor_tensor(out=gsn[:, ck, :], in0=gsn[:, ck, :], in1=ng[0:64, :], op=ALU.mult), [gsn, ng], [gsn])
            for ck in range(8):
                csl = slice(ck * 64, (ck + 1) * 64)
                for hd in range(4):
                    vsl = v_sb[:, ck, hd * 128:(hd + 1) * 128]
                    at = self.rot()
                    self.P(lambda e: e.matmul(at[0:64, 0:64], lhsT=Bm_sb[:, hd, csl], rhs=A_sb[:, hd, csl], start=True, stop=True), [Bm_sb, A_sb], [at])
                    self.V(lambda e: e.tensor_tensor(out=atm[:], in0=at[0:64, 0:64], in1=tri[:], op=ALU.mult), [at, tri], [atm])
                    tp = self.rot()
                    self.P(lambda e: e.transpose(out=tp[0:64, 0:128], in_=Kt_sb[:, hd, csl], identity=self.ident_f[:]), [Kt_sb, self.ident_f], [tp])
                    self.A(lambda e: e.activation(out=KtT[:], in_=tp[0:64, 0:128], func=AF.Copy), [tp], [KtT])
                    o = self.rot()
                    self.P(lambda e: e.matmul(o[0:64, 0:128], lhsT=atm[:], rhs=vsl, start=True, stop=False), [atm, v_sb], [o])
                    self.P(lambda e: e.matmul(o[0:64, 0:128], lhsT=A_sb[:, hd, csl], rhs=Sb[:, hd, :], start=False, stop=True), [A_sb, Sb], [o])
                    dS = self.rot()
                    self.P(lambda e: e.matmul(dS[:, 0:128], lhsT=KtT[:], rhs=vsl, start=True, stop=True), [KtT, v_sb], [dS])
                    self.V(lambda e: e.scalar_tensor_tensor(out=St[:, hd, :], in0=St[:, hd, :], scalar=ebl[:, hd, ck:ck + 1], in1=dS[:, 0:128], op0=ALU.mult, op1=ALU.add), [St, ebl, dS], [St])
                    self.A(lambda e: e.activation(out=Sb[:, hd, :], in_=St[:, hd, :], func=AF.Copy), [St], [Sb])
                    self.A(lambda e: e.activation(out=junk[:], in_=o[0:64, 0:128], func=AF.Square, accum_out=ss[:, 0:1]), [o], [junk, ss])
                    self.V(lambda e: e.tensor_scalar(out=ss[:], in0=ss[:], scalar1=1.0 / 128.0, scalar2=RMS_EPS, op0=ALU.mult, op1=ALU.add), [ss], [ss])
                    self.A(lambda e: e.activation(out=ss[:], in_=ss[:], func=AF.Sqrt), [ss], [ss])
                    self.V(lambda e: e.reciprocal(out=ss[:], in_=ss[:]), [ss], [ss])
                    self.V(lambda e: e.scalar_tensor_tensor(out=oc[:, ck, hd * 128:(hd + 1) * 128], in0=o[0:64, 0:128], scalar=ss[:, 0:1], in1=gsn[:, ck, hd * 128:(hd + 1) * 128], op0=ALU.mult, op1=ALU.mult), [o, ss, gsn], [oc])
            for hd in range(4):
                tp = self.rot()
                for ck in range(8):
                    self.P(lambda e: e.transpose(out=tp[:, ck * 64:(ck + 1) * 64], in_=oc[:, ck, hd * 128:(hd + 1) * 128], identity=self.ident_f[0:64, 0:64]), [oc, self.ident_f], [tp])
                self.A(lambda e: e.activation(out=ocT[:, hd, :], in_=tp[:, :], func=AF.Copy), [tp], [ocT])
            self.st(d["mixT"].t[0:4, :, tsl].rearrange("c p t -> p c t"), ocT[:], [ocT], [d["mixT"]], disjoint=True)


Builder.phase_odd_proj = phase_odd_proj


_PROG = None


def kernel(**inputs):
    global _PROG
    inputs = {k: np.asarray(v) for k, v in inputs.items()}
    if _PROG is None:
        _PROG = build_program()
    nc, b = _PROG
    consts = host_consts()
    in_maps = []
    for core in range(8):
        m = host_layout(inputs, core)
        m.update(consts)
        in_maps.append({k: m[k] for k in b.inp.keys()})
    res = run_bass_kernel_spmd(nc, in_maps, core_ids=list(range(8)))
    out = np.stack([np.asarray(r["out"]) for r in res.results], axis=0)
    return out.astype(np.float32)
```
